# Optimizing a Trainium2 kernel written in Bass

```python
import math
import jax
import jax.numpy as jnp
from jax import lax
import numpy as np


D_MODEL = 1024
BATCH = 8
SEQ = 2048
DEPTH = 4

N_MIXERS = 3
N_MEM = 256
MIX_WIDTH = (3 * D_MODEL) // 4
XATTN_HEADS = 4
XATTN_WIDTH = D_MODEL - MIX_WIDTH
XATTN_HEAD_DIM = XATTN_WIDTH // XATTN_HEADS
CAT_WIDTH = MIX_WIDTH + XATTN_WIDTH

S5_GROUP = 16
S5_GROUPS = MIX_WIDTH // S5_GROUP
S5_STATE = 64
S5_DT_MIN = 1e-3
S5_DT_MAX = 1e-1
S5_IN = MIX_WIDTH

ML_HEADS = 4
ML_QK = MIX_WIDTH // 2
ML_DQK = ML_QK // ML_HEADS
ML_DV = MIX_WIDTH // ML_HEADS
ML_CONV = 4
ML_CHUNK = 64
ML_IN = 2 * ML_QK + 2 * MIX_WIDTH + 2 * ML_HEADS

RET_HEADS = 4
RET_QK = MIX_WIDTH // 2
RET_DQK = RET_QK // RET_HEADS
RET_DV = MIX_WIDTH // RET_HEADS
RET_CHUNK = 128
RET_IN = 2 * RET_QK + 2 * MIX_WIDTH
ROPE_BASE = 10000.0

N_EXPERTS = 32
TOP_K = 4
D_EXPERT = D_MODEL
SWIGLU_LIMIT = 7.0
SWIGLU_ALPHA = 1.702
MOE_BLOCK = 256

DEEPNORM_ALPHA = (2.0 * DEPTH) ** 0.25
DEEPNORM_BETA = (8.0 * DEPTH) ** -0.25
LN_EPS = 1e-5

kernel_name = 'hybrid_s5_mlstm_retention_moe_deepnorm'


def layer_norm(h, g, b):
    hf = h.astype(jnp.float32)
    mu = jnp.mean(hf, axis=-1, keepdims=True)
    var = jnp.mean(jnp.square(hf - mu), axis=-1, keepdims=True)
    return ((hf - mu) * lax.rsqrt(var + LN_EPS) * g + b).astype(h.dtype)


def head_norm(h, g):
    hf = h.astype(jnp.float32)
    mu = jnp.mean(hf, axis=-1, keepdims=True)
    var = jnp.mean(jnp.square(hf - mu), axis=-1, keepdims=True)
    return ((hf - mu) * lax.rsqrt(var + LN_EPS)).astype(h.dtype) * g.reshape(h.shape[-2], h.shape[-1])


def to_chunks(t, chunk):
    b, l, nh, d = t.shape
    return t.reshape(b, l // chunk, chunk, nh, d).transpose(0, 3, 1, 2, 4)


def from_chunks(t):
    b, nh, nc, cl, d = t.shape
    return t.transpose(0, 2, 3, 1, 4).reshape(b, nc * cl, nh, d)


def rope_tables(positions, dim):
    inv = ROPE_BASE ** (-jnp.arange(0, dim, 2, dtype=jnp.float32) / dim)
    ang = positions.astype(jnp.float32)[..., None] * inv
    return jnp.cos(ang)[:, :, None, :], jnp.sin(ang)[:, :, None, :]


def apply_rope(t, cos, sin):
    t1, t2 = jnp.split(t, 2, axis=-1)
    return jnp.concatenate([t1 * cos - t2 * sin, t2 * cos + t1 * sin], axis=-1)


def causal_dwconv(t, w):
    return lax.conv_general_dilated(t, w.astype(t.dtype)[:, None, :], window_strides=(1,),
                                    padding=[(w.shape[0] - 1, 0)],
                                    dimension_numbers=('NWC', 'WIO', 'NWC'),
                                    feature_group_count=t.shape[-1])


def _complex_linear_combine(e1, e2):
    a1r, a1i, b1r, b1i = e1
    a2r, a2i, b2r, b2i = e2
    return (a2r * a1r - a2i * a1i, a2r * a1i + a2i * a1r,
            a2r * b1r - a2i * b1i + b2r, a2r * b1i + a2i * b1r + b2i)


def s5_mixer(u, a_re, a_im, log_dt, b_re, b_im, c_re, c_im, d_skip, w_glu, b_glu):
    bsz, seqlen, _ = u.shape
    ug = u.reshape(bsz, seqlen, S5_GROUPS, S5_GROUP).astype(jnp.float32)
    lam_re = jnp.minimum(a_re.astype(jnp.float32), -1e-4)
    lam_im = a_im.astype(jnp.float32)
    dt = jnp.exp(log_dt.astype(jnp.float32))[:, None]
    mag = jnp.exp(dt * lam_re)
    ab_re = mag * jnp.cos(dt * lam_im)
    ab_im = mag * jnp.sin(dt * lam_im)
    den = lam_re * lam_re + lam_im * lam_im
    num_re = ab_re - 1.0
    coef_re = (num_re * lam_re + ab_im * lam_im) / den
    coef_im = (ab_im * lam_re - num_re * lam_im) / den
    bre = b_re.astype(jnp.float32)
    bim = b_im.astype(jnp.float32)
    bb_re = coef_re[..., None] * bre - coef_im[..., None] * bim
    bb_im = coef_re[..., None] * bim + coef_im[..., None] * bre
    bu_re = jnp.einsum('gph,blgh->lbgp', bb_re, ug)
    bu_im = jnp.einsum('gph,blgh->lbgp', bb_im, ug)
    a_seq_re = jnp.broadcast_to(ab_re[None, None], (seqlen, 1, S5_GROUPS, S5_STATE))
    a_seq_im = jnp.broadcast_to(ab_im[None, None], (seqlen, 1, S5_GROUPS, S5_STATE))
    _, _, x_re, x_im = lax.associative_scan(_complex_linear_combine,
                                            (a_seq_re, a_seq_im, bu_re, bu_im), axis=0)
    y = (jnp.einsum('ghp,lbgp->blgh', c_re.astype(jnp.float32), x_re)
         - jnp.einsum('ghp,lbgp->blgh', c_im.astype(jnp.float32), x_im))
    y = y.reshape(bsz, seqlen, MIX_WIDTH) + d_skip * u
    y = jax.nn.gelu(y)
    return y * jax.nn.sigmoid(y @ w_glu + b_glu)


def mlstm_mixer(p, conv_q, conv_k, b_i, b_f, norm_g):
    bsz, seqlen, _ = p.shape
    q_pre, k_pre, v, o, ig_pre, fg_pre = jnp.split(
        p, [ML_QK, 2 * ML_QK, 2 * ML_QK + MIX_WIDTH, 2 * ML_QK + 2 * MIX_WIDTH,
            2 * ML_QK + 2 * MIX_WIDTH + ML_HEADS], axis=-1)
    q = jax.nn.silu(causal_dwconv(q_pre, conv_q))
    k = jax.nn.silu(causal_dwconv(k_pre, conv_k))
    qc = to_chunks(q.reshape(bsz, seqlen, ML_HEADS, ML_DQK), ML_CHUNK) * (ML_DQK ** -0.5)
    kc = to_chunks(k.reshape(bsz, seqlen, ML_HEADS, ML_DQK), ML_CHUNK)
    vc = to_chunks(v.reshape(bsz, seqlen, ML_HEADS, ML_DV), ML_CHUNK)
    ig = (ig_pre + b_i).astype(jnp.float32)
    lf = jax.nn.log_sigmoid((fg_pre + b_f).astype(jnp.float32))
    igc = to_chunks(ig[..., None], ML_CHUNK)[..., 0]
    lfc = to_chunks(lf[..., None], ML_CHUNK)[..., 0]
    bcum = jnp.cumsum(lfc, axis=-1)
    btot = bcum[..., -1]
    w = btot[..., None] - bcum + igc
    m_loc = jnp.max(w, axis=-1)
    e = jnp.exp(w - m_loc[..., None])
    kv = jnp.einsum('bhcs,bhcsk,bhcsv->bhckv', e, kc, vc).astype(jnp.float32)
    nk = jnp.einsum('bhcs,bhcsk->bhck', e, kc).astype(jnp.float32)

    def step(carry, inp):
        c_st, n_st, m_st = carry
        kv_c, nk_c, mloc_c, btot_c = inp
        m_new = jnp.maximum(btot_c + m_st, mloc_c)
        sa = jnp.exp(btot_c + m_st - m_new)
        sb = jnp.exp(mloc_c - m_new)
        c_new = sa[..., None, None] * c_st + sb[..., None, None] * kv_c
        n_new = sa[..., None] * n_st + sb[..., None] * nk_c
        return (c_new, n_new, m_new), (c_st, n_st, m_st)

    init = (jnp.zeros((bsz, ML_HEADS, ML_DQK, ML_DV), jnp.float32),
            jnp.zeros((bsz, ML_HEADS, ML_DQK), jnp.float32),
            jnp.zeros((bsz, ML_HEADS), jnp.float32))
    _, (c_prev, n_prev, m_prev) = lax.scan(
        step, init, (jnp.moveaxis(kv, 2, 0), jnp.moveaxis(nk, 2, 0),
                     jnp.moveaxis(m_loc, 2, 0), jnp.moveaxis(btot, 2, 0)))
    c_prev = jnp.moveaxis(c_prev, 0, 2)
    n_prev = jnp.moveaxis(n_prev, 0, 2)
    m_prev = jnp.moveaxis(m_prev, 0, 2)
    idx = jnp.arange(ML_CHUNK)
    causal = idx[:, None] >= idx[None, :]
    dmat = jnp.where(causal, bcum[..., :, None] - bcum[..., None, :] + igc[..., None, :], -jnp.inf)
    g = bcum + m_prev[..., None]
    m_row = jnp.maximum(g, jnp.max(dmat, axis=-1))
    inter = jnp.exp(g - m_row)
    s_qk = jnp.einsum('bhcjd,bhcsd->bhcjs', qc, kc).astype(jnp.float32) * jnp.exp(dmat - m_row[..., None])
    num = (inter[..., None] * jnp.einsum('bhcjd,bhcdv->bhcjv', qc, c_prev)
           + jnp.einsum('bhcjs,bhcsv->bhcjv', s_qk, vc))
    den = inter * jnp.einsum('bhcjd,bhcd->bhcj', qc, n_prev) + jnp.sum(s_qk, axis=-1)
    h = num / jnp.maximum(jnp.abs(den), jnp.exp(-m_row))[..., None]
    h = head_norm(from_chunks(h), norm_g).reshape(bsz, seqlen, MIX_WIDTH)
    return jax.nn.sigmoid(o) * h


def retention_mixer(p, cos, sin, norm_g):
    bsz, seqlen, _ = p.shape
    q, k, v, gate = jnp.split(p, [RET_QK, 2 * RET_QK, 2 * RET_QK + MIX_WIDTH], axis=-1)
    q = apply_rope(q.reshape(bsz, seqlen, RET_HEADS, RET_DQK), cos, sin)
    k = apply_rope(k.reshape(bsz, seqlen, RET_HEADS, RET_DQK), cos, sin) * (RET_DQK ** -0.5)
    qc = to_chunks(q, RET_CHUNK)
    kc = to_chunks(k, RET_CHUNK)
    vc = to_chunks(v.reshape(bsz, seqlen, RET_HEADS, RET_DV), RET_CHUNK)
    log_gamma = jnp.log(1.0 - jnp.power(2.0, -5.0 - jnp.arange(RET_HEADS, dtype=jnp.float32)))
    idx = jnp.arange(RET_CHUNK, dtype=jnp.float32)
    rel = idx[:, None] - idx[None, :]
    decay_intra = jnp.where(rel >= 0, jnp.exp(jnp.maximum(rel, 0.0) * log_gamma[:, None, None]), 0.0)
    s = jnp.einsum('bhcjd,bhcsd->bhcjs', qc, kc) * decay_intra[:, None]
    intra = jnp.einsum('bhcjs,bhcsv->bhcjv', s, vc)
    zeta = jnp.exp((RET_CHUNK - 1 - idx) * log_gamma[:, None])
    r = jnp.einsum('bhcsk,hs,bhcsv->bhckv', kc, zeta, vc).astype(jnp.float32)
    chunk_decay = jnp.exp(RET_CHUNK * log_gamma)[None, :, None, None]

    def step(state, r_c):
        return chunk_decay * state + r_c, state

    _, s_prev = lax.scan(step, jnp.zeros((bsz, RET_HEADS, RET_DQK, RET_DV), jnp.float32),
                         jnp.moveaxis(r, 2, 0))
    s_prev = jnp.moveaxis(s_prev, 0, 2)
    xi = jnp.exp((idx + 1.0) * log_gamma[:, None])
    cross = jnp.einsum('bhcjk,bhckv->bhcjv', qc, s_prev) * xi[:, None, :, None]
    y = head_norm(from_chunks(intra + cross), norm_g).reshape(bsz, seqlen, MIX_WIDTH)
    return jax.nn.silu(gate) * y


def memory_cross_attention(xq, mem_k, mem_v):
    bsz, seqlen, _ = xq.shape
    q = xq.reshape(bsz, seqlen, XATTN_HEADS, XATTN_HEAD_DIM) * (XATTN_HEAD_DIM ** -0.5)
    scores = jnp.einsum('blhd,bmhd->bhlm', q, mem_k).astype(jnp.float32)
    probs = jax.nn.softmax(scores, axis=-1).astype(mem_v.dtype)
    return jnp.einsum('bhlm,bmhd->blhd', probs, mem_v).reshape(bsz, seqlen, XATTN_WIDTH)


def moe_ffn(h, router_w, router_b, w_gu, b_gu, w_down, b_down):
    bsz, seqlen, d = h.shape
    t = bsz * seqlen
    xt = h.reshape(t, d)
    logits = (xt @ router_w + router_b).astype(jnp.float32)
    top_val, top_idx = lax.top_k(logits, TOP_K)
    gates = jax.nn.softmax(top_val, axis=-1)
    n_assign = t * TOP_K
    e_flat = top_idx.reshape(n_assign).astype(jnp.int32)
    g_flat = gates.reshape(n_assign)
    tok_flat = jnp.arange(n_assign, dtype=jnp.int32) // TOP_K
    counts = jax.ops.segment_sum(jnp.ones((n_assign,), jnp.int32), e_flat, num_segments=N_EXPERTS)
    padded = ((counts + MOE_BLOCK - 1) // MOE_BLOCK) * MOE_BLOCK
    pad_end = jnp.cumsum(padded)
    pad_start = pad_end - padded
    raw_start = jnp.cumsum(counts) - counts
    order = jnp.argsort(e_flat)
    se = e_flat[order]
    dest = pad_start[se] + (jnp.arange(n_assign, dtype=jnp.int32) - raw_start[se])
    n_rows = (-(-n_assign // MOE_BLOCK) + N_EXPERTS) * MOE_BLOCK
    n_blocks = n_rows // MOE_BLOCK
    row_tok = jnp.zeros((n_rows,), jnp.int32).at[dest].set(tok_flat[order])
    row_gate = jnp.zeros((n_rows,), jnp.float32).at[dest].set(g_flat[order])
    block_start = jnp.arange(n_blocks, dtype=jnp.int32) * MOE_BLOCK
    block_exp = jnp.minimum(jnp.sum((block_start[:, None] >= pad_end[None, :]).astype(jnp.int32), axis=1),
                            N_EXPERTS - 1)
    xr = xt[row_tok].reshape(n_blocks, MOE_BLOCK, d)

    def expert_block(args):
        xb, e = args
        gu = xb @ w_gu[e] + b_gu[e]
        x_glu = jnp.minimum(gu[..., :D_EXPERT], SWIGLU_LIMIT)
        x_lin = jnp.clip(gu[..., D_EXPERT:], -SWIGLU_LIMIT, SWIGLU_LIMIT)
        act = x_glu * jax.nn.sigmoid(SWIGLU_ALPHA * x_glu) * (x_lin + 1.0)
        return act @ w_down[e] + b_down[e]

    yr = lax.map(expert_block, (xr, block_exp)).reshape(n_rows, d)
    out = jnp.zeros((t, d), yr.dtype).at[row_tok].add(row_gate[:, None].astype(yr.dtype) * yr)
    return out.reshape(bsz, seqlen, d)


def _normal(k, shape, scale):
    return jax.random.normal(k, shape, jnp.float32) * scale


def _s5_params(key, prefix):
    k = jax.random.split(key, 11)
    n = jnp.arange(S5_STATE, dtype=jnp.float32)[None, :]
    gp = (S5_GROUPS, S5_STATE)
    return {
        prefix + 'w_in': _normal(k[0], (D_MODEL, S5_IN + XATTN_WIDTH), D_MODEL ** -0.5),
        prefix + 's5_a_re': -0.5 + _normal(k[1], gp, 0.01),
        prefix + 's5_a_im': math.pi * n + _normal(k[2], gp, 0.01),
        prefix + 's5_log_dt': jax.random.uniform(k[3], (S5_GROUPS,), jnp.float32,
                                                 minval=math.log(S5_DT_MIN), maxval=math.log(S5_DT_MAX)),
        prefix + 's5_b_re': _normal(k[4], (S5_GROUPS, S5_STATE, S5_GROUP), (2 * S5_GROUP) ** -0.5),
        prefix + 's5_b_im': _normal(k[5], (S5_GROUPS, S5_STATE, S5_GROUP), (2 * S5_GROUP) ** -0.5),
        prefix + 's5_c_re': _normal(k[6], (S5_GROUPS, S5_GROUP, S5_STATE), S5_STATE ** -0.5),
        prefix + 's5_c_im': _normal(k[7], (S5_GROUPS, S5_GROUP, S5_STATE), S5_STATE ** -0.5),
        prefix + 's5_d': 1.0 + _normal(k[8], (MIX_WIDTH,), 0.1),
        prefix + 's5_w_glu': _normal(k[9], (MIX_WIDTH, MIX_WIDTH), MIX_WIDTH ** -0.5),
        prefix + 's5_b_glu': _normal(k[10], (MIX_WIDTH,), 0.01),
    }


def setup_inputs(seed: int = 0) -> dict:
    key = jax.random.key(seed)
    k = jax.random.split(key, 32)
    inputs = {
        'x': _normal(k[0], (BATCH, SEQ, D_MODEL), 1.0),
        'mem': _normal(k[1], (BATCH, N_MEM, D_MODEL), 1.0),
        'positions': (jnp.arange(SEQ, dtype=jnp.int32)[None, :]
                      + jax.random.randint(k[2], (BATCH, 1), 0, 4096, dtype=jnp.int32)),
        'mem_w_k': _normal(k[3], (D_MODEL, XATTN_WIDTH), D_MODEL ** -0.5),
        'mem_w_v': _normal(k[4], (D_MODEL, XATTN_WIDTH), D_MODEL ** -0.5),
    }
    inputs.update(_s5_params(k[5], 'l0_'))
    inputs.update({
        'l1_w_in': _normal(k[6], (D_MODEL, ML_IN + XATTN_WIDTH), D_MODEL ** -0.5),
        'l1_ml_conv_q': _normal(k[7], (ML_CONV, ML_QK), ML_CONV ** -0.5),
        'l1_ml_conv_k': _normal(k[8], (ML_CONV, ML_QK), ML_CONV ** -0.5),
        'l1_ml_b_i': _normal(k[9], (ML_HEADS,), 0.1),
        'l1_ml_b_f': jnp.linspace(3.0, 6.0, ML_HEADS, dtype=jnp.float32) + _normal(k[10], (ML_HEADS,), 0.1),
        'l1_ml_norm_g': 1.0 + _normal(k[11], (MIX_WIDTH,), 0.1),
        'l2_w_in': _normal(k[12], (D_MODEL, RET_IN + XATTN_WIDTH), D_MODEL ** -0.5),
        'l2_ret_norm_g': 1.0 + _normal(k[13], (MIX_WIDTH,), 0.1),
    })
    inputs.update(_s5_params(k[14], 'l3_'))
    inputs.update({
        'w_out': _normal(k[15], (DEPTH, CAT_WIDTH, D_MODEL), CAT_WIDTH ** -0.5 * DEEPNORM_BETA),
        'ln1_g': 1.0 + _normal(k[16], (DEPTH, D_MODEL), 0.1),
        'ln1_b': _normal(k[17], (DEPTH, D_MODEL), 0.01),
        'ln2_g': 1.0 + _normal(k[18], (DEPTH, D_MODEL), 0.1),
        'ln2_b': _normal(k[19], (DEPTH, D_MODEL), 0.01),
        'router_w': _normal(k[20], (DEPTH, D_MODEL, N_EXPERTS), D_MODEL ** -0.5),
        'router_b': _normal(k[21], (DEPTH, N_EXPERTS), 0.01),
        'exp_w_gu': _normal(k[22], (DEPTH, N_EXPERTS, D_MODEL, 2 * D_EXPERT), D_MODEL ** -0.5),
        'exp_b_gu': _normal(k[23], (DEPTH, N_EXPERTS, 2 * D_EXPERT), 0.01),
        'exp_w_down': _normal(k[24], (DEPTH, N_EXPERTS, D_EXPERT, D_MODEL), D_EXPERT ** -0.5 * DEEPNORM_BETA),
        'exp_b_down': _normal(k[25], (DEPTH, N_EXPERTS, D_MODEL), 0.01),
    })
    return inputs


def reference(x, mem, positions, mem_w_k, mem_w_v,
              l0_w_in, l0_s5_a_re, l0_s5_a_im, l0_s5_log_dt, l0_s5_b_re, l0_s5_b_im,
              l0_s5_c_re, l0_s5_c_im, l0_s5_d, l0_s5_w_glu, l0_s5_b_glu,
              l1_w_in, l1_ml_conv_q, l1_ml_conv_k, l1_ml_b_i, l1_ml_b_f, l1_ml_norm_g,
              l2_w_in, l2_ret_norm_g,
              l3_w_in, l3_s5_a_re, l3_s5_a_im, l3_s5_log_dt, l3_s5_b_re, l3_s5_b_im,
              l3_s5_c_re, l3_s5_c_im, l3_s5_d, l3_s5_w_glu, l3_s5_b_glu,
              w_out, ln1_g, ln1_b, ln2_g, ln2_b, router_w, router_b,
              exp_w_gu, exp_b_gu, exp_w_down, exp_b_down):
    bsz, seqlen, _ = x.shape
    cos, sin = rope_tables(positions, RET_DQK)
    mem_k = (mem @ mem_w_k).reshape(bsz, N_MEM, XATTN_HEADS, XATTN_HEAD_DIM)
    mem_v = (mem @ mem_w_v).reshape(bsz, N_MEM, XATTN_HEADS, XATTN_HEAD_DIM)
    w_ins = (l0_w_in, l1_w_in, l2_w_in, l3_w_in)
    mixer_params = (
        (l0_s5_a_re, l0_s5_a_im, l0_s5_log_dt, l0_s5_b_re, l0_s5_b_im,
         l0_s5_c_re, l0_s5_c_im, l0_s5_d, l0_s5_w_glu, l0_s5_b_glu),
        (l1_ml_conv_q, l1_ml_conv_k, l1_ml_b_i, l1_ml_b_f, l1_ml_norm_g),
        (l2_ret_norm_g,),
        (l3_s5_a_re, l3_s5_a_im, l3_s5_log_dt, l3_s5_b_re, l3_s5_b_im,
         l3_s5_c_re, l3_s5_c_im, l3_s5_d, l3_s5_w_glu, l3_s5_b_glu),
    )
    h = x
    for i in range(DEPTH):
        kind = i % N_MIXERS
        proj = h @ w_ins[i]
        p_mix = proj[..., :-XATTN_WIDTH]
        xq = proj[..., -XATTN_WIDTH:]
        if kind == 0:
            y_mix = s5_mixer(p_mix, *mixer_params[i])
        elif kind == 1:
            y_mix = mlstm_mixer(p_mix, *mixer_params[i])
        else:
            y_mix = retention_mixer(p_mix, cos, sin, *mixer_params[i])
        y_mem = memory_cross_attention(xq, mem_k, mem_v)
        y = jnp.concatenate([y_mix.astype(y_mem.dtype), y_mem], axis=-1) @ w_out[i]
        h = layer_norm(DEEPNORM_ALPHA * h + y, ln1_g[i], ln1_b[i])
        y = moe_ffn(h, router_w[i], router_b[i], exp_w_gu[i], exp_b_gu[i], exp_w_down[i], exp_b_down[i])
        h = layer_norm(DEEPNORM_ALPHA * h + y, ln2_g[i], ln2_b[i])
    return h
```

```python
import math
import numpy as np
import concourse.bass as bass
import concourse.mybir as mybir
from concourse.bass_utils import run_bass_kernel_spmd
from contextlib import ExitStack

F32 = mybir.dt.float32
F32R = mybir.dt.float32r
I32 = mybir.dt.int32
U32 = mybir.dt.uint32
ALU = mybir.AluOpType
AF = mybir.ActivationFunctionType
AX = mybir.AxisListType

L = 2048
D = 1024
NT = 16
NB = 4
DEPTH = 4
NE = 32
CAP = 384
NSLOT = NE * CAP
ALPHA = (2.0 * DEPTH) ** 0.25
EPS = 1e-5
TWO_PI = 2.0 * math.pi
C1 = 6.28125
C2 = TWO_PI - C1
MAGIC = 12582912.0
PI_LO = 3.1415925

SAME_ENGINE_SYNC = True
EPOCH = 20000
REG = {}
DMA_RING = {"sp": 16, "pool": 8, "act": 6}


class Sched:
    def __init__(self, nc):
        self.nc = nc
        self.ops = []
        self.n_eng = {e: 0 for e in ("pe", "act", "dve", "pool", "sp")}
        self.n_dma = {}
        self.dma_rr = {q: 0 for q in DMA_RING}
        self.W = {}
        self.R = {}
        self.seen = {e: {} for e in self.n_eng}
        self.sig = set()
        self.floor = {}
        self.last_c = {}
        self.stopped = False

    def barrier(self):
        for e, o in self.last_c.items():
            self.floor[e] = o
        for s, o in self.n_dma.items():
            self.floor[s] = o

    @staticmethod
    def _dep(deps, so):
        for s, o in so.items():
            if o > deps.get(s, 0):
                deps[s] = o

    def _gather(self, table, res, deps):
        name, key = res
        d = table.get(name)
        if not d:
            return
        if key is None:
            for so in d.values():
                self._dep(deps, so)
        else:
            if key in d:
                self._dep(deps, d[key])
            if None in d:
                self._dep(deps, d[None])

    def add(self, eng, fn, reads=(), writes=(), acc=(), dma=False):
        if self.stopped:
            return None
        deps = {}
        for r in reads:
            self._gather(self.W, r, deps)
        for w in writes:
            self._gather(self.W, w, deps)
            self._gather(self.R, w, deps)
        for a in acc:
            self._gather(self.R, a, deps)
        self._dep(deps, self.floor)
        self.n_eng[eng] += 1
        if dma:
            slot = self.dma_rr[eng] % DMA_RING[eng]
            self.dma_rr[eng] += 1
            stream = ("dma", eng, slot)
            prev = self.n_dma.get(stream, 0)
            if prev:
                self._dep(deps, {stream: prev})
            self.n_dma[stream] = prev + 1
            ev = (stream, prev + 1)
        else:
            ev = (eng, self.n_eng[eng])
            self.last_c[eng] = self.n_eng[eng]
        waits = []
        seen = self.seen[eng]
        for s, o in deps.items():
            if s == eng and not dma and (eng == "pe" or not SAME_ENGINE_SYNC):
                continue
            if seen.get(s, 0) >= o:
                continue
            seen[s] = o
            waits.append((s, o))
            self.sig.add((s, o))
        self.ops.append((eng, fn, waits, ev, dma))
        for (name, key) in reads:
            self.R.setdefault(name, {}).setdefault(key, {})[ev[0]] = ev[1]
        for (name, key) in writes:
            if key is None:
                self.W[name] = {None: {ev[0]: ev[1]}}
                self.R[name] = {}
            else:
                self.W.setdefault(name, {})[key] = {ev[0]: ev[1]}
                self.R.setdefault(name, {})[key] = {}
        for (name, key) in acc:
            self.W.setdefault(name, {}).setdefault(key, {})[ev[0]] = ev[1]
        return ev

    def emit(self):
        nc = self.nc
        waits = []
        for s, o in self.n_dma.items():
            if self.seen["sp"].get(s, 0) < o:
                waits.append((s, o))
        self.ops.append(("sp", None, waits, None, False))
        sigmap = {}
        per = {e: sorted(o for (s, o) in self.sig if s == e) for e in self.n_eng}
        for e, lst in per.items():
            for i, o in enumerate(lst):
                sigmap[(e, o)] = (i // EPOCH, i % EPOCH + 1)
        with ExitStack() as es:
            esem = {}
            for e in self.n_eng:
                for k in range(max(1, (len(per[e]) + EPOCH - 1) // EPOCH)):
                    esem[(e, k)] = es.enter_context(nc.semaphore(f"s_{e}_{k}"))
            dsem = {}
            for s in self.n_dma:
                dsem[s] = es.enter_context(nc.semaphore(f"d_{s[1]}_{s[2]}"))
            block = es.enter_context(nc.Block())
            streams = {e: [] for e in self.n_eng}
            for (eng, fn, w, ev, dma) in self.ops:
                streams[eng].append((fn, w, ev, dma))
            sig = self.sig

            def lower(s, o):
                if isinstance(s, tuple):
                    return dsem[s], 16 * o
                k, v = sigmap[(s, o)]
                return esem[(s, k)], v

            def run(name, eng):
                if name == "pool":
                    REG["bc"] = eng.to_reg(NSLOT - 1)
                for (fn, w, ev, dma) in streams[name]:
                    for (s, o) in w:
                        sem, v = lower(s, o)
                        eng.wait_ge(sem, v)
                    if fn is None:
                        continue
                    ins = fn(eng)
                    if dma:
                        ins.then_inc(dsem[ev[0]], 16)
                    elif ev in sig:
                        k, v = sigmap[ev]
                        ins.then_inc(esem[(name, k)], 1)

            @block.tensor
            def _(e):
                run("pe", e)

            @block.scalar
            def _(e):
                run("act", e)

            @block.vector
            def _(e):
                run("dve", e)

            @block.gpsimd
            def _(e):
                run("pool", e)

            @block.sync
            def _(e):
                run("sp", e)


def _ap(x):
    return x[0] if isinstance(x, tuple) else x


def _rk(x):
    if isinstance(x, tuple):
        return (x[0].tensor.name, x[1])
    return (x.tensor.name, None)


class KB:
    def __init__(self, nc, es):
        self.nc = nc
        self.es = es
        self.S = Sched(nc)

    def sb(self, name, shape, dt=F32):
        return self.es.enter_context(self.nc.sbuf_tensor(name, shape, dt))

    def _add(self, eng, fn, ins, outs, acc=(), dma=False):
        self.S.add(eng, fn, [_rk(i) for i in ins if i is not None and not isinstance(i, (int, float))],
                   [_rk(o) for o in outs], [_rk(a) for a in acc], dma)

    def dma(self, out, in_, q="sp", acc=False):
        o, i = _ap(out), _ap(in_)
        self._add(q, lambda e: e.dma_start(out=o, in_=i), [in_], [] if acc else [out], [out] if acc else [], dma=True)

    def mm(self, out, lhsT, rhs, start=True, stop=True, sgc=False):
        o, l, r = _ap(out), _ap(lhsT), _ap(rhs)
        self._add("pe", lambda e: e.matmul(o, lhsT=l, rhs=r, start=start, stop=stop, skip_group_check=sgc), [lhsT, rhs], [out])

    def tr(self, out, in_, ident):
        o, i, d = _ap(out), _ap(in_), _ap(ident)
        self._add("pe", lambda e: e.transpose(o, i, d), [in_, ident], [out])

    def act(self, out, in_, func, bias=0.0, scale=1.0, accum_out=None, extra_ins=()):
        o, i = _ap(out), _ap(in_)
        b = _ap(bias) if not isinstance(bias, (int, float)) else float(bias)
        sc = _ap(scale) if not isinstance(scale, (int, float)) else float(scale)
        ac = _ap(accum_out) if accum_out is not None else None
        outs = [out] + ([accum_out] if accum_out is not None else [])

        def fn(e):
            kw = {}
            if ac is not None:
                kw["accum_out"] = ac
            return e.activation(out=o, in_=i, func=func, bias=b, scale=sc, **kw)
        self._add("act", fn, [in_, bias, scale] + list(extra_ins), outs)

    def copy(self, eng, out, in_):
        o, i = _ap(out), _ap(in_)
        if eng == "act":
            self._add("act", lambda e: e.copy(out=o, in_=i), [in_], [out])
        else:
            self._add(eng, lambda e: e.tensor_copy(out=o, in_=i), [in_], [out])

    def tt(self, eng, out, in0, in1, op):
        o, a, b = _ap(out), _ap(in0), _ap(in1)
        self._add(eng, lambda e: e.tensor_tensor(out=o, in0=a, in1=b, op=op), [in0, in1], [out])

    def ts(self, eng, out, in0, s1, s2, op0, op1=None, accum_out=None):
        o, a = _ap(out), _ap(in0)
        x1 = _ap(s1) if not isinstance(s1, (int, float)) else float(s1)
        x2 = None if s2 is None else (_ap(s2) if not isinstance(s2, (int, float)) else float(s2))
        ac = _ap(accum_out) if accum_out is not None else None
        outs = [out] + ([accum_out] if accum_out is not None else [])

        def fn(e):
            kw = {}
            if op1 is not None:
                kw["op1"] = op1
            if ac is not None:
                kw["accum_out"] = ac
            return e.tensor_scalar(out=o, in0=a, scalar1=x1, scalar2=x2, op0=op0, **kw)
        self._add(eng, fn, [in0, s1, s2], outs)

    def stt(self, out, in0, scalar, in1, op0, op1, accum_out=None):
        o, a, b = _ap(out), _ap(in0), _ap(in1)
        sc = _ap(scalar) if not isinstance(scalar, (int, float)) else float(scalar)
        ac = _ap(accum_out) if accum_out is not None else None
        outs = [out] + ([accum_out] if accum_out is not None else [])

        def fn(e):
            kw = {}
            if ac is not None:
                kw["accum_out"] = ac
            return e.scalar_tensor_tensor(out=o, in0=a, scalar=sc, in1=b, op0=op0, op1=op1, **kw)
        self._add("dve", fn, [in0, scalar, in1], outs)

    def scan(self, out, data0, data1, initial):
        o, a, b = _ap(out), _ap(data0), _ap(data1)
        ini = _ap(initial) if not isinstance(initial, (int, float)) else float(initial)
        self._add("dve", lambda e: e.tensor_tensor_scan(out=o, data0=a, data1=b, initial=ini, op0=ALU.mult, op1=ALU.add),
                  [data0, data1, initial], [out])

    def memset(self, eng, out, val):
        o = _ap(out)
        self._add(eng, lambda e: e.memset(o, val), [], [out])

    def recip(self, out, in_):
        o, i = _ap(out), _ap(in_)
        self._add("dve", lambda e: e.reciprocal(out=o, in_=i), [in_], [out])

    def generic(self, eng, fn, ins, outs):
        self._add(eng, fn, ins, outs)


def host_consts():
    c = {}
    c["ident"] = np.eye(128, dtype=np.float32)
    i = np.arange(128)
    c["stri"] = (i[:, None] < i[None, :]).astype(np.float32)
    c["ones"] = np.ones((128, 128), np.float32)
    c["iota32"] = np.tile(np.arange(32, dtype=np.float32)[None, :], (128, 1))
    c["ebase"] = np.tile((np.arange(32, dtype=np.float32) * CAP)[None, :], (128, 1))
    c["jota"] = np.tile(np.arange(1, 129, dtype=np.float32)[None, :], (128, 1))
    c["tri"] = (i[:, None] <= i[None, :]).astype(np.float32)
    lg_ = np.log(1.0 - np.power(2.0, -5.0 - np.arange(4, dtype=np.float32))).astype(np.float32)
    c["lfc"] = np.tile(lg_[None, :], (128, 1)).astype(np.float32)
    inv = (10000.0 ** (-np.arange(0, 96, 2, dtype=np.float32) / 96.0)).astype(np.float32)
    c["invf"] = np.concatenate([inv, inv])[:, None].astype(np.float32)
    c["sgn"] = np.concatenate([-np.ones(48), np.ones(48)])[:, None].astype(np.float32)
    return c


def s5_layouts(p):
    o = {}
    bblk = np.zeros((128, 6, 2, 2, 128), np.float32)
    cblk = np.zeros((128, 2, 24, 64), np.float32)
    are = np.zeros((128, 24), np.float32)
    aim = np.zeros((128, 24), np.float32)
    ldt = np.zeros((128, 24), np.float32)
    for q in range(24):
        for gi in range(2):
            g = 2 * q + gi
            r0 = 32 * (q % 4) + 16 * gi
            bblk[r0:r0 + 16, q // 4, 0, q % 2, 64 * gi:64 * gi + 64] = p["b_re"][g].T
            bblk[r0:r0 + 16, q // 4, 1, q % 2, 64 * gi:64 * gi + 64] = p["b_im"][g].T
            co = 32 * (q % 2) + 16 * gi
            cblk[64 * gi:64 * gi + 64, 0, q, co:co + 16] = p["c_re"][g].T
            cblk[64 * gi:64 * gi + 64, 1, q, co:co + 16] = p["c_im"][g].T
            are[64 * gi:64 * gi + 64, q] = p["a_re"][g]
            aim[64 * gi:64 * gi + 64, q] = p["a_im"][g]
            ldt[64 * gi:64 * gi + 64, q] = p["log_dt"][g]
    o["bblk"] = bblk
    o["cblk"] = cblk
    o["are"] = are
    o["aim"] = aim
    o["ldt"] = ldt
    o["dsk"] = np.ascontiguousarray(p["d"].reshape(6, 128).T)
    o["bglu"] = np.ascontiguousarray(p["b_glu"].reshape(6, 128).T)
    o["wglu"] = np.ascontiguousarray(p["w_glu"])
    return o


KINDS = [0, 1, 2, 0]
N_IN = [1024, 2568, 2560, 1024]
BT = 256
NBLK = L // BT
TPB = BT // 128


def layer_norm(K, z, out, g, b, stats, mv, rstd, keyed=True):
    zin = [(z[:, 0:512], 0), (z[:, 512:1024], 1)] if keyed else [z[:], z[:]]
    for hh in range(2):
        K.generic("dve", (lambda e, hh=hh: e.bn_stats(out=stats[:, hh, :], in_=z[:, hh * 512:(hh + 1) * 512])),
                  [zin[hh]], [(stats[:, hh, :], hh)])
    K.generic("dve", lambda e: e.bn_aggr(out=mv[:], in_=stats[:, :, :].rearrange("p a b -> p (a b)")),
              [(stats[:, 0, :], 0), (stats[:, 1, :], 1)], [mv[:]])
    K.ts("dve", rstd[:], mv[:, 1:2], EPS, None, ALU.add)
    K.act(rstd[:], rstd[:], AF.Sqrt)
    K.recip(rstd[:], rstd[:])
    K.generic("dve", lambda e: e.tensor_scalar(out=out[:], in0=z[:], scalar1=mv[:, 0:1], scalar2=rstd[:, 0:1],
                                               op0=ALU.subtract, op1=ALU.mult),
              zin + [mv[:], rstd[:]], [out[:]])
    K.tt("pool", out[:], out[:], g[:], ALU.mult)
    K.tt("pool", out[:], out[:], b[:], ALU.add)


class StopBuild(Exception):
    pass


def build(nlayers=DEPTH, dbg=(), stop=None):
    nc = bass.Bass("TRN2", target_bir_lowering=False)
    nc.dge_precook = False
    T = {}

    def din(name, shape, dt=F32):
        T[name] = nc.dram_tensor(name, list(shape), dt, kind="ExternalInput")
        return T[name]

    def dscr(name, shape, dt=F32):
        T[name] = nc.dram_tensor(name, list(shape), dt, kind="Internal")
        return T[name]

    din("x", [L, D]); din("mem", [256, D]); din("pos", [1, L], I32)
    din("mem_w_k", [D, 256], F32R); din("mem_w_v", [D, 256], F32R)
    for k, v in host_consts().items():
        din("c_" + k, v.shape)
    for l in range(nlayers):
        din(f"w_in{l}", [D, N_IN[l]], F32R)
        if KINDS[l] == 0:
            din(f"bblk{l}", [128, 6, 2, 2, 128], F32R); din(f"cblk{l}", [128, 2, 24, 64])
            din(f"are{l}", [128, 24]); din(f"aim{l}", [128, 24]); din(f"ldt{l}", [128, 24])
            din(f"dsk{l}", [128, 6]); din(f"bglu{l}", [128, 6]); din(f"wglu{l}", [768, 768], F32R)
        if KINDS[l] == 1:
            din(f"convq{l}", [96, 4, 4]); din(f"convk{l}", [96, 4, 4]); din(f"bi{l}", [1, 4]); din(f"bf{l}", [1, 4])
        if KINDS[l] in (1, 2):
            din(f"ng{l}", [1, 768])
    din("w_out", [DEPTH, D, D], F32R)
    for n in ("ln1_g", "ln1_b", "ln2_g", "ln2_b"):
        din(n, [DEPTH, D])
    din("router_w", [DEPTH, D, NE]); din("router_b", [DEPTH, NE])
    din("exp_w_gu", [nlayers, NE, D, 2 * D], F32R)
    din("exp_b_gu_l", [nlayers, 128, NE, 16])
    din("exp_w_down", [nlayers, NE, D, D], F32R)
    din("exp_b_down", [nlayers, NE, D], F32R)
    out_t = nc.dram_tensor("out", [L, D], F32, kind="ExternalOutput")
    dscr("H1", [L, D]); dscr("H2", [L, D]); dscr("HT", [D, L], F32R)
    dscr("Xs", [NSLOT, D]); dscr("Ys", [NSLOT, D])
    dbg_t = {n: nc.dram_tensor("dbg_" + n, [L, D], F32, kind="ExternalOutput") for n in dbg}

    with ExitStack() as es:
        K = KB(nc, es)
        sb = K.sb
        ps = [es.enter_context(nc.psum_tensor(f"ps{i}", [128, 512], F32)) for i in range(6)]
        psY = es.enter_context(nc.psum_tensor("psY", [128, 1024], F32))

        def tr8(src, dst_of_hb, engs=("dve", "act")):
            for hb in range(2):
                for c4 in range(4):
                    c = hb * 4 + c4
                    K.tr(ps[hb][:, c4 * 128:(c4 + 1) * 128], src[:, c * 128:(c + 1) * 128], ident[:])
                K.copy(engs[hb], dst_of_hb(hb), ps[hb][:, :].rearrange("p (c t) -> p c t", c=4))

        ident = sb("ident", [128, 128]); stri = sb("stri", [128, 128])
        ones = sb("ones", [128, 128]); iota32 = sb("iota32", [128, 32]); ebase = sb("ebase", [128, 32])
        jota = sb("jota", [128, 128]); tri = sb("tri", [128, 128]); lfc = sb("lfc", [128, 4])
        invf = sb("invf", [96, 1]); sgn = sb("sgn", [96, 1])
        for t_, n_ in ((ident, "ident"), (stri, "stri"), (ones, "ones"), (iota32, "iota32"),
                       (ebase, "ebase"), (jota, "jota"), (tri, "tri"), (lfc, "lfc"), (invf, "invf"), (sgn, "sgn")):
            K.dma(t_[:], T["c_" + n_].ap())
        ones_r = sb("ones_r", [128, 128], F32R)
        K.copy("dve", ones_r[:], ones[:])
        kT = sb("kT", [64, 4, 256], F32R)
        vv = sb("vv", [128, 2, 256], F32R)
        gates_all = sb("gates_all", [128, NT, 4])
        dest_all = sb("dest_all", [128, NT, 4], I32)
        mask_all = sb("mask_all", [128, NT, 32])

        with ExitStack() as es2:
            def sb2(name, shape, dt=F32):
                return es2.enter_context(nc.sbuf_tensor(name, shape, dt))
            zrow = sb2("zrow", [128, 1024])
            K.memset("pool", zrow[:], 0.0)
            for r in range(0, NSLOT, 128):
                K.dma((T["Xs"][r:r + 128, :], r // CAP), zrow[:])
            memT = sb2("memT", [128, 8, 256], F32R)
            wk = sb2("wk", [128, 8, 256], F32R)
            wv = sb2("wv", [128, 8, 256], F32R)
            mt = [sb2(f"mt{i}", [128, 1024]) for i in range(2)]
            K.dma(wk[:], T["mem_w_k"].ap().rearrange("(c p) n -> p c n", p=128))
            K.dma(wv[:], T["mem_w_v"].ap().rearrange("(c p) n -> p c n", p=128))
            for m in range(2):
                K.dma(mt[m][:], T["mem"][m * 128:(m + 1) * 128, :])
                tr8(mt[m], lambda hb, m=m: memT[:, hb * 4:(hb + 1) * 4, m * 128:(m + 1) * 128])
            for h in range(4):
                for c in range(8):
                    K.mm(ps[2][0:64, 0:256], wk[:, c, 64 * h:64 * h + 64], memT[:, c, :], start=(c == 0), stop=(c == 7))
                K.copy("dve", kT[:, h, :], ps[2][0:64, 0:256])
            for m in range(2):
                for c in range(8):
                    K.mm(ps[3][:, 0:256], memT[:, c, m * 128:(m + 1) * 128], wv[:, c, :], start=(c == 0), stop=(c == 7))
                K.copy("act", vv[:, m, :], ps[3][:, 0:256])
            HTv = T["HT"].ap().rearrange("(c p) t -> p c t", p=128)
            xts = [sb2(f"xts{i}", [128, 8, 128], F32R) for i in range(2)]
            for t in range(NT):
                xt = mt[t % 2]
                K.dma(xt[:], T["x"][t * 128:(t + 1) * 128, :])
                tr8(xt, lambda hb, t=t: xts[t % 2][:, hb * 4:(hb + 1) * 4, :])
                K.dma((HTv[:, :, t * 128:(t + 1) * 128], t // TPB), xts[t % 2][:])

        if stop == "setup":
            K.S.stopped = True
        for l in range(nlayers):
            kind = KINDS[l]
            Hres = T["x"] if l == 0 else T["H2"]
            last = (l == nlayers - 1)
            with ExitStack() as esA:
                K.S.barrier()
                def sbA(name, shape, dt=F32):
                    return esA.enter_context(nc.sbuf_tensor(f"{name}_{l}", shape, dt))
                g1 = sbA("g1", [128, 1024]); b1 = sbA("b1", [128, 1024])
                K.dma(g1[:], T["ln1_g"][l:l + 1, :].partition_broadcast(128))
                K.dma(b1[:], T["ln1_b"][l:l + 1, :].partition_broadcast(128))
                rw = sbA("rw", [128, 8, 32]); rb = sbA("rb", [1, 32])
                K.dma(rw[:], T["router_w"][l].rearrange("(c p) n -> p c n", p=128))
                K.dma(rb[:], T["router_b"][l:l + 1, :])
                hTb = sbA("hTb", [128, 8, BT], F32R)
                wp = [sbA(f"wp{i}", [128, 8, 128], F32R) for i in range(3)]
                wpi = [0]
                xqT = sbA("xqT", [64, 4, BT], F32R)
                catT = sbA("catT", [128, 6, BT], F32R)
                Eb = [sbA(f"E{i}", [128, BT], F32R) for i in range(2)]
                rec = sbA("rec", [64, BT])
                hres = [sbA(f"hres{i}", [128, 1024]) for i in range(2)]
                zt = sbA("zt", [128, 1024])
                h1t = [sbA(f"h1t{i}", [128, 1024]) for i in range(2)]
                h1T = sbA("h1T", [128, 8, 128])
                stats = sbA("stats", [128, 2, 6]); mv = sbA("mv", [128, 2]); rstd = sbA("rstd", [128, 1])
                lg = sbA("lg", [128, 32]); m8 = sbA("m8", [128, 8]); i8 = sbA("i8", [128, 8], U32)
                negm = sbA("negm", [128, 1]); e4 = sbA("e4", [128, 4]); ssum = sbA("ssum", [128, 1])
                idxf = sbA("idxf", [128, 4]); ovf = sbA("ovf", [128, 32]); slot = sbA("slot", [128, 32])
                junk = sbA("junk", [128, 32]); destf = sbA("destf", [128, 4])
                w_in = T[f"w_in{l}"].ap().rearrange("(c p) n -> p c n", p=128)
                HTv = T["HT"].ap().rearrange("(c p) t -> p c t", p=128)

                def next_wp():
                    w = wp[wpi[0] % 3]
                    wpi[0] += 1
                    return w

                def proj_fm(col0, ncols, evac):
                    w = next_wp()
                    pb = ps[wpi[0] % 2]
                    K.dma(w[:, :, 0:ncols], w_in[:, :, col0:col0 + ncols])
                    for c in range(8):
                        K.mm(pb[0:ncols, 0:BT], w[:, c, 0:ncols], hTb[:, c, :], start=(c == 0), stop=(c == 7))
                    evac(pb[0:ncols, 0:BT])

                if kind == 0:
                    bblk = sbA("bblk", [128, 6, 2, 2, 128], F32R)
                    K.dma(bblk[:], T[f"bblk{l}"].ap())
                    dsk = sbA("dsk", [128, 6])
                    K.dma(dsk[:], T[f"dsk{l}"].ap())
                    bglu = sbA("bglu", [128, 6])
                    K.dma(bglu[:], T[f"bglu{l}"].ap())
                    CSC = sbA("CSC", [128, 24, 384])
                    COS = CSC[:, :, 0:128]; SIN = CSC[:, :, 128:256]
                    rmag = sbA("rmag", [128, 24]); theta = sbA("theta", [128, 24])
                    Cp = sbA("Cp", [128, 24, 64]); Cin = sbA("Cin", [128, 24, 64])
                    with ExitStack() as esP:
                        def sbP(name, shape):
                            return esP.enter_context(nc.sbuf_tensor(f"{name}_{l}", shape, F32))
                        cblk = sbP("cblk", [128, 2, 24, 64])
                        K.dma(cblk[:], T[f"cblk{l}"].ap())
                        are = sbP("are", [128, 24]); aim = sbP("aim", [128, 24]); ldt = sbP("ldt", [128, 24])
                        K.dma(are[:], T[f"are{l}"].ap()); K.dma(aim[:], T[f"aim{l}"].ap()); K.dma(ldt[:], T[f"ldt{l}"].ap())
                        lam = sbP("lam", [128, 24]); dtt = sbP("dtt", [128, 24]); tmp = sbP("tmp", [128, 24])
                        tmp2 = sbP("tmp2", [128, 24]); sn = sbP("sn", [128, 24]); cs = sbP("cs", [128, 24])
                        abr = sbP("abr", [128, 24]); abi = sbP("abi", [128, 24]); den = sbP("den", [128, 24])
                        cre = sbP("cre", [128, 24]); cim = sbP("cim", [128, 24])
                        ANG = sbP("ANG", [128, 24, 128]); KK = sbP("KK", [128, 24, 128])
                        t32a = sbP("t32a", [128, 24, 64]); t32b = sbP("t32b", [128, 24, 64])

                        def range_sin(out, ang, kk, shift):
                            K.ts("dve", kk, ang, 1.0 / TWO_PI, shift / TWO_PI, ALU.mult, ALU.add)
                            K.ts("dve", kk, kk, MAGIC, MAGIC, ALU.add, ALU.subtract)
                            K.stt(out, kk, -C1, ang, ALU.mult, ALU.add)
                            K.stt(out, kk, -C2, out, ALU.mult, ALU.add)
                            K.ts("dve", out, out, shift, None, ALU.add)
                            K.ts("dve", out, out, -PI_LO, PI_LO, ALU.max, ALU.min)
                            K.act(out, out, AF.Sin)

                        K.ts("dve", lam[:], are[:], -1e-4, None, ALU.min)
                        K.act(dtt[:], ldt[:], AF.Exp)
                        K.tt("dve", tmp[:], dtt[:], lam[:], ALU.mult)
                        K.act(rmag[:], tmp[:], AF.Exp)
                        K.tt("dve", theta[:], dtt[:], aim[:], ALU.mult)
                        range_sin(sn[:], theta[:], tmp[:], 0.0)
                        range_sin(cs[:], theta[:], tmp[:], math.pi / 2)
                        K.tt("dve", abr[:], rmag[:], cs[:], ALU.mult)
                        K.tt("dve", abi[:], rmag[:], sn[:], ALU.mult)
                        K.ts("dve", abr[:], abr[:], -1.0, None, ALU.add)
                        K.tt("dve", den[:], lam[:], lam[:], ALU.mult)
                        K.tt("dve", tmp[:], aim[:], aim[:], ALU.mult)
                        K.tt("dve", den[:], den[:], tmp[:], ALU.add)
                        K.recip(den[:], den[:])
                        K.tt("dve", tmp[:], abr[:], lam[:], ALU.mult)
                        K.tt("dve", tmp2[:], abi[:], aim[:], ALU.mult)
                        K.tt("dve", tmp[:], tmp[:], tmp2[:], ALU.add)
                        K.tt("dve", cre[:], tmp[:], den[:], ALU.mult)
                        K.tt("dve", tmp[:], abi[:], lam[:], ALU.mult)
                        K.tt("dve", tmp2[:], abr[:], aim[:], ALU.mult)
                        K.tt("dve", tmp[:], tmp[:], tmp2[:], ALU.subtract)
                        K.tt("dve", cim[:], tmp[:], den[:], ALU.mult)
                        creb = cre[:].unsqueeze(2).to_broadcast([128, 24, 64])
                        cimb = cim[:].unsqueeze(2).to_broadcast([128, 24, 64])
                        K.tt("dve", t32a[:], cblk[:, 0, :, :], creb, ALU.mult)
                        K.tt("dve", t32b[:], cblk[:, 1, :, :], cimb, ALU.mult)
                        K.tt("dve", Cp[:], t32a[:], t32b[:], ALU.subtract)
                        K.tt("dve", t32a[:], cblk[:, 0, :, :], cimb, ALU.mult)
                        K.tt("dve", t32b[:], cblk[:, 1, :, :], creb, ALU.mult)
                        K.tt("dve", t32a[:], t32a[:], t32b[:], ALU.add)
                        K.ts("dve", Cin[:], t32a[:], -1.0, None, ALU.mult)
                        for q in range(24):
                            K.ts("dve", ANG[:, q, :], jota[:], theta[:, q:q + 1], None, ALU.mult)
                        range_sin(SIN, ANG[:, :, :], KK[:, :, :], 0.0)
                        range_sin(COS, ANG[:, :, :], KK[:, :, :], math.pi / 2)
                        K.copy("dve", CSC[:, :, 256:384], COS)

                    K.S.barrier()
                    if stop == f"P{l}":
                        K.S.stopped = True
                    uT = catT
                    ygT = sbA("ygT", [128, 6, BT], F32R)
                    ygT_f = ygT.bitcast(F32)
                    car_re = sbA("car_re", [128, 24]); car_im = sbA("car_im", [128, 24])
                    K.memset("pool", car_re[:], 0.0); K.memset("pool", car_im[:], 0.0)
                    NBUF = 4
                    mk = lambda n, w_: [sbA(f"{n}_{i}", [128, w_]) for i in range(NBUF)]
                    T12, T43, RR, WW, VAC, VBD, XX = [mk(n, 256) for n in ("T12", "T43", "RR", "WW", "VAC", "VBD", "XX")]
                    ytok = sbA("ytok", [128, 768]); yx2 = sbA("yx2", [128, 768])
                    sgl = [sbA(f"sgl{i}", [128, BT]) for i in range(2)]
                    wgluv = T[f"wglu{l}"].ap().rearrange("(c p) n -> p c n", p=128)


                if kind in (1, 2):
                    LNS = math.log(96.0 ** -0.5)
                    qT = sbA("qT", [96, 4, BT], F32R); kTt = sbA("kTt", [96, 4, BT], F32R)
                    qT_f = qT.bitcast(F32); kTt_f = kTt.bitcast(F32)
                    vt = [sbA(f"vt{i}", [128, 4, 194], F32R) for i in range(2)]
                    for v_ in vt:
                        K.memset("pool", v_.bitcast(F32)[:], 0.0)
                        K.copy("dve", v_[:, :, 192:193], ones[:, 0:4].unsqueeze(2))
                    clns = sbA("clns", [128, 1])
                    K.memset("pool", clns[:], LNS)
                    gt = [sbA(f"gt{i}", [128, 768]) for i in range(2)]
                    gps = sbA("gps", [128, 8]); lf = sbA("lf", [128, 4]); igb = sbA("igb", [128, 4]); bcol = sbA("bcol", [128, 4])
                    bias1 = sbA("bias1", [128, 4]); bias2 = sbA("bias2", [128, 4])
                    rhsB = [sbA(f"rhsB{i}", [128, 128]) for i in range(2)]
                    DT = [sbA(f"DT{i}", [128, 128]) for i in range(2)]
                    SD = [sbA(f"SD{i}", [128, 128], F32R) for i in range(2)]
                    EBt = [sbA(f"EBt{i}", [96, 128]) for i in range(2)]
                    qs = [sbA(f"qs{i}", [96, 128], F32R) for i in range(2)]
                    kw = [sbA(f"kw{i}", [128, 96], F32R) for i in range(2)]
                    wvv = [sbA(f"wvv{i}", [128, 1]) for i in range(2)]
                    ebt = [sbA(f"ebt{i}", [96, 1]) for i in range(2)]
                    Cst = [sbA(f"Cst{i}", [96, 4, 194], F32R) for i in range(2)]
                    Cst_f = [c_.bitcast(F32) for c_ in Cst]
                    K.memset("pool", Cst_f[0][:], 0.0); K.memset("pool", Cst_f[1][:], 0.0)
                    hN = sbA("hN", [128, 192]); dn = sbA("dn", [128, 1]); hst = sbA("hst", [128, 6]); hmv = sbA("hmv", [128, 2])
                    hrs = sbA("hrs", [128, 1])
                    ymx = sbA("ymx", [128, 768]); gsg = sbA("gsg", [128, 768])
                    ngb = sbA("ngb", [128, 768])
                    K.dma(ngb[:], T[f"ng{l}"].ap().partition_broadcast(128))
                    if kind == 1:
                        qpre = sbA("qpre", [96, 4, 3 + BT]); kpre = sbA("kpre", [96, 4, 3 + BT])
                        K.memset("pool", qpre[:], 0.0); K.memset("pool", kpre[:], 0.0)
                        cacc = sbA("cacc", [96, BT])
                        convq = sbA("convq", [96, 4, 4]); convk = sbA("convk", [96, 4, 4])
                        K.dma(convq[:], T[f"convq{l}"].ap()); K.dma(convk[:], T[f"convk{l}"].ap())
                        bib = sbA("bib", [128, 4]); bfb = sbA("bfb", [128, 4])
                        K.dma(bib[:], T[f"bi{l}"].ap().partition_broadcast(128))
                        K.dma(bfb[:], T[f"bf{l}"].ap().partition_broadcast(128))
                    else:
                        RC = sbA("RC", [96, L]); RS = sbA("RS", [96, L])
                        qraw = sbA("qraw", [96, BT]); qsw = sbA("qsw", [96, BT])
                        with ExitStack() as esR:
                            posb = esR.enter_context(nc.sbuf_tensor(f"posb_{l}", [96, L], I32))
                            posf = esR.enter_context(nc.sbuf_tensor(f"posf_{l}", [96, L], F32))
                            kkr = esR.enter_context(nc.sbuf_tensor(f"kkr_{l}", [96, L], F32))
                            K.dma(posb[:], T["pos"].ap().partition_broadcast(96))
                            K.copy("dve", posf[:], posb[:])
                            K.ts("dve", posf[:], posf[:], invf[:, 0:1], None, ALU.mult)

                            def range_sin2(out, ang, kk, shift):
                                K.ts("dve", kk, ang, 1.0 / TWO_PI, shift / TWO_PI, ALU.mult, ALU.add)
                                K.ts("dve", kk, kk, MAGIC, MAGIC, ALU.add, ALU.subtract)
                                K.stt(out, kk, -C1, ang, ALU.mult, ALU.add)
                                K.stt(out, kk, -C2, out, ALU.mult, ALU.add)
                                K.ts("dve", out, out, shift, None, ALU.add)
                                K.ts("dve", out, out, -PI_LO, PI_LO, ALU.max, ALU.min)
                                K.act(out, out, AF.Sin)
                            range_sin2(RS[:], posf[:], kkr[:], 0.0)
                            range_sin2(RC[:], posf[:], kkr[:], math.pi / 2)
                            K.ts("dve", RS[:], RS[:], sgn[:, 0:1], None, ALU.mult)
                        K.S.barrier()

                for tb in range(NBLK):
                    tsl = slice(tb * BT, (tb + 1) * BT)
                    K.dma(hTb[:], HTv[:, :, tsl])
                    xq0 = N_IN[l] - 256
                    for h in range(4):
                        proj_fm(xq0 + 64 * h, 64, lambda p, h=h: K.copy("act", xqT[:, h, :], p))
                    if stop == "J0":
                        K.S.stopped = True
                    if kind == 0:
                        for c in range(6):
                            proj_fm(128 * c, 128, lambda p, c=c: K.copy("dve" if c % 2 else "act", uT[:, c, :], p))
                        for s in range(TPB):
                            ssl = slice(s * 128, (s + 1) * 128)

                            def bu(q):
                                c, r0 = q // 4, 64 * ((q % 4) // 2)
                                pq = ps[2] if q % 2 == 0 else ps[5]
                                K.mm(pq[:, 0:128], bblk[r0:r0 + 64, c, 0, q % 2, :], uT[r0:r0 + 64, c, ssl])
                                K.mm(pq[:, 128:256], bblk[r0:r0 + 64, c, 1, q % 2, :], uT[r0:r0 + 64, c, ssl])
                            bu(0)
                            if stop == "U0":
                                K.S.stopped = True
                            for q in range(24):
                                c = q // 4
                                b = q % NBUF
                                pq = ps[2] if q % 2 == 0 else ps[5]
                                BR = pq[:, 0:128]
                                BI = pq[:, 128:256]
                                if q + 1 < 24:
                                    bu(q + 1)
                                K.tt("dve", T12[b][:], pq[:, 0:256], CSC[:, q, 0:256], ALU.mult)
                                K.tt("dve", T43[b][:], pq[:, 0:256], CSC[:, q, 128:384], ALU.mult)
                                K.tt("dve", RR[b][:, 0:128], T12[b][:, 0:128], T12[b][:, 128:256], ALU.add)
                                K.tt("dve", RR[b][:, 128:256], T43[b][:, 128:256], T43[b][:, 0:128], ALU.subtract)
                                K.scan(WW[b][:, 0:128], rmag[:, q:q + 1].to_broadcast([128, 128]), RR[b][:, 0:128], (car_re[:, q:q + 1], q))
                                K.scan(WW[b][:, 128:256], rmag[:, q:q + 1].to_broadcast([128, 128]), RR[b][:, 128:256], (car_im[:, q:q + 1], q))
                                K.tt("dve", VAC[b][:].rearrange("p (a b) -> p a b", a=2),
                                     WW[b][:, 0:128].unsqueeze(1).to_broadcast([128, 2, 128]),
                                     CSC[:, q, 0:256].rearrange("p (a b) -> p a b", a=2), ALU.mult)
                                K.tt("dve", VBD[b][:].rearrange("p (a b) -> p a b", a=2),
                                     WW[b][:, 128:256].unsqueeze(1).to_broadcast([128, 2, 128]),
                                     CSC[:, q, 128:384].rearrange("p (a b) -> p a b", a=2), ALU.mult)
                                K.tt("dve", XX[b][:, 0:128], VAC[b][:, 0:128], VBD[b][:, 0:128], ALU.subtract)
                                K.tt("dve", XX[b][:, 128:256], VAC[b][:, 128:256], VBD[b][:, 128:256], ALU.add)
                                K.copy("pool", (car_re[:, q:q + 1], q), XX[b][:, 127:128])
                                K.copy("pool", (car_im[:, q:q + 1], q), XX[b][:, 255:256])
                                hq = (q % 4) // 2
                                yo = (psY[64 * hq:64 * hq + 64, 128 * c:128 * c + 128], c // 4)
                                K.mm(yo, Cp[:, q, :], XX[b][:, 0:128], start=(q % 2 == 0), stop=False, sgc=True)
                                K.mm(yo, Cin[:, q, :], XX[b][:, 128:256], start=False, stop=(q % 2 == 1), sgc=True)
                            uT_f = uT.bitcast(F32)
                            for c in range(6):
                                K.stt((ytok[:, 128 * c:128 * c + 128], c), uT_f[:, c, ssl], dsk[:, c:c + 1],
                                      (psY[:, 128 * c:128 * c + 128], c // 4), ALU.mult, ALU.add)
                            K.act(yx2[:], ytok[:], AF.Square)
                            K.ts("dve", yx2[:], yx2[:], 0.044715, 1.0, ALU.mult, ALU.add)
                            K.tt("dve", yx2[:], yx2[:], ytok[:], ALU.mult)
                            K.act(yx2[:], yx2[:], AF.Sigmoid, scale=1.5957691216057308)
                            K.tt("dve", ygT[:, :, ssl], ytok[:].rearrange("p (c t) -> p c t", c=6),
                                 yx2[:].rearrange("p (c t) -> p c t", c=6), ALU.mult)
                        if stop == "S0":
                            K.S.stopped = True
                        for j in range(6):
                            w = next_wp()
                            pb = ps[j % 2]
                            K.dma(w[:, 0:6, :], wgluv[:, :, 128 * j:128 * j + 128])
                            for c in range(6):
                                K.mm(pb[:, 0:BT], w[:, c, :], ygT[:, c, :], start=(c == 0), stop=(c == 5))
                            K.act(sgl[j % 2][:], pb[:, 0:BT], AF.Sigmoid, bias=bglu[:, j:j + 1])
                            K.tt("dve", catT[:, j, :], ygT_f[:, j, :], sgl[j % 2][:], ALU.mult)
                    else:
                        for h in range(4):
                            if kind == 1:
                                proj_fm(96 * h, 96, lambda p, h=h: K.copy("act", qpre[:, h, 3:3 + BT], p))
                                proj_fm(384 + 96 * h, 96, lambda p, h=h: K.copy("act", kpre[:, h, 3:3 + BT], p))
                                for (pre, cw, dstT) in ((qpre, convq, qT), (kpre, convk, kTt)):
                                    K.ts("dve", cacc[:], pre[:, h, 3:3 + BT], cw[:, h, 3:4], None, ALU.mult)
                                    for w_ in (2, 1, 0):
                                        K.stt(cacc[:], pre[:, h, w_:w_ + BT], cw[:, h, w_:w_ + 1], cacc[:], ALU.mult, ALU.add)
                                    K.act(dstT[:, h, :], cacc[:], AF.Silu)
                            else:
                                for (c0_, dstT, dst_f) in ((0, qT, qT_f), (384, kTt, kTt_f)):
                                    proj_fm(c0_ + 96 * h, 96, lambda p: K.copy("act", qraw[:], p))
                                    w = next_wp()
                                    pb = ps[wpi[0] % 2]
                                    K.dma(w[:, :, 0:48], w_in[:, :, c0_ + 96 * h + 48:c0_ + 96 * h + 96])
                                    K.dma(w[:, :, 48:96], w_in[:, :, c0_ + 96 * h:c0_ + 96 * h + 48])
                                    for c in range(8):
                                        K.mm(pb[0:96, 0:BT], w[:, c, 0:96], hTb[:, c, :], start=(c == 0), stop=(c == 7))
                                    K.tt("dve", qsw[:], pb[0:96, 0:BT], RS[:, tsl], ALU.mult)
                                    K.tt("pool", qraw[:], qraw[:], RC[:, tsl], ALU.mult)
                                    K.tt("dve", dstT[:, h, :], qraw[:], qsw[:], ALU.add)
                        if kind == 1:
                            K.copy("pool", qpre[:, :, 0:3], qpre[:, :, BT:BT + 3])
                            K.copy("pool", kpre[:, :, 0:3], kpre[:, :, BT:BT + 3])
                        for pc in range(12):
                            w = next_wp()
                            K.dma(w[:], w_in[:, :, 768 + 128 * pc:768 + 128 * pc + 128])
                            for j in range(TPB):
                                pb = ps[(pc * TPB + j) % 2]
                                for c in range(8):
                                    K.mm(pb[:, 0:128], hTb[:, c, j * 128:(j + 1) * 128], w[:, c, :], start=(c == 0), stop=(c == 7))
                                if pc < 6:
                                    n0 = 128 * pc
                                    while n0 < 128 * pc + 128:
                                        hh_ = n0 // 192
                                        n1 = min(128 * pc + 128, 192 * (hh_ + 1))
                                        K.copy("act" if (n0 // 64) % 2 else "dve", vt[j][:, hh_, n0 - 192 * hh_:n1 - 192 * hh_],
                                               pb[:, n0 - 128 * pc:n1 - 128 * pc])
                                        n0 = n1
                                else:
                                    K.copy("act", gt[j][:, 128 * (pc - 6):128 * (pc - 6) + 128], pb[:, 0:128])
                        if kind == 1:
                            wg_ = next_wp()
                            K.dma(wg_[:, :, 0:8], w_in[:, :, 2304:2312])
                        for j in range(TPB):
                            csl = slice(j * 128, (j + 1) * 128)
                            if kind == 1:
                                for c in range(8):
                                    K.mm(ps[2][:, 0:8], hTb[:, c, csl], wg_[:, c, 0:8], start=(c == 0), stop=(c == 7))
                                K.copy("dve", gps[:], ps[2][:, 0:8])
                                K.tt("dve", igb[:], gps[:, 0:4], bib[:], ALU.add)
                                K.tt("dve", lf[:], gps[:, 4:8], bfb[:], ALU.add)
                                K.act(lf[:], lf[:], AF.Exp, scale=-1.0)
                                K.act(lf[:], lf[:], AF.Ln, bias=ones[:, 0:1])
                                K.ts("dve", lf[:], lf[:], -1.0, None, ALU.mult)
                                lfx = lf
                            else:
                                lfx = lfc
                            K.mm(ps[2][:, 0:4], tri[:], lfx[:], start=True, stop=True)
                            if kind == 1:
                                K.tt("dve", bias2[:], igb[:], ps[2][:, 0:4], ALU.subtract)
                                K.ts("dve", bias1[:], bias2[:], LNS, None, ALU.add)
                            else:
                                K.ts("dve", bias1[:], ps[2][:, 0:4], -1.0, LNS, ALU.mult, ALU.add)
                                K.copy("dve", bias2[:], bias1[:])
                            for h in range(4):
                                b = h % 2
                                cur = Cst[(tb * TPB + j) % 2]; nxt = Cst[(tb * TPB + j + 1) % 2]
                                cur_f = Cst_f[(tb * TPB + j) % 2]
                                K.ts("dve", rhsB[b][:], tri[:], lfx[:, h:h + 1], None, ALU.mult)
                                K.mm(ps[3][:, 0:128], ones[:], rhsB[b][:])
                                K.act(DT[b][:], ps[3][:, 0:128], AF.Exp, bias=bias1[:, h:h + 1])
                                K.tt("pool", DT[b][:], DT[b][:], tri[:], ALU.mult)
                                K.act(EBt[b][:], ps[3][0:96, 0:128], AF.Exp, bias=(clns[0:96, 0:1] if kind == 1 else 0.0))
                                K.tt("dve", qs[b][:], qT_f[:, h, csl], EBt[b][:], ALU.mult)
                                K.act(wvv[b][:], ps[3][:, 127:128], AF.Exp, bias=bias2[:, h:h + 1])
                                K.act(ebt[b][:], ps[3][0:96, 127:128], AF.Exp)
                                K.tr(ps[4][:, 0:96], kTt_f[:, h, csl], ident[0:96, 0:96])
                                K.ts("dve", kw[b][:], ps[4][:, 0:96], wvv[b][:, 0:1], None, ALU.mult)
                                K.mm(ps[5][:, 0:128], kTt[:, h, csl], qT[:, h, csl])
                                K.tt("dve", SD[b][:], ps[5][:, 0:128], DT[b][:], ALU.mult)
                                K.mm((psY[:, 0:194], 0), SD[b][:], vt[j][:, h, :], start=True, stop=False)
                                K.mm((psY[:, 0:194], 0), qs[b][:], (cur[:, h, :], h), start=False, stop=True)
                                K.mm((psY[0:96, 512:706], 1), kw[b][:], vt[j][:, h, :], start=True, stop=True)
                                K.stt((nxt[:, h, :], h), (cur_f[:, h, :], h), ebt[b][:, 0:1], (psY[0:96, 512:706], 1), ALU.mult, ALU.add)
                                if kind == 1:
                                    K.act(dn[:], (psY[:, 192:193], 0), AF.Abs)
                                    K.ts("dve", dn[:], dn[:], 1.0, None, ALU.max)
                                    K.recip(dn[:], dn[:])
                                    K.ts("dve", hN[:], (psY[:, 0:192], 0), dn[:, 0:1], None, ALU.mult)
                                else:
                                    K.copy("dve", hN[:], (psY[:, 0:192], 0))
                                K.generic("dve", lambda e, hst=hst, hN=hN: e.bn_stats(out=hst[:], in_=hN[:]), [hN[:]], [hst[:]])
                                K.generic("dve", lambda e, hst=hst, hmv=hmv: e.bn_aggr(out=hmv[:], in_=hst[:]), [hst[:]], [hmv[:]])
                                K.ts("dve", hrs[:], hmv[:, 1:2], EPS, None, ALU.add)
                                K.act(hrs[:], hrs[:], AF.Sqrt)
                                K.recip(hrs[:], hrs[:])
                                K.ts("dve", (ymx[:, 192 * h:192 * h + 192], h), hN[:], hmv[:, 0:1], hrs[:, 0:1], ALU.subtract, ALU.mult)
                            K.tt("pool", ymx[:], ymx[:], ngb[:], ALU.mult)
                            K.act(gsg[:], gt[j][:], AF.Sigmoid if kind == 1 else AF.Silu)
                            K.tt("dve", ymx[:], ymx[:], gsg[:], ALU.mult)
                            for c in range(6):
                                pb = ps[c // 4]
                                K.tr(pb[:, (c % 4) * 128:(c % 4) * 128 + 128], ymx[:, 128 * c:128 * c + 128], ident[:])
                            K.copy("act", catT[:, 0:4, csl], ps[0][:, :].rearrange("p (c t) -> p c t", c=4))
                            K.copy("dve", catT[:, 4:6, csl], ps[1][:, 0:256].rearrange("p (c t) -> p c t", c=2))

                    if stop == "G0":
                        K.S.stopped = True
                    for h in range(4):
                        for m in range(2):
                            K.mm(ps[3 + m][:, 0:BT], kT[:, h, m * 128:(m + 1) * 128], xqT[:, h, :])
                            K.act(Eb[m][:], ps[3 + m][:, 0:BT], AF.Exp, scale=0.125)
                        for m in range(2):
                            K.mm(ps[5][0:64, 0:BT], vv[:, m, 64 * h:64 * h + 64], Eb[m][:], start=(m == 0), stop=(m == 1))
                        for m in range(2):
                            K.mm(ps[2][0:64, 0:BT], ones_r[:, 0:64], Eb[m][:], start=(m == 0), stop=(m == 1))
                        K.recip(rec[:], ps[2][0:64, 0:BT])
                        K.tt("dve", xqT[:, h, :], ps[5][0:64, 0:BT], rec[:], ALU.mult)
                    ymT = xqT

                    if stop == "X0":
                        K.S.stopped = True
                    accs = [(psY[:, 0:512], 0), (psY[:, 512:1024], 1), ps[3][:, :], ps[4][:, :]]
                    for c in range(10):
                        w = next_wp()
                        wv_ = w[:, :, :].rearrange("p a b -> p (a b)")
                        if c < 6:
                            K.dma(w[:], T["w_out"][l, 128 * c:128 * c + 128, :].rearrange("p (a b) -> p a b", b=128))
                        else:
                            hq = c - 6
                            K.generic_dma = None
                            K.dma((w[0:64, :, :], None), T["w_out"][l, 768 + 64 * hq:768 + 64 * hq + 64, :].rearrange("p (a b) -> p a b", b=128))
                        for j in range(TPB):
                            jsl = slice(j * 128, (j + 1) * 128)
                            for hh in range(2):
                                a = accs[j * 2 + hh]
                                if c < 6:
                                    K.mm(a, catT[:, c, jsl], wv_[:, hh * 512:(hh + 1) * 512], start=(c == 0), stop=False)
                                else:
                                    K.mm(a, ymT[:, c - 6, jsl], wv_[0:64, hh * 512:(hh + 1) * 512], start=False, stop=(c == 9))
                    if stop == "O0":
                        K.S.stopped = True
                    for j in range(TPB):
                        ti = tb * TPB + j
                        hr = hres[ti % 2]; z = zt; h1 = h1t[ti % 2]
                        K.dma(hr[:], (Hres[ti * 128:(ti + 1) * 128, :], ti))
                        for hh in range(2):
                            K.stt((z[:, hh * 512:(hh + 1) * 512], hh), hr[:, hh * 512:(hh + 1) * 512], ALPHA, accs[j * 2 + hh], ALU.mult, ALU.add)
                        layer_norm(K, z, h1, g1, b1, stats, mv, rstd)
                        K.dma((T["H1"][ti * 128:(ti + 1) * 128, :], ti), h1[:], q="act")
                        if stop == "L0":
                            K.S.stopped = True
                        tr8(h1, lambda hb: h1T[:, hb * 4:(hb + 1) * 4, :])
                        for c in range(8):
                            K.mm(ps[5][:, 0:32], h1T[:, c, :], rw[:, c, :], start=(c == 0), stop=False)
                        K.mm(ps[5][:, 0:32], ones[0:1, :], rb[0:1, :], start=False, stop=True)
                        K.copy("dve", lg[:], ps[5][:, 0:32])
                        K.generic("dve", lambda e, m8=m8, lg=lg: e.max(out=m8[:], in_=lg[:]), [lg[:]], [m8[:]])
                        K.generic("dve", lambda e, m8=m8, lg=lg, i8=i8: e.max_index(out=i8[:], in_max=m8[:], in_values=lg[:]), [lg[:], m8[:]], [i8[:]])
                        K.ts("dve", negm[:], m8[:, 0:1], -1.0, None, ALU.mult)
                        K.act(e4[:], m8[:, 0:4], AF.Exp, bias=negm[:], accum_out=ssum[:])
                        K.recip(ssum[:], ssum[:])
                        K.ts("dve", (gates_all[:, ti, :], ti), e4[:], ssum[:], None, ALU.mult)
                        K.copy("dve", idxf[:], i8[:, 0:4])
                        K.ts("dve", (mask_all[:, ti, :], ti), lg[:], m8[:, 3:4], None, ALU.is_ge)
                        for t2_ in range(ti):
                            K.mm(ps[2][:, 0:32], ones[:], (mask_all[:, t2_, :], t2_), start=(t2_ == 0), stop=False)
                        K.mm(ps[2][:, 0:32], stri[:], (mask_all[:, ti, :], ti), start=(ti == 0), stop=True)
                        K.ts("dve", ovf[:], ps[2][:, 0:32], float(CAP), 1.0e6, ALU.is_ge, ALU.mult)
                        K.tt("dve", slot[:], ps[2][:, 0:32], ebase[:], ALU.add)
                        K.tt("dve", slot[:], slot[:], ovf[:], ALU.add)
                        for k in range(4):
                            K.stt(junk[:], iota32[:], idxf[:, k:k + 1], slot[:], ALU.is_equal, ALU.mult, accum_out=(destf[:, k:k + 1], k))
                        K.generic("dve", lambda e, ti=ti, destf=destf: e.tensor_copy(out=dest_all[:, ti, :], in_=destf[:]),
                                  [(destf[:, k:k + 1], k) for k in range(4)], [(dest_all[:, ti, :], ti)])
                        if stop == "R0":
                            K.S.stopped = True
                        for k in range(4):
                            def sc(e, ti=ti, k=k, h1=h1):
                                return e.indirect_dma_start(
                                    out=T["Xs"].ap(), out_offset=bass.IndirectOffsetOnAxis(ap=dest_all[:, ti, k:k + 1], axis=0),
                                    in_=h1[:], in_offset=None, bounds_check=REG["bc"], oob_is_err=False)
                            K.S.add("pool", sc, [_rk(h1[:]), _rk((dest_all[:, ti, :], ti))], [],
                                    [("Xs", e_) for e_ in range(NE)], dma=True)
            if stop == f"A{l}":
                K.S.stopped = True

            with ExitStack() as esB:
                K.S.barrier()
                def sbB(name, shape, dt=F32):
                    return esB.enter_context(nc.sbuf_tensor(f"{name}_{l}", shape, dt))
                XT = [sbB(f"XT{i}", [128, 8, CAP], F32R) for i in range(2)]
                xs = [sbB(f"xs{i}", [128, 1024]) for i in range(2)]
                NWG = 6
                wg = [sbB(f"wg{i}", [128, 8, 256], F32R) for i in range(NWG)]
                NWD = 10
                wd = [sbB(f"wd{i}", [128, 1024], F32R) for i in range(NWD)]
                bd = [sbB(f"bd{i}", [1, 1024], F32R) for i in range(2)]
                actT = sbB("actT", [128, 8, CAP], F32R)
                bgu = sbB("bgu", [128, NE, 16])
                K.dma(bgu[:], T["exp_b_gu_l"][l])
                K.ts("dve", bgu[:, :, 8:16], bgu[:, :, 8:16], 1.0, None, ALU.add)
                gg, sg, ll = [[sbB(f"{n}{i}", [128, CAP]) for i in range(2)] for n in ("gg", "sg", "ll")]
                yt = [sbB(f"yt{i}", [128, 1024]) for i in range(2)]
                xi = 0; gi_ = 0; yi = 0; wdi = 0
                def load_xt(e2):
                    nonlocal_xi = xi_box
                    X2 = XT[e2 % 2]
                    for t in range(3):
                        x_ = xs[nonlocal_xi[0] % 2]; nonlocal_xi[0] += 1
                        r0 = e2 * CAP + t * 128
                        K.dma(x_[:], (T["Xs"][r0:r0 + 128, :], e2))
                        tr8(x_, lambda hb, X2=X2, t=t: X2[:, hb * 4:(hb + 1) * 4, t * 128:(t + 1) * 128])
                xi_box = [0]
                load_xt(0)
                for e_ in range(NE):
                    X = XT[e_ % 2]
                    wgu = T["exp_w_gu"][l, e_].rearrange("(c p) n -> p c n", p=128)
                    for jj in range(4):
                        wG = wg[gi_ % NWG]; gi_ += 1
                        wL = wg[gi_ % NWG]; gi_ += 1
                        K.dma(wG[:], wgu[:, :, 256 * jj:256 * jj + 256])
                        K.dma(wL[:], wgu[:, :, 1024 + 256 * jj:1024 + 256 * jj + 256])
                        for j2 in range(2):
                            j = 2 * jj + j2
                            pG = ps[2 + 2 * j2]; pL = ps[3 + 2 * j2]
                            for c in range(8):
                                K.mm(pG[:, 0:CAP], wG[:, c, 128 * j2:128 * j2 + 128], X[:, c, :], start=(c == 0), stop=(c == 7))
                            for c in range(8):
                                K.mm(pL[:, 0:CAP], wL[:, c, 128 * j2:128 * j2 + 128], X[:, c, :], start=(c == 0), stop=(c == 7))
                            b = j2
                            K.ts("dve", gg[b][:], pG[:, 0:CAP], bgu[:, e_, j:j + 1], 7.0, ALU.add, ALU.min)
                            K.act(sg[b][:], gg[b][:], AF.Silu, scale=1.702)
                            K.act(ll[b][:], pL[:, 0:CAP], AF.Identity, bias=bgu[:, e_, 8 + j:9 + j])
                            K.ts("dve", ll[b][:], ll[b][:], 8.0, -6.0, ALU.min, ALU.max)
                            K.stt((actT[:, j, :], j), sg[b][:], 1.0 / 1.702, ll[b][:], ALU.mult, ALU.mult)
                    wds = []
                    for j in range(8):
                        w = wd[wdi % NWD]; wdi += 1
                        wds.append(w)
                        K.dma(w[:], T["exp_w_down"][l, e_, 128 * j:128 * j + 128, :])
                    K.dma(bd[e_ % 2][:], T["exp_b_down"][l, e_:e_ + 1, :])
                    if e_ + 1 < NE:
                        load_xt(e_ + 1)
                    for t in range(3):
                        y_ = yt[yi % 2]; yi += 1
                        for hh in range(2):
                            pb = (psY[:, hh * 512:(hh + 1) * 512], hh)
                            for j in range(8):
                                K.mm(pb, (actT[:, j, t * 128:(t + 1) * 128], j), wds[j][:, hh * 512:(hh + 1) * 512],
                                     start=(j == 0), stop=False)
                            K.mm(pb, ones_r[0:1, :], bd[e_ % 2][0:1, hh * 512:(hh + 1) * 512], start=False, stop=True)
                            K.copy("act" if hh else "dve", (y_[:, hh * 512:(hh + 1) * 512], hh), pb)
                        r0 = e_ * CAP + t * 128
                        K.S.add("act", (lambda e, r0=r0, y_=y_: e.dma_start(out=T["Ys"][r0:r0 + 128, :], in_=y_[:])),
                                [_rk(y_[:])], [], [("Ys", None)], dma=True)
            if stop == f"B{l}":
                K.S.stopped = True

            with ExitStack() as esC:
                K.S.barrier()
                def sbC(name, shape, dt=F32):
                    return esC.enter_context(nc.sbuf_tensor(f"{name}_{l}", shape, dt))
                g2 = sbC("g2", [128, 1024]); b2 = sbC("b2", [128, 1024])
                K.dma(g2[:], T["ln2_g"][l:l + 1, :].partition_broadcast(128))
                K.dma(b2[:], T["ln2_b"][l:l + 1, :].partition_broadcast(128))
                yg = [sbC(f"yg{i}", [128, 1024]) for i in range(8)]
                for y_ in yg:
                    K.memset("pool", y_[:], 0.0)
                hr2 = [sbC(f"hr2{i}", [128, 1024]) for i in range(2)]
                macc = [sbC(f"macc{i}", [128, 1024]) for i in range(2)]
                zt2 = [sbC(f"zt2{i}", [128, 1024]) for i in range(2)]
                h2t = [sbC(f"h2t{i}", [128, 1024]) for i in range(2)]
                hT2 = [sbC(f"hT2{i}", [128, 8, 128], F32R) for i in range(2)]
                stats = sbC("stats2", [128, 2, 6]); mv = sbC("mv2", [128, 2]); rstd = sbC("rstd2", [128, 1])
                HTv = T["HT"].ap().rearrange("(c p) t -> p c t", p=128)
                for ti in range(NT):
                    hr = hr2[ti % 2]; m_ = macc[ti % 2]; z = zt2[ti % 2]; h2 = h2t[ti % 2]
                    K.dma(hr[:], (T["H1"][ti * 128:(ti + 1) * 128, :], ti))
                    for k in range(4):
                        y_ = yg[(ti % 2) * 4 + k]

                        def ga(e, ti=ti, k=k, y_=y_):
                            return e.indirect_dma_start(
                                out=y_[:], out_offset=None, in_=T["Ys"].ap(),
                                in_offset=bass.IndirectOffsetOnAxis(ap=dest_all[:, ti, k:k + 1], axis=0),
                                bounds_check=REG["bc"], oob_is_err=False)
                        K.S.add("pool", ga, [("Ys", None), _rk((dest_all[:, ti, :], ti))], [_rk(y_[:])], [], dma=True)
                        if k == 0:
                            K.ts("dve", m_[:], y_[:], (gates_all[:, ti, 0:1], ti), None, ALU.mult)
                        else:
                            K.stt(m_[:], y_[:], (gates_all[:, ti, k:k + 1], ti), m_[:], ALU.mult, ALU.add)
                    K.stt(z[:], hr[:], ALPHA, m_[:], ALU.mult, ALU.add)
                    layer_norm(K, z, h2, g2, b2, stats, mv, rstd, keyed=False)
                    dst = out_t if last else T["H2"]
                    K.dma((dst[ti * 128:(ti + 1) * 128, :], ti), h2[:], q="act")
                    if not last:
                        hx = hT2[ti % 2]
                        tr8(h2, lambda hb, hx=hx: hx[:, hb * 4:(hb + 1) * 4, :])
                        K.dma((HTv[:, :, ti * 128:(ti + 1) * 128], ti // TPB), hx[:], q="act")

        K.S.stopped = False
        K.S.barrier()
        for n, t_ in dbg_t.items():
            for ti in range(NT):
                K.dma((t_[ti * 128:(ti + 1) * 128, :], ti), (T[n][ti * 128:(ti + 1) * 128, :], ti))
        K.S.emit()
    return nc


def make_in_maps(inputs, nlayers=DEPTH, cores=range(8)):
    f = lambda a: np.ascontiguousarray(a, dtype=np.float32)
    shared = {}
    shared["mem_w_k"] = f(inputs["mem_w_k"]); shared["mem_w_v"] = f(inputs["mem_w_v"])
    for k, v in host_consts().items():
        shared["c_" + k] = v
    for l in range(nlayers):
        shared[f"w_in{l}"] = f(inputs[f"l{l}_w_in"])
        if KINDS[l] == 0:
            p = {n: np.asarray(inputs[f"l{l}_s5_{n}"]) for n in
                 ("a_re", "a_im", "log_dt", "b_re", "b_im", "c_re", "c_im", "d", "w_glu", "b_glu")}
            for k, v in s5_layouts(p).items():
                shared[f"{k}{l}"] = f(v)
        if KINDS[l] == 1:
            cq = np.asarray(inputs[f"l{l}_ml_conv_q"]).reshape(4, 4, 96)
            ck = np.asarray(inputs[f"l{l}_ml_conv_k"]).reshape(4, 4, 96)
            shared[f"convq{l}"] = f(cq.transpose(2, 1, 0)); shared[f"convk{l}"] = f(ck.transpose(2, 1, 0))
            shared[f"bi{l}"] = f(np.asarray(inputs[f"l{l}_ml_b_i"]).reshape(1, 4))
            shared[f"bf{l}"] = f(np.asarray(inputs[f"l{l}_ml_b_f"]).reshape(1, 4))
            shared[f"ng{l}"] = f(np.asarray(inputs[f"l{l}_ml_norm_g"]).reshape(1, 768))
        if KINDS[l] == 2:
            shared[f"ng{l}"] = f(np.asarray(inputs[f"l{l}_ret_norm_g"]).reshape(1, 768))
    for n in ("w_out", "ln1_g", "ln1_b", "ln2_g", "ln2_b", "router_w", "router_b"):
        shared[n] = f(inputs[n])
    for n in ("exp_w_gu", "exp_w_down", "exp_b_down"):
        shared[n] = f(inputs[n][:nlayers])
    bgu = np.asarray(inputs["exp_b_gu"]).reshape(DEPTH, NE, 16, 128)
    shared["exp_b_gu_l"] = f(bgu.transpose(0, 3, 1, 2)[:nlayers])
    maps = []
    for c in cores:
        m = dict(shared)
        m["x"] = f(inputs["x"][c]); m["mem"] = f(inputs["mem"][c])
        m["pos"] = np.ascontiguousarray(np.asarray(inputs["positions"][c]).reshape(1, L).astype(np.int32))
        maps.append(m)
    return maps


def kernel(**inputs):
    nc = build()
    maps = make_in_maps(inputs)
    res = run_bass_kernel_spmd(nc, maps, core_ids=list(range(8)))
    return np.stack([np.asarray(r["out"]) for r in res.results], axis=0).astype(np.float32)
```

```python
import math
import numpy as np
import concourse.bass as bass
import concourse.mybir as mybir
from concourse.bass_utils import run_bass_kernel_spmd
from contextlib import ExitStack

F32 = mybir.dt.float32
F32R = mybir.dt.float32r
I32 = mybir.dt.int32
U32 = mybir.dt.uint32
ALU = mybir.AluOpType
AF = mybir.ActivationFunctionType
AX = mybir.AxisListType

L = 2048
D = 1024
NT = 16
NB = 4
DEPTH = 4
NE = 32
CAP = 384
NSLOT = NE * CAP
ALPHA = (2.0 * DEPTH) ** 0.25
EPS = 1e-5
TWO_PI = 2.0 * math.pi
C1 = 6.28125
C2 = TWO_PI - C1
MAGIC = 12582912.0
PI_LO = 3.1415925

SAME_ENGINE_SYNC = True
EPOCH = 20000
REG = {}
DMA_RING = {"sp": 16, "pool": 8, "act": 6}


class Sched:
    def __init__(self, nc):
        self.nc = nc
        self.ops = []
        self.n_eng = {e: 0 for e in ("pe", "act", "dve", "pool", "sp")}
        self.n_dma = {}
        self.dma_rr = {q: 0 for q in DMA_RING}
        self.W = {}
        self.R = {}
        self.seen = {e: {} for e in self.n_eng}
        self.sig = set()
        self.floor = {}
        self.last_c = {}
        self.stopped = False

    def barrier(self):
        for e, o in self.last_c.items():
            self.floor[e] = o
        for s, o in self.n_dma.items():
            self.floor[s] = o

    @staticmethod
    def _dep(deps, so):
        for s, o in so.items():
            if o > deps.get(s, 0):
                deps[s] = o

    def _gather(self, table, res, deps):
        name, key = res
        d = table.get(name)
        if not d:
            return
        if key is None:
            for so in d.values():
                self._dep(deps, so)
        else:
            if key in d:
                self._dep(deps, d[key])
            if None in d:
                self._dep(deps, d[None])

    def add(self, eng, fn, reads=(), writes=(), acc=(), dma=False):
        if self.stopped:
            return None
        deps = {}
        for r in reads:
            self._gather(self.W, r, deps)
        for w in writes:
            self._gather(self.W, w, deps)
            self._gather(self.R, w, deps)
        for a in acc:
            self._gather(self.R, a, deps)
        self._dep(deps, self.floor)
        self.n_eng[eng] += 1
        if dma:
            slot = self.dma_rr[eng] % DMA_RING[eng]
            self.dma_rr[eng] += 1
            stream = ("dma", eng, slot)
            prev = self.n_dma.get(stream, 0)
            if prev:
                self._dep(deps, {stream: prev})
            self.n_dma[stream] = prev + 1
            ev = (stream, prev + 1)
        else:
            ev = (eng, self.n_eng[eng])
            self.last_c[eng] = self.n_eng[eng]
        waits = []
        seen = self.seen[eng]
        for s, o in deps.items():
            if s == eng and not dma and (eng == "pe" or not SAME_ENGINE_SYNC):
                continue
            if seen.get(s, 0) >= o:
                continue
            seen[s] = o
            waits.append((s, o))
            self.sig.add((s, o))
        self.ops.append((eng, fn, waits, ev, dma))
        for (name, key) in reads:
            self.R.setdefault(name, {}).setdefault(key, {})[ev[0]] = ev[1]
        for (name, key) in writes:
            if key is None:
                self.W[name] = {None: {ev[0]: ev[1]}}
                self.R[name] = {}
            else:
                self.W.setdefault(name, {})[key] = {ev[0]: ev[1]}
                self.R.setdefault(name, {})[key] = {}
        for (name, key) in acc:
            self.W.setdefault(name, {}).setdefault(key, {})[ev[0]] = ev[1]
        return ev

    def emit(self):
        nc = self.nc
        waits = []
        for s, o in self.n_dma.items():
            if self.seen["sp"].get(s, 0) < o:
                waits.append((s, o))
        self.ops.append(("sp", None, waits, None, False))
        sigmap = {}
        per = {e: sorted(o for (s, o) in self.sig if s == e) for e in self.n_eng}
        for e, lst in per.items():
            for i, o in enumerate(lst):
                sigmap[(e, o)] = (i // EPOCH, i % EPOCH + 1)
        with ExitStack() as es:
            esem = {}
            for e in self.n_eng:
                for k in range(max(1, (len(per[e]) + EPOCH - 1) // EPOCH)):
                    esem[(e, k)] = es.enter_context(nc.semaphore(f"s_{e}_{k}"))
            dsem = {}
            for s in self.n_dma:
                dsem[s] = es.enter_context(nc.semaphore(f"d_{s[1]}_{s[2]}"))
            block = es.enter_context(nc.Block())
            streams = {e: [] for e in self.n_eng}
            for (eng, fn, w, ev, dma) in self.ops:
                streams[eng].append((fn, w, ev, dma))
            sig = self.sig

            def lower(s, o):
                if isinstance(s, tuple):
                    return dsem[s], 16 * o
                k, v = sigmap[(s, o)]
                return esem[(s, k)], v

            def run(name, eng):
                if name == "pool":
                    REG["bc"] = eng.to_reg(NSLOT - 1)
                for (fn, w, ev, dma) in streams[name]:
                    for (s, o) in w:
                        sem, v = lower(s, o)
                        eng.wait_ge(sem, v)
                    if fn is None:
                        continue
                    ins = fn(eng)
                    if dma:
                        ins.then_inc(dsem[ev[0]], 16)
                    elif ev in sig:
                        k, v = sigmap[ev]
                        ins.then_inc(esem[(name, k)], 1)

            @block.tensor
            def _(e):
                run("pe", e)

            @block.scalar
            def _(e):
                run("act", e)

            @block.vector
            def _(e):
                run("dve", e)

            @block.gpsimd
            def _(e):
                run("pool", e)

            @block.sync
            def _(e):
                run("sp", e)


def _ap(x):
    return x[0] if isinstance(x, tuple) else x


def _rk(x):
    if isinstance(x, tuple):
        return (x[0].tensor.name, x[1])
    return (x.tensor.name, None)


class KB:
    def __init__(self, nc, es):
        self.nc = nc
        self.es = es
        self.S = Sched(nc)

    def sb(self, name, shape, dt=F32):
        return self.es.enter_context(self.nc.sbuf_tensor(name, shape, dt))

    def _add(self, eng, fn, ins, outs, acc=(), dma=False):
        self.S.add(eng, fn, [_rk(i) for i in ins if i is not None and not isinstance(i, (int, float))],
                   [_rk(o) for o in outs], [_rk(a) for a in acc], dma)

    def dma(self, out, in_, q="sp", acc=False):
        o, i = _ap(out), _ap(in_)
        self._add(q, lambda e: e.dma_start(out=o, in_=i), [in_], [] if acc else [out], [out] if acc else [], dma=True)

    def mm(self, out, lhsT, rhs, start=True, stop=True, sgc=False):
        o, l, r = _ap(out), _ap(lhsT), _ap(rhs)
        self._add("pe", lambda e: e.matmul(o, lhsT=l, rhs=r, start=start, stop=stop, skip_group_check=sgc), [lhsT, rhs], [out])

    def tr(self, out, in_, ident):
        o, i, d = _ap(out), _ap(in_), _ap(ident)
        self._add("pe", lambda e: e.transpose(o, i, d), [in_, ident], [out])

    def act(self, out, in_, func, bias=0.0, scale=1.0, accum_out=None, extra_ins=()):
        o, i = _ap(out), _ap(in_)
        b = _ap(bias) if not isinstance(bias, (int, float)) else float(bias)
        sc = _ap(scale) if not isinstance(scale, (int, float)) else float(scale)
        ac = _ap(accum_out) if accum_out is not None else None
        outs = [out] + ([accum_out] if accum_out is not None else [])

        def fn(e):
            kw = {}
            if ac is not None:
                kw["accum_out"] = ac
            return e.activation(out=o, in_=i, func=func, bias=b, scale=sc, **kw)
        self._add("act", fn, [in_, bias, scale] + list(extra_ins), outs)

    def copy(self, eng, out, in_):
        o, i = _ap(out), _ap(in_)
        if eng == "act":
            self._add("act", lambda e: e.copy(out=o, in_=i), [in_], [out])
        else:
            self._add(eng, lambda e: e.tensor_copy(out=o, in_=i), [in_], [out])

    def tt(self, eng, out, in0, in1, op):
        o, a, b = _ap(out), _ap(in0), _ap(in1)
        self._add(eng, lambda e: e.tensor_tensor(out=o, in0=a, in1=b, op=op), [in0, in1], [out])

    def ts(self, eng, out, in0, s1, s2, op0, op1=None, accum_out=None):
        o, a = _ap(out), _ap(in0)
        x1 = _ap(s1) if not isinstance(s1, (int, float)) else float(s1)
        x2 = None if s2 is None else (_ap(s2) if not isinstance(s2, (int, float)) else float(s2))
        ac = _ap(accum_out) if accum_out is not None else None
        outs = [out] + ([accum_out] if accum_out is not None else [])

        def fn(e):
            kw = {}
            if op1 is not None:
                kw["op1"] = op1
            if ac is not None:
                kw["accum_out"] = ac
            return e.tensor_scalar(out=o, in0=a, scalar1=x1, scalar2=x2, op0=op0, **kw)
        self._add(eng, fn, [in0, s1, s2], outs)

    def stt(self, out, in0, scalar, in1, op0, op1, accum_out=None):
        o, a, b = _ap(out), _ap(in0), _ap(in1)
        sc = _ap(scalar) if not isinstance(scalar, (int, float)) else float(scalar)
        ac = _ap(accum_out) if accum_out is not None else None
        outs = [out] + ([accum_out] if accum_out is not None else [])

        def fn(e):
            kw = {}
            if ac is not None:
                kw["accum_out"] = ac
            return e.scalar_tensor_tensor(out=o, in0=a, scalar=sc, in1=b, op0=op0, op1=op1, **kw)
        self._add("dve", fn, [in0, scalar, in1], outs)

    def scan(self, out, data0, data1, initial):
        o, a, b = _ap(out), _ap(data0), _ap(data1)
        ini = _ap(initial) if not isinstance(initial, (int, float)) else float(initial)
        self._add("dve", lambda e: e.tensor_tensor_scan(out=o, data0=a, data1=b, initial=ini, op0=ALU.mult, op1=ALU.add),
                  [data0, data1, initial], [out])

    def memset(self, eng, out, val):
        o = _ap(out)
        self._add(eng, lambda e: e.memset(o, val), [], [out])

    def recip(self, out, in_):
        o, i = _ap(out), _ap(in_)
        self._add("dve", lambda e: e.reciprocal(out=o, in_=i), [in_], [out])

    def generic(self, eng, fn, ins, outs):
        self._add(eng, fn, ins, outs)


def host_consts():
    c = {}
    c["ident"] = np.eye(128, dtype=np.float32)
    i = np.arange(128)
    c["stri"] = (i[:, None] < i[None, :]).astype(np.float32)
    c["ones"] = np.ones((128, 128), np.float32)
    c["iota32"] = np.tile(np.arange(32, dtype=np.float32)[None, :], (128, 1))
    c["ebase"] = np.tile((np.arange(32, dtype=np.float32) * CAP)[None, :], (128, 1))
    c["jota"] = np.tile(np.arange(1, 129, dtype=np.float32)[None, :], (128, 1))
    c["tri"] = (i[:, None] <= i[None, :]).astype(np.float32)
    lg_ = np.log(1.0 - np.power(2.0, -5.0 - np.arange(4, dtype=np.float32))).astype(np.float32)
    c["lfc"] = np.tile(lg_[None, :], (128, 1)).astype(np.float32)
    inv = (10000.0 ** (-np.arange(0, 96, 2, dtype=np.float32) / 96.0)).astype(np.float32)
    c["invf"] = np.concatenate([inv, inv])[:, None].astype(np.float32)
    c["sgn"] = np.concatenate([-np.ones(48), np.ones(48)])[:, None].astype(np.float32)
    return c


def s5_layouts(p):
    o = {}
    bblk = np.zeros((128, 6, 2, 2, 128), np.float32)
    cblk = np.zeros((128, 2, 24, 64), np.float32)
    are = np.zeros((128, 24), np.float32)
    aim = np.zeros((128, 24), np.float32)
    ldt = np.zeros((128, 24), np.float32)
    for q in range(24):
        for gi in range(2):
            g = 2 * q + gi
            r0 = 32 * (q % 4) + 16 * gi
            bblk[r0:r0 + 16, q // 4, 0, q % 2, 64 * gi:64 * gi + 64] = p["b_re"][g].T
            bblk[r0:r0 + 16, q // 4, 1, q % 2, 64 * gi:64 * gi + 64] = p["b_im"][g].T
            co = 32 * (q % 2) + 16 * gi
            cblk[64 * gi:64 * gi + 64, 0, q, co:co + 16] = p["c_re"][g].T
            cblk[64 * gi:64 * gi + 64, 1, q, co:co + 16] = p["c_im"][g].T
            are[64 * gi:64 * gi + 64, q] = p["a_re"][g]
            aim[64 * gi:64 * gi + 64, q] = p["a_im"][g]
            ldt[64 * gi:64 * gi + 64, q] = p["log_dt"][g]
    o["bblk"] = bblk
    o["cblk"] = cblk
    o["are"] = are
    o["aim"] = aim
    o["ldt"] = ldt
    o["dsk"] = np.ascontiguousarray(p["d"].reshape(6, 128).T)
    o["bglu"] = np.ascontiguousarray(p["b_glu"].reshape(6, 128).T)
    o["wglu"] = np.ascontiguousarray(p["w_glu"])
    return o


KINDS = [0, 1, 2, 0]
N_IN = [1024, 2568, 2560, 1024]
BT = 256
NBLK = L // BT
TPB = BT // 128


def layer_norm(K, z, out, g, b, stats, mv, rstd, keyed=True):
    zin = [(z[:, 0:512], 0), (z[:, 512:1024], 1)] if keyed else [z[:], z[:]]
    for hh in range(2):
        K.generic("dve", (lambda e, hh=hh: e.bn_stats(out=stats[:, hh, :], in_=z[:, hh * 512:(hh + 1) * 512])),
                  [zin[hh]], [(stats[:, hh, :], hh)])
    K.generic("dve", lambda e: e.bn_aggr(out=mv[:], in_=stats[:, :, :].rearrange("p a b -> p (a b)")),
              [(stats[:, 0, :], 0), (stats[:, 1, :], 1)], [mv[:]])
    K.ts("dve", rstd[:], mv[:, 1:2], EPS, None, ALU.add)
    K.act(rstd[:], rstd[:], AF.Sqrt)
    K.recip(rstd[:], rstd[:])
    K.generic("dve", lambda e: e.tensor_scalar(out=out[:], in0=z[:], scalar1=mv[:, 0:1], scalar2=rstd[:, 0:1],
                                               op0=ALU.subtract, op1=ALU.mult),
              zin + [mv[:], rstd[:]], [out[:]])
    K.tt("pool", out[:], out[:], g[:], ALU.mult)
    K.tt("pool", out[:], out[:], b[:], ALU.add)


class StopBuild(Exception):
    pass


def build(nlayers=DEPTH, dbg=(), stop=None):
    nc = bass.Bass("TRN2", target_bir_lowering=False)
    nc.dge_precook = False
    T = {}

    def din(name, shape, dt=F32):
        T[name] = nc.dram_tensor(name, list(shape), dt, kind="ExternalInput")
        return T[name]

    def dscr(name, shape, dt=F32):
        T[name] = nc.dram_tensor(name, list(shape), dt, kind="Internal")
        return T[name]

    din("x", [L, D]); din("mem", [256, D]); din("pos", [1, L], I32)
    din("mem_w_k", [D, 256], F32R); din("mem_w_v", [D, 256], F32R)
    for k, v in host_consts().items():
        din("c_" + k, v.shape)
    for l in range(nlayers):
        din(f"w_in{l}", [D, N_IN[l]], F32R)
        if KINDS[l] == 0:
            din(f"bblk{l}", [128, 6, 2, 2, 128], F32R); din(f"cblk{l}", [128, 2, 24, 64])
            din(f"are{l}", [128, 24]); din(f"aim{l}", [128, 24]); din(f"ldt{l}", [128, 24])
            din(f"dsk{l}", [128, 6]); din(f"bglu{l}", [128, 6]); din(f"wglu{l}", [768, 768], F32R)
        if KINDS[l] == 1:
            din(f"convq{l}", [96, 4, 4]); din(f"convk{l}", [96, 4, 4]); din(f"bi{l}", [1, 4]); din(f"bf{l}", [1, 4])
        if KINDS[l] in (1, 2):
            din(f"ng{l}", [1, 768])
    din("w_out", [DEPTH, D, D], F32R)
    for n in ("ln1_g", "ln1_b", "ln2_g", "ln2_b"):
        din(n, [DEPTH, D])
    din("router_w", [DEPTH, D, NE]); din("router_b", [DEPTH, NE])
    din("exp_w_gu", [nlayers, NE, D, 2 * D], F32R)
    din("exp_b_gu_l", [nlayers, 128, NE, 16])
    din("exp_w_down", [nlayers, NE, D, D], F32R)
    din("exp_b_down", [nlayers, NE, D], F32R)
    out_t = nc.dram_tensor("out", [L, D], F32, kind="ExternalOutput")
    dscr("H1", [L, D]); dscr("H2", [L, D]); dscr("HT", [D, L], F32R)
    dscr("Xs", [NSLOT, D]); dscr("Ys", [NSLOT, D])
    dbg_t = {n: nc.dram_tensor("dbg_" + n, [L, D], F32, kind="ExternalOutput") for n in dbg}

    with ExitStack() as es:
        K = KB(nc, es)
        sb = K.sb
        ps = [es.enter_context(nc.psum_tensor(f"ps{i}", [128, 512], F32)) for i in range(6)]
        psY = es.enter_context(nc.psum_tensor("psY", [128, 1024], F32))

        def tr8(src, dst_of_hb, engs=("dve", "act")):
            for hb in range(2):
                for c4 in range(4):
                    c = hb * 4 + c4
                    K.tr(ps[hb][:, c4 * 128:(c4 + 1) * 128], src[:, c * 128:(c + 1) * 128], ident[:])
                K.copy(engs[hb], dst_of_hb(hb), ps[hb][:, :].rearrange("p (c t) -> p c t", c=4))

        ident = sb("ident", [128, 128]); stri = sb("stri", [128, 128])
        ones = sb("ones", [128, 128]); iota32 = sb("iota32", [128, 32]); ebase = sb("ebase", [128, 32])
        jota = sb("jota", [128, 128]); tri = sb("tri", [128, 128]); lfc = sb("lfc", [128, 4])
        invf = sb("invf", [96, 1]); sgn = sb("sgn", [96, 1])
        for t_, n_ in ((ident, "ident"), (stri, "stri"), (ones, "ones"), (iota32, "iota32"),
                       (ebase, "ebase"), (jota, "jota"), (tri, "tri"), (lfc, "lfc"), (invf, "invf"), (sgn, "sgn")):
            K.dma(t_[:], T["c_" + n_].ap())
        ones_r = sb("ones_r", [128, 128], F32R)
        K.copy("dve", ones_r[:], ones[:])
        kT = sb("kT", [64, 4, 256], F32R)
        vv = sb("vv", [128, 2, 256], F32R)
        gates_all = sb("gates_all", [128, NT, 4])
        dest_all = sb("dest_all", [128, NT, 4], I32)
        mask_all = sb("mask_all", [128, NT, 32])

        with ExitStack() as es2:
            def sb2(name, shape, dt=F32):
                return es2.enter_context(nc.sbuf_tensor(name, shape, dt))
            zrow = sb2("zrow", [128, 1024])
            K.memset("pool", zrow[:], 0.0)
            for r in range(0, NSLOT, 128):
                K.dma((T["Xs"][r:r + 128, :], r // CAP), zrow[:])
            memT = sb2("memT", [128, 8, 256], F32R)
            wk = sb2("wk", [128, 8, 256], F32R)
            wv = sb2("wv", [128, 8, 256], F32R)
            mt = [sb2(f"mt{i}", [128, 1024]) for i in range(2)]
            K.dma(wk[:], T["mem_w_k"].ap().rearrange("(c p) n -> p c n", p=128))
            K.dma(wv[:], T["mem_w_v"].ap().rearrange("(c p) n -> p c n", p=128))
            for m in range(2):
                K.dma(mt[m][:], T["mem"][m * 128:(m + 1) * 128, :])
                tr8(mt[m], lambda hb, m=m: memT[:, hb * 4:(hb + 1) * 4, m * 128:(m + 1) * 128])
            for h in range(4):
                for c in range(8):
                    K.mm(ps[2][0:64, 0:256], wk[:, c, 64 * h:64 * h + 64], memT[:, c, :], start=(c == 0), stop=(c == 7))
                K.copy("dve", kT[:, h, :], ps[2][0:64, 0:256])
            for m in range(2):
                for c in range(8):
                    K.mm(ps[3][:, 0:256], memT[:, c, m * 128:(m + 1) * 128], wv[:, c, :], start=(c == 0), stop=(c == 7))
                K.copy("act", vv[:, m, :], ps[3][:, 0:256])
            HTv = T["HT"].ap().rearrange("(c p) t -> p c t", p=128)
            xts = [sb2(f"xts{i}", [128, 8, 128], F32R) for i in range(2)]
            for t in range(NT):
                xt = mt[t % 2]
                K.dma(xt[:], T["x"][t * 128:(t + 1) * 128, :])
                tr8(xt, lambda hb, t=t: xts[t % 2][:, hb * 4:(hb + 1) * 4, :])
                K.dma((HTv[:, :, t * 128:(t + 1) * 128], t // TPB), xts[t % 2][:])

        if stop == "setup":
            K.S.stopped = True
        for l in range(nlayers):
            kind = KINDS[l]
            Hres = T["x"] if l == 0 else T["H2"]
            last = (l == nlayers - 1)
            with ExitStack() as esA:
                K.S.barrier()
                def sbA(name, shape, dt=F32):
                    return esA.enter_context(nc.sbuf_tensor(f"{name}_{l}", shape, dt))
                g1 = sbA("g1", [128, 1024]); b1 = sbA("b1", [128, 1024])
                K.dma(g1[:], T["ln1_g"][l:l + 1, :].partition_broadcast(128))
                K.dma(b1[:], T["ln1_b"][l:l + 1, :].partition_broadcast(128))
                rw = sbA("rw", [128, 8, 32]); rb = sbA("rb", [1, 32])
                K.dma(rw[:], T["router_w"][l].rearrange("(c p) n -> p c n", p=128))
                K.dma(rb[:], T["router_b"][l:l + 1, :])
                hTb = sbA("hTb", [128, 8, BT], F32R)
                wp = [sbA(f"wp{i}", [128, 8, 128], F32R) for i in range(3)]
                wpi = [0]
                xqT = sbA("xqT", [64, 4, BT], F32R)
                catT = sbA("catT", [128, 6, BT], F32R)
                Eb = [sbA(f"E{i}", [128, BT], F32R) for i in range(2)]
                rec = sbA("rec", [64, BT])
                hres = [sbA(f"hres{i}", [128, 1024]) for i in range(2)]
                zt = sbA("zt", [128, 1024])
                h1t = [sbA(f"h1t{i}", [128, 1024]) for i in range(2)]
                h1T = sbA("h1T", [128, 8, 128])
                stats = sbA("stats", [128, 2, 6]); mv = sbA("mv", [128, 2]); rstd = sbA("rstd", [128, 1])
                lg = sbA("lg", [128, 32]); m8 = sbA("m8", [128, 8]); i8 = sbA("i8", [128, 8], U32)
                negm = sbA("negm", [128, 1]); e4 = sbA("e4", [128, 4]); ssum = sbA("ssum", [128, 1])
                idxf = sbA("idxf", [128, 4]); ovf = sbA("ovf", [128, 32]); slot = sbA("slot", [128, 32])
                junk = sbA("junk", [128, 32]); destf = sbA("destf", [128, 4])
                w_in = T[f"w_in{l}"].ap().rearrange("(c p) n -> p c n", p=128)
                HTv = T["HT"].ap().rearrange("(c p) t -> p c t", p=128)

                def next_wp():
                    w = wp[wpi[0] % 3]
                    wpi[0] += 1
                    return w

                def proj_fm(col0, ncols, evac):
                    w = next_wp()
                    pb = ps[wpi[0] % 2]
                    K.dma(w[:, :, 0:ncols], w_in[:, :, col0:col0 + ncols])
                    for c in range(8):
                        K.mm(pb[0:ncols, 0:BT], w[:, c, 0:ncols], hTb[:, c, :], start=(c == 0), stop=(c == 7))
                    evac(pb[0:ncols, 0:BT])

                if kind == 0:
                    bblk = sbA("bblk", [128, 6, 2, 2, 128], F32R)
                    K.dma(bblk[:], T[f"bblk{l}"].ap())
                    dsk = sbA("dsk", [128, 6])
                    K.dma(dsk[:], T[f"dsk{l}"].ap())
                    bglu = sbA("bglu", [128, 6])
                    K.dma(bglu[:], T[f"bglu{l}"].ap())
                    CSC = sbA("CSC", [128, 24, 384])
                    COS = CSC[:, :, 0:128]; SIN = CSC[:, :, 128:256]
                    rmag = sbA("rmag", [128, 24]); theta = sbA("theta", [128, 24])
                    Cp = sbA("Cp", [128, 24, 64]); Cin = sbA("Cin", [128, 24, 64])
                    with ExitStack() as esP:
                        def sbP(name, shape):
                            return esP.enter_context(nc.sbuf_tensor(f"{name}_{l}", shape, F32))
                        cblk = sbP("cblk", [128, 2, 24, 64])
                        K.dma(cblk[:], T[f"cblk{l}"].ap())
                        are = sbP("are", [128, 24]); aim = sbP("aim", [128, 24]); ldt = sbP("ldt", [128, 24])
                        K.dma(are[:], T[f"are{l}"].ap()); K.dma(aim[:], T[f"aim{l}"].ap()); K.dma(ldt[:], T[f"ldt{l}"].ap())
                        lam = sbP("lam", [128, 24]); dtt = sbP("dtt", [128, 24]); tmp = sbP("tmp", [128, 24])
                        tmp2 = sbP("tmp2", [128, 24]); sn = sbP("sn", [128, 24]); cs = sbP("cs", [128, 24])
                        abr = sbP("abr", [128, 24]); abi = sbP("abi", [128, 24]); den = sbP("den", [128, 24])
                        cre = sbP("cre", [128, 24]); cim = sbP("cim", [128, 24])
                        ANG = sbP("ANG", [128, 24, 128]); KK = sbP("KK", [128, 24, 128])
                        t32a = sbP("t32a", [128, 24, 64]); t32b = sbP("t32b", [128, 24, 64])

                        def range_sin(out, ang, kk, shift):
                            K.ts("dve", kk, ang, 1.0 / TWO_PI, shift / TWO_PI, ALU.mult, ALU.add)
                            K.ts("dve", kk, kk, MAGIC, MAGIC, ALU.add, ALU.subtract)
                            K.stt(out, kk, -C1, ang, ALU.mult, ALU.add)
                            K.stt(out, kk, -C2, out, ALU.mult, ALU.add)
                            K.ts("dve", out, out, shift, None, ALU.add)
                            K.ts("dve", out, out, -PI_LO, PI_LO, ALU.max, ALU.min)
                            K.act(out, out, AF.Sin)

                        K.ts("dve", lam[:], are[:], -1e-4, None, ALU.min)
                        K.act(dtt[:], ldt[:], AF.Exp)
                        K.tt("dve", tmp[:], dtt[:], lam[:], ALU.mult)
                        K.act(rmag[:], tmp[:], AF.Exp)
                        K.tt("dve", theta[:], dtt[:], aim[:], ALU.mult)
                        range_sin(sn[:], theta[:], tmp[:], 0.0)
                        range_sin(cs[:], theta[:], tmp[:], math.pi / 2)
                        K.tt("dve", abr[:], rmag[:], cs[:], ALU.mult)
                        K.tt("dve", abi[:], rmag[:], sn[:], ALU.mult)
                        K.ts("dve", abr[:], abr[:], -1.0, None, ALU.add)
                        K.tt("dve", den[:], lam[:], lam[:], ALU.mult)
                        K.tt("dve", tmp[:], aim[:], aim[:], ALU.mult)
                        K.tt("dve", den[:], den[:], tmp[:], ALU.add)
                        K.recip(den[:], den[:])
                        K.tt("dve", tmp[:], abr[:], lam[:], ALU.mult)
                        K.tt("dve", tmp2[:], abi[:], aim[:], ALU.mult)
                        K.tt("dve", tmp[:], tmp[:], tmp2[:], ALU.add)
                        K.tt("dve", cre[:], tmp[:], den[:], ALU.mult)
                        K.tt("dve", tmp[:], abi[:], lam[:], ALU.mult)
                        K.tt("dve", tmp2[:], abr[:], aim[:], ALU.mult)
                        K.tt("dve", tmp[:], tmp[:], tmp2[:], ALU.subtract)
                        K.tt("dve", cim[:], tmp[:], den[:], ALU.mult)
                        creb = cre[:].unsqueeze(2).to_broadcast([128, 24, 64])
                        cimb = cim[:].unsqueeze(2).to_broadcast([128, 24, 64])
                        K.tt("dve", t32a[:], cblk[:, 0, :, :], creb, ALU.mult)
                        K.tt("dve", t32b[:], cblk[:, 1, :, :], cimb, ALU.mult)
                        K.tt("dve", Cp[:], t32a[:], t32b[:], ALU.subtract)
                        K.tt("dve", t32a[:], cblk[:, 0, :, :], cimb, ALU.mult)
                        K.tt("dve", t32b[:], cblk[:, 1, :, :], creb, ALU.mult)
                        K.tt("dve", t32a[:], t32a[:], t32b[:], ALU.add)
                        K.ts("dve", Cin[:], t32a[:], -1.0, None, ALU.mult)
                        for q in range(24):
                            K.ts("dve", ANG[:, q, :], jota[:], theta[:, q:q + 1], None, ALU.mult)
                        range_sin(SIN, ANG[:, :, :], KK[:, :, :], 0.0)
                        range_sin(COS, ANG[:, :, :], KK[:, :, :], math.pi / 2)
                        K.copy("dve", CSC[:, :, 256:384], COS)

                    K.S.barrier()
                    if stop == f"P{l}":
                        K.S.stopped = True
                    uT = catT
                    ygT = sbA("ygT", [128, 6, BT], F32R)
                    ygT_f = ygT.bitcast(F32)
                    car_re = sbA("car_re", [128, 24]); car_im = sbA("car_im", [128, 24])
                    K.memset("pool", car_re[:], 0.0); K.memset("pool", car_im[:], 0.0)
                    NBUF = 4
                    mk = lambda n, w_: [sbA(f"{n}_{i}", [128, w_]) for i in range(NBUF)]
                    T12, T43, RR, WW, VAC, VBD, XX = [mk(n, 256) for n in ("T12", "T43", "RR", "WW", "VAC", "VBD", "XX")]
                    ytok = sbA("ytok", [128, 768]); yx2 = sbA("yx2", [128, 768])
                    sgl = [sbA(f"sgl{i}", [128, BT]) for i in range(2)]
                    wgluv = T[f"wglu{l}"].ap().rearrange("(c p) n -> p c n", p=128)


                if kind in (1, 2):
                    LNS = math.log(96.0 ** -0.5)
                    qT = sbA("qT", [96, 4, BT], F32R); kTt = sbA("kTt", [96, 4, BT], F32R)
                    qT_f = qT.bitcast(F32); kTt_f = kTt.bitcast(F32)
                    vt = [sbA(f"vt{i}", [128, 4, 194], F32R) for i in range(2)]
                    for v_ in vt:
                        K.memset("pool", v_.bitcast(F32)[:], 0.0)
                        K.copy("dve", v_[:, :, 192:193], ones[:, 0:4].unsqueeze(2))
                    clns = sbA("clns", [128, 1])
                    K.memset("pool", clns[:], LNS)
                    gt = [sbA(f"gt{i}", [128, 768]) for i in range(2)]
                    gps = sbA("gps", [128, 8]); lf = sbA("lf", [128, 4]); igb = sbA("igb", [128, 4]); bcol = sbA("bcol", [128, 4])
                    bias1 = sbA("bias1", [128, 4]); bias2 = sbA("bias2", [128, 4])
                    rhsB = [sbA(f"rhsB{i}", [128, 128]) for i in range(2)]
                    DT = [sbA(f"DT{i}", [128, 128]) for i in range(2)]
                    SD = [sbA(f"SD{i}", [128, 128], F32R) for i in range(2)]
                    EBt = [sbA(f"EBt{i}", [96, 128]) for i in range(2)]
                    qs = [sbA(f"qs{i}", [96, 128], F32R) for i in range(2)]
                    kw = [sbA(f"kw{i}", [128, 96], F32R) for i in range(2)]
                    wvv = [sbA(f"wvv{i}", [128, 1]) for i in range(2)]
                    ebt = [sbA(f"ebt{i}", [96, 1]) for i in range(2)]
                    Cst = [sbA(f"Cst{i}", [96, 4, 194], F32R) for i in range(2)]
                    Cst_f = [c_.bitcast(F32) for c_ in Cst]
                    K.memset("pool", Cst_f[0][:], 0.0); K.memset("pool", Cst_f[1][:], 0.0)
                    hN = sbA("hN", [128, 192]); dn = sbA("dn", [128, 1]); hst = sbA("hst", [128, 6]); hmv = sbA("hmv", [128, 2])
                    hrs = sbA("hrs", [128, 1])
                    ymx = sbA("ymx", [128, 768]); gsg = sbA("gsg", [128, 768])
                    ngb = sbA("ngb", [128, 768])
                    K.dma(ngb[:], T[f"ng{l}"].ap().partition_broadcast(128))
                    if kind == 1:
                        qpre = sbA("qpre", [96, 4, 3 + BT]); kpre = sbA("kpre", [96, 4, 3 + BT])
                        K.memset("pool", qpre[:], 0.0); K.memset("pool", kpre[:], 0.0)
                        cacc = sbA("cacc", [96, BT])
                        convq = sbA("convq", [96, 4, 4]); convk = sbA("convk", [96, 4, 4])
                        K.dma(convq[:], T[f"convq{l}"].ap()); K.dma(convk[:], T[f"convk{l}"].ap())
                        bib = sbA("bib", [128, 4]); bfb = sbA("bfb", [128, 4])
                        K.dma(bib[:], T[f"bi{l}"].ap().partition_broadcast(128))
                        K.dma(bfb[:], T[f"bf{l}"].ap().partition_broadcast(128))
                    else:
                        RC = sbA("RC", [96, L]); RS = sbA("RS", [96, L])
                        qraw = sbA("qraw", [96, BT]); qsw = sbA("qsw", [96, BT])
                        with ExitStack() as esR:
                            posb = esR.enter_context(nc.sbuf_tensor(f"posb_{l}", [96, L], I32))
                            posf = esR.enter_context(nc.sbuf_tensor(f"posf_{l}", [96, L], F32))
                            kkr = esR.enter_context(nc.sbuf_tensor(f"kkr_{l}", [96, L], F32))
                            K.dma(posb[:], T["pos"].ap().partition_broadcast(96))
                            K.copy("dve", posf[:], posb[:])
                            K.ts("dve", posf[:], posf[:], invf[:, 0:1], None, ALU.mult)

                            def range_sin2(out, ang, kk, shift):
                                K.ts("dve", kk, ang, 1.0 / TWO_PI, shift / TWO_PI, ALU.mult, ALU.add)
                                K.ts("dve", kk, kk, MAGIC, MAGIC, ALU.add, ALU.subtract)
                                K.stt(out, kk, -C1, ang, ALU.mult, ALU.add)
                                K.stt(out, kk, -C2, out, ALU.mult, ALU.add)
                                K.ts("dve", out, out, shift, None, ALU.add)
                                K.ts("dve", out, out, -PI_LO, PI_LO, ALU.max, ALU.min)
                                K.act(out, out, AF.Sin)
                            range_sin2(RS[:], posf[:], kkr[:], 0.0)
                            range_sin2(RC[:], posf[:], kkr[:], math.pi / 2)
                            K.ts("dve", RS[:], RS[:], sgn[:, 0:1], None, ALU.mult)
                        K.S.barrier()

                for tb in range(NBLK):
                    tsl = slice(tb * BT, (tb + 1) * BT)
                    K.dma(hTb[:], HTv[:, :, tsl])
                    xq0 = N_IN[l] - 256
                    for h in range(4):
                        proj_fm(xq0 + 64 * h, 64, lambda p, h=h: K.copy("act", xqT[:, h, :], p))
                    if stop == "J0":
                        K.S.stopped = True
                    if kind == 0:
                        for c in range(6):
                            proj_fm(128 * c, 128, lambda p, c=c: K.copy("dve" if c % 2 else "act", uT[:, c, :], p))
                        for s in range(TPB):
                            ssl = slice(s * 128, (s + 1) * 128)

                            banks = [ps[2], ps[5], ps[3], ps[4]]

                            def bu(q):
                                c, r0 = q // 4, 64 * ((q % 4) // 2)
                                pq = banks[q % 4]
                                K.mm(pq[:, 0:128], bblk[r0:r0 + 64, c, 0, q % 2, :], uT[r0:r0 + 64, c, ssl])
                                K.mm(pq[:, 128:256], bblk[r0:r0 + 64, c, 1, q % 2, :], uT[r0:r0 + 64, c, ssl])
                            for q in range(4):
                                bu(q)
                            for g in range(6):
                                qs_ = range(4 * g, 4 * g + 4)
                                c = g
                                for q in qs_:
                                    K.tt("dve", T12[q % 4][:], banks[q % 4][:, 0:256], CSC[:, q, 0:256], ALU.mult)
                                for q in qs_:
                                    K.tt("dve", T43[q % 4][:], banks[q % 4][:, 0:256], CSC[:, q, 128:384], ALU.mult)
                                if g + 1 < 6:
                                    for q in range(4 * g + 4, 4 * g + 8):
                                        bu(q)
                                for q in qs_:
                                    b = q % 4
                                    K.tt("dve", RR[b][:, 0:128], T12[b][:, 0:128], T12[b][:, 128:256], ALU.add)
                                for q in qs_:
                                    b = q % 4
                                    K.tt("dve", RR[b][:, 128:256], T43[b][:, 128:256], T43[b][:, 0:128], ALU.subtract)
                                for q in qs_:
                                    b = q % 4
                                    K.scan(WW[b][:, 0:128], rmag[:, q:q + 1].to_broadcast([128, 128]), RR[b][:, 0:128], (car_re[:, q:q + 1], q))
                                for q in qs_:
                                    b = q % 4
                                    K.scan(WW[b][:, 128:256], rmag[:, q:q + 1].to_broadcast([128, 128]), RR[b][:, 128:256], (car_im[:, q:q + 1], q))
                                for q in qs_:
                                    b = q % 4
                                    K.tt("dve", VAC[b][:].rearrange("p (a b) -> p a b", a=2),
                                         WW[b][:, 0:128].unsqueeze(1).to_broadcast([128, 2, 128]),
                                         CSC[:, q, 0:256].rearrange("p (a b) -> p a b", a=2), ALU.mult)
                                for q in qs_:
                                    b = q % 4
                                    K.tt("dve", VBD[b][:].rearrange("p (a b) -> p a b", a=2),
                                         WW[b][:, 128:256].unsqueeze(1).to_broadcast([128, 2, 128]),
                                         CSC[:, q, 128:384].rearrange("p (a b) -> p a b", a=2), ALU.mult)
                                for q in qs_:
                                    b = q % 4
                                    K.tt("dve", XX[b][:, 0:128], VAC[b][:, 0:128], VBD[b][:, 0:128], ALU.subtract)
                                for q in qs_:
                                    b = q % 4
                                    K.tt("dve", XX[b][:, 128:256], VAC[b][:, 128:256], VBD[b][:, 128:256], ALU.add)
                                for q in qs_:
                                    b = q % 4
                                    K.copy("pool", (car_re[:, q:q + 1], q), XX[b][:, 127:128])
                                    K.copy("pool", (car_im[:, q:q + 1], q), XX[b][:, 255:256])
                                for q in qs_:
                                    b = q % 4
                                    hq = (q % 4) // 2
                                    yo = (psY[64 * hq:64 * hq + 64, 128 * c:128 * c + 128], c // 4)
                                    K.mm(yo, Cp[:, q, :], XX[b][:, 0:128], start=(q % 2 == 0), stop=False, sgc=True)
                                    K.mm(yo, Cin[:, q, :], XX[b][:, 128:256], start=False, stop=(q % 2 == 1), sgc=True)
                            uT_f = uT.bitcast(F32)
                            for c in range(6):
                                K.stt((ytok[:, 128 * c:128 * c + 128], c), uT_f[:, c, ssl], dsk[:, c:c + 1],
                                      (psY[:, 128 * c:128 * c + 128], c // 4), ALU.mult, ALU.add)
                            K.act(yx2[:], ytok[:], AF.Square)
                            K.ts("dve", yx2[:], yx2[:], 0.044715, 1.0, ALU.mult, ALU.add)
                            K.tt("dve", yx2[:], yx2[:], ytok[:], ALU.mult)
                            K.act(yx2[:], yx2[:], AF.Sigmoid, scale=1.5957691216057308)
                            K.tt("dve", ygT[:, :, ssl], ytok[:].rearrange("p (c t) -> p c t", c=6),
                                 yx2[:].rearrange("p (c t) -> p c t", c=6), ALU.mult)
                        if stop == "S0":
                            K.S.stopped = True
                        for j in range(6):
                            w = next_wp()
                            pb = ps[j % 2]
                            K.dma(w[:, 0:6, :], wgluv[:, :, 128 * j:128 * j + 128])
                            for c in range(6):
                                K.mm(pb[:, 0:BT], w[:, c, :], ygT[:, c, :], start=(c == 0), stop=(c == 5))
                            K.act(sgl[j % 2][:], pb[:, 0:BT], AF.Sigmoid, bias=bglu[:, j:j + 1])
                            K.tt("dve", catT[:, j, :], ygT_f[:, j, :], sgl[j % 2][:], ALU.mult)
                    else:
                        for h in range(4):
                            if kind == 1:
                                proj_fm(96 * h, 96, lambda p, h=h: K.copy("act", qpre[:, h, 3:3 + BT], p))
                                proj_fm(384 + 96 * h, 96, lambda p, h=h: K.copy("act", kpre[:, h, 3:3 + BT], p))
                                for (pre, cw, dstT) in ((qpre, convq, qT), (kpre, convk, kTt)):
                                    K.ts("dve", cacc[:], pre[:, h, 3:3 + BT], cw[:, h, 3:4], None, ALU.mult)
                                    for w_ in (2, 1, 0):
                                        K.stt(cacc[:], pre[:, h, w_:w_ + BT], cw[:, h, w_:w_ + 1], cacc[:], ALU.mult, ALU.add)
                                    K.act(dstT[:, h, :], cacc[:], AF.Silu)
                            else:
                                for (c0_, dstT, dst_f) in ((0, qT, qT_f), (384, kTt, kTt_f)):
                                    proj_fm(c0_ + 96 * h, 96, lambda p: K.copy("act", qraw[:], p))
                                    w = next_wp()
                                    pb = ps[wpi[0] % 2]
                                    K.dma(w[:, :, 0:48], w_in[:, :, c0_ + 96 * h + 48:c0_ + 96 * h + 96])
                                    K.dma(w[:, :, 48:96], w_in[:, :, c0_ + 96 * h:c0_ + 96 * h + 48])
                                    for c in range(8):
                                        K.mm(pb[0:96, 0:BT], w[:, c, 0:96], hTb[:, c, :], start=(c == 0), stop=(c == 7))
                                    K.tt("dve", qsw[:], pb[0:96, 0:BT], RS[:, tsl], ALU.mult)
                                    K.tt("pool", qraw[:], qraw[:], RC[:, tsl], ALU.mult)
                                    K.tt("dve", dstT[:, h, :], qraw[:], qsw[:], ALU.add)
                        if kind == 1:
                            K.copy("pool", qpre[:, :, 0:3], qpre[:, :, BT:BT + 3])
                            K.copy("pool", kpre[:, :, 0:3], kpre[:, :, BT:BT + 3])
                        for pc in range(12):
                            w = next_wp()
                            K.dma(w[:], w_in[:, :, 768 + 128 * pc:768 + 128 * pc + 128])
                            for j in range(TPB):
                                pb = ps[(pc * TPB + j) % 2]
                                for c in range(8):
                                    K.mm(pb[:, 0:128], hTb[:, c, j * 128:(j + 1) * 128], w[:, c, :], start=(c == 0), stop=(c == 7))
                                if pc < 6:
                                    n0 = 128 * pc
                                    while n0 < 128 * pc + 128:
                                        hh_ = n0 // 192
                                        n1 = min(128 * pc + 128, 192 * (hh_ + 1))
                                        K.copy("act" if (n0 // 64) % 2 else "dve", vt[j][:, hh_, n0 - 192 * hh_:n1 - 192 * hh_],
                                               pb[:, n0 - 128 * pc:n1 - 128 * pc])
                                        n0 = n1
                                else:
                                    K.copy("act", gt[j][:, 128 * (pc - 6):128 * (pc - 6) + 128], pb[:, 0:128])
                        if kind == 1:
                            wg_ = next_wp()
                            K.dma(wg_[:, :, 0:8], w_in[:, :, 2304:2312])
                        for j in range(TPB):
                            csl = slice(j * 128, (j + 1) * 128)
                            if kind == 1:
                                for c in range(8):
                                    K.mm(ps[2][:, 0:8], hTb[:, c, csl], wg_[:, c, 0:8], start=(c == 0), stop=(c == 7))
                                K.copy("dve", gps[:], ps[2][:, 0:8])
                                K.tt("dve", igb[:], gps[:, 0:4], bib[:], ALU.add)
                                K.tt("dve", lf[:], gps[:, 4:8], bfb[:], ALU.add)
                                K.act(lf[:], lf[:], AF.Exp, scale=-1.0)
                                K.act(lf[:], lf[:], AF.Ln, bias=ones[:, 0:1])
                                K.ts("dve", lf[:], lf[:], -1.0, None, ALU.mult)
                                lfx = lf
                            else:
                                lfx = lfc
                            K.mm(ps[2][:, 0:4], tri[:], lfx[:], start=True, stop=True)
                            if kind == 1:
                                K.tt("dve", bias2[:], igb[:], ps[2][:, 0:4], ALU.subtract)
                                K.ts("dve", bias1[:], bias2[:], LNS, None, ALU.add)
                            else:
                                K.ts("dve", bias1[:], ps[2][:, 0:4], -1.0, LNS, ALU.mult, ALU.add)
                                K.copy("dve", bias2[:], bias1[:])
                            for h in range(4):
                                b = h % 2
                                cur = Cst[(tb * TPB + j) % 2]; nxt = Cst[(tb * TPB + j + 1) % 2]
                                cur_f = Cst_f[(tb * TPB + j) % 2]
                                K.ts("dve", rhsB[b][:], tri[:], lfx[:, h:h + 1], None, ALU.mult)
                                K.mm(ps[3][:, 0:128], ones[:], rhsB[b][:])
                                K.act(DT[b][:], ps[3][:, 0:128], AF.Exp, bias=bias1[:, h:h + 1])
                                K.tt("pool", DT[b][:], DT[b][:], tri[:], ALU.mult)
                                K.act(EBt[b][:], ps[3][0:96, 0:128], AF.Exp, bias=(clns[0:96, 0:1] if kind == 1 else 0.0))
                                K.tt("dve", qs[b][:], qT_f[:, h, csl], EBt[b][:], ALU.mult)
                                K.act(wvv[b][:], ps[3][:, 127:128], AF.Exp, bias=bias2[:, h:h + 1])
                                K.act(ebt[b][:], ps[3][0:96, 127:128], AF.Exp)
                                K.tr(ps[4][:, 0:96], kTt_f[:, h, csl], ident[0:96, 0:96])
                                K.ts("dve", kw[b][:], ps[4][:, 0:96], wvv[b][:, 0:1], None, ALU.mult)
                                K.mm(ps[5][:, 0:128], kTt[:, h, csl], qT[:, h, csl])
                                K.tt("dve", SD[b][:], ps[5][:, 0:128], DT[b][:], ALU.mult)
                                K.mm((psY[:, 0:194], 0), SD[b][:], vt[j][:, h, :], start=True, stop=False)
                                K.mm((psY[:, 0:194], 0), qs[b][:], (cur[:, h, :], h), start=False, stop=True)
                                K.mm((psY[0:96, 512:706], 1), kw[b][:], vt[j][:, h, :], start=True, stop=True)
                                K.stt((nxt[:, h, :], h), (cur_f[:, h, :], h), ebt[b][:, 0:1], (psY[0:96, 512:706], 1), ALU.mult, ALU.add)
                                if kind == 1:
                                    K.act(dn[:], (psY[:, 192:193], 0), AF.Abs)
                                    K.ts("dve", dn[:], dn[:], 1.0, None, ALU.max)
                                    K.recip(dn[:], dn[:])
                                    K.ts("dve", hN[:], (psY[:, 0:192], 0), dn[:, 0:1], None, ALU.mult)
                                else:
                                    K.copy("dve", hN[:], (psY[:, 0:192], 0))
                                K.generic("dve", lambda e, hst=hst, hN=hN: e.bn_stats(out=hst[:], in_=hN[:]), [hN[:]], [hst[:]])
                                K.generic("dve", lambda e, hst=hst, hmv=hmv: e.bn_aggr(out=hmv[:], in_=hst[:]), [hst[:]], [hmv[:]])
                                K.ts("dve", hrs[:], hmv[:, 1:2], EPS, None, ALU.add)
                                K.act(hrs[:], hrs[:], AF.Sqrt)
                                K.recip(hrs[:], hrs[:])
                                K.ts("dve", (ymx[:, 192 * h:192 * h + 192], h), hN[:], hmv[:, 0:1], hrs[:, 0:1], ALU.subtract, ALU.mult)
                            K.tt("pool", ymx[:], ymx[:], ngb[:], ALU.mult)
                            K.act(gsg[:], gt[j][:], AF.Sigmoid if kind == 1 else AF.Silu)
                            K.tt("dve", ymx[:], ymx[:], gsg[:], ALU.mult)
                            for c in range(6):
                                pb = ps[c // 4]
                                K.tr(pb[:, (c % 4) * 128:(c % 4) * 128 + 128], ymx[:, 128 * c:128 * c + 128], ident[:])
                            K.copy("act", catT[:, 0:4, csl], ps[0][:, :].rearrange("p (c t) -> p c t", c=4))
                            K.copy("dve", catT[:, 4:6, csl], ps[1][:, 0:256].rearrange("p (c t) -> p c t", c=2))

                    if stop == "G0":
                        K.S.stopped = True
                    for h in range(4):
                        for m in range(2):
                            K.mm(ps[3 + m][:, 0:BT], kT[:, h, m * 128:(m + 1) * 128], xqT[:, h, :])
                            K.act(Eb[m][:], ps[3 + m][:, 0:BT], AF.Exp, scale=0.125)
                        for m in range(2):
                            K.mm(ps[5][0:64, 0:BT], vv[:, m, 64 * h:64 * h + 64], Eb[m][:], start=(m == 0), stop=(m == 1))
                        for m in range(2):
                            K.mm(ps[2][0:64, 0:BT], ones_r[:, 0:64], Eb[m][:], start=(m == 0), stop=(m == 1))
                        K.recip(rec[:], ps[2][0:64, 0:BT])
                        K.tt("dve", xqT[:, h, :], ps[5][0:64, 0:BT], rec[:], ALU.mult)
                    ymT = xqT

                    if stop == "X0":
                        K.S.stopped = True
                    accs = [(psY[:, 0:512], 0), (psY[:, 512:1024], 1), ps[3][:, :], ps[4][:, :]]
                    for c in range(10):
                        w = next_wp()
                        wv_ = w[:, :, :].rearrange("p a b -> p (a b)")
                        if c < 6:
                            K.dma(w[:], T["w_out"][l, 128 * c:128 * c + 128, :].rearrange("p (a b) -> p a b", b=128))
                        else:
                            hq = c - 6
                            K.generic_dma = None
                            K.dma((w[0:64, :, :], None), T["w_out"][l, 768 + 64 * hq:768 + 64 * hq + 64, :].rearrange("p (a b) -> p a b", b=128))
                        for j in range(TPB):
                            jsl = slice(j * 128, (j + 1) * 128)
                            for hh in range(2):
                                a = accs[j * 2 + hh]
                                if c < 6:
                                    K.mm(a, catT[:, c, jsl], wv_[:, hh * 512:(hh + 1) * 512], start=(c == 0), stop=False)
                                else:
                                    K.mm(a, ymT[:, c - 6, jsl], wv_[0:64, hh * 512:(hh + 1) * 512], start=False, stop=(c == 9))
                    if stop == "O0":
                        K.S.stopped = True
                    for j in range(TPB):
                        ti = tb * TPB + j
                        hr = hres[ti % 2]; z = zt; h1 = h1t[ti % 2]
                        K.dma(hr[:], (Hres[ti * 128:(ti + 1) * 128, :], ti))
                        for hh in range(2):
                            K.stt((z[:, hh * 512:(hh + 1) * 512], hh), hr[:, hh * 512:(hh + 1) * 512], ALPHA, accs[j * 2 + hh], ALU.mult, ALU.add)
                        layer_norm(K, z, h1, g1, b1, stats, mv, rstd)
                        K.dma((T["H1"][ti * 128:(ti + 1) * 128, :], ti), h1[:], q="act")
                        if stop == "L0":
                            K.S.stopped = True
                        tr8(h1, lambda hb: h1T[:, hb * 4:(hb + 1) * 4, :])
                        for c in range(8):
                            K.mm(ps[5][:, 0:32], h1T[:, c, :], rw[:, c, :], start=(c == 0), stop=False)
                        K.mm(ps[5][:, 0:32], ones[0:1, :], rb[0:1, :], start=False, stop=True)
                        K.copy("dve", lg[:], ps[5][:, 0:32])
                        K.generic("dve", lambda e, m8=m8, lg=lg: e.max(out=m8[:], in_=lg[:]), [lg[:]], [m8[:]])
                        K.generic("dve", lambda e, m8=m8, lg=lg, i8=i8: e.max_index(out=i8[:], in_max=m8[:], in_values=lg[:]), [lg[:], m8[:]], [i8[:]])
                        K.ts("dve", negm[:], m8[:, 0:1], -1.0, None, ALU.mult)
                        K.act(e4[:], m8[:, 0:4], AF.Exp, bias=negm[:], accum_out=ssum[:])
                        K.recip(ssum[:], ssum[:])
                        K.ts("dve", (gates_all[:, ti, :], ti), e4[:], ssum[:], None, ALU.mult)
                        K.copy("dve", idxf[:], i8[:, 0:4])
                        K.ts("dve", (mask_all[:, ti, :], ti), lg[:], m8[:, 3:4], None, ALU.is_ge)
                        for t2_ in range(ti):
                            K.mm(ps[2][:, 0:32], ones[:], (mask_all[:, t2_, :], t2_), start=(t2_ == 0), stop=False)
                        K.mm(ps[2][:, 0:32], stri[:], (mask_all[:, ti, :], ti), start=(ti == 0), stop=True)
                        K.ts("dve", ovf[:], ps[2][:, 0:32], float(CAP), 1.0e6, ALU.is_ge, ALU.mult)
                        K.tt("dve", slot[:], ps[2][:, 0:32], ebase[:], ALU.add)
                        K.tt("dve", slot[:], slot[:], ovf[:], ALU.add)
                        for k in range(4):
                            K.stt(junk[:], iota32[:], idxf[:, k:k + 1], slot[:], ALU.is_equal, ALU.mult, accum_out=(destf[:, k:k + 1], k))
                        K.generic("dve", lambda e, ti=ti, destf=destf: e.tensor_copy(out=dest_all[:, ti, :], in_=destf[:]),
                                  [(destf[:, k:k + 1], k) for k in range(4)], [(dest_all[:, ti, :], ti)])
                        if stop == "R0":
                            K.S.stopped = True
                        for k in range(4):
                            def sc(e, ti=ti, k=k, h1=h1):
                                return e.indirect_dma_start(
                                    out=T["Xs"].ap(), out_offset=bass.IndirectOffsetOnAxis(ap=dest_all[:, ti, k:k + 1], axis=0),
                                    in_=h1[:], in_offset=None, bounds_check=REG["bc"], oob_is_err=False)
                            K.S.add("pool", sc, [_rk(h1[:]), _rk((dest_all[:, ti, :], ti))], [],
                                    [("Xs", e_) for e_ in range(NE)], dma=True)
            if stop == f"A{l}":
                K.S.stopped = True

            with ExitStack() as esB:
                K.S.barrier()
                def sbB(name, shape, dt=F32):
                    return esB.enter_context(nc.sbuf_tensor(f"{name}_{l}", shape, dt))
                XT = [sbB(f"XT{i}", [128, 8, CAP], F32R) for i in range(2)]
                xs = [sbB(f"xs{i}", [128, 1024]) for i in range(2)]
                NWG = 6
                wg = [sbB(f"wg{i}", [128, 8, 256], F32R) for i in range(NWG)]
                NWD = 10
                wd = [sbB(f"wd{i}", [128, 1024], F32R) for i in range(NWD)]
                bd = [sbB(f"bd{i}", [1, 1024], F32R) for i in range(2)]
                actT = sbB("actT", [128, 8, CAP], F32R)
                bgu = sbB("bgu", [128, NE, 16])
                K.dma(bgu[:], T["exp_b_gu_l"][l])
                K.ts("dve", bgu[:, :, 8:16], bgu[:, :, 8:16], 1.0, None, ALU.add)
                gg, sg, ll = [[sbB(f"{n}{i}", [128, CAP]) for i in range(2)] for n in ("gg", "sg", "ll")]
                yt = [sbB(f"yt{i}", [128, 1024]) for i in range(2)]
                xi = 0; gi_ = 0; yi = 0; wdi = 0
                def load_xt(e2):
                    nonlocal_xi = xi_box
                    X2 = XT[e2 % 2]
                    for t in range(3):
                        x_ = xs[nonlocal_xi[0] % 2]; nonlocal_xi[0] += 1
                        r0 = e2 * CAP + t * 128
                        K.dma(x_[:], (T["Xs"][r0:r0 + 128, :], e2))
                        tr8(x_, lambda hb, X2=X2, t=t: X2[:, hb * 4:(hb + 1) * 4, t * 128:(t + 1) * 128])
                xi_box = [0]
                load_xt(0)
                for e_ in range(NE):
                    X = XT[e_ % 2]
                    wgu = T["exp_w_gu"][l, e_].rearrange("(c p) n -> p c n", p=128)
                    for jj in range(4):
                        wG = wg[gi_ % NWG]; gi_ += 1
                        wL = wg[gi_ % NWG]; gi_ += 1
                        K.dma(wG[:], wgu[:, :, 256 * jj:256 * jj + 256])
                        K.dma(wL[:], wgu[:, :, 1024 + 256 * jj:1024 + 256 * jj + 256])
                        for j2 in range(2):
                            j = 2 * jj + j2
                            pG = ps[2 + 2 * j2]; pL = ps[3 + 2 * j2]
                            for c in range(8):
                                K.mm(pG[:, 0:CAP], wG[:, c, 128 * j2:128 * j2 + 128], X[:, c, :], start=(c == 0), stop=(c == 7))
                            for c in range(8):
                                K.mm(pL[:, 0:CAP], wL[:, c, 128 * j2:128 * j2 + 128], X[:, c, :], start=(c == 0), stop=(c == 7))
                            b = j2
                            K.ts("dve", gg[b][:], pG[:, 0:CAP], bgu[:, e_, j:j + 1], 7.0, ALU.add, ALU.min)
                            K.act(sg[b][:], gg[b][:], AF.Silu, scale=1.702)
                            K.act(ll[b][:], pL[:, 0:CAP], AF.Identity, bias=bgu[:, e_, 8 + j:9 + j])
                            K.ts("dve", ll[b][:], ll[b][:], 8.0, -6.0, ALU.min, ALU.max)
                            K.stt((actT[:, j, :], j), sg[b][:], 1.0 / 1.702, ll[b][:], ALU.mult, ALU.mult)
                    wds = []
                    for j in range(8):
                        w = wd[wdi % NWD]; wdi += 1
                        wds.append(w)
                        K.dma(w[:], T["exp_w_down"][l, e_, 128 * j:128 * j + 128, :])
                    K.dma(bd[e_ % 2][:], T["exp_b_down"][l, e_:e_ + 1, :])
                    if e_ + 1 < NE:
                        load_xt(e_ + 1)
                    for t in range(3):
                        y_ = yt[yi % 2]; yi += 1
                        for hh in range(2):
                            pb = (psY[:, hh * 512:(hh + 1) * 512], hh)
                            for j in range(8):
                                K.mm(pb, (actT[:, j, t * 128:(t + 1) * 128], j), wds[j][:, hh * 512:(hh + 1) * 512],
                                     start=(j == 0), stop=False)
                            K.mm(pb, ones_r[0:1, :], bd[e_ % 2][0:1, hh * 512:(hh + 1) * 512], start=False, stop=True)
                            K.copy("act" if hh else "dve", (y_[:, hh * 512:(hh + 1) * 512], hh), pb)
                        r0 = e_ * CAP + t * 128
                        K.S.add("act", (lambda e, r0=r0, y_=y_: e.dma_start(out=T["Ys"][r0:r0 + 128, :], in_=y_[:])),
                                [_rk(y_[:])], [], [("Ys", None)], dma=True)
            if stop == f"B{l}":
                K.S.stopped = True

            with ExitStack() as esC:
                K.S.barrier()
                def sbC(name, shape, dt=F32):
                    return esC.enter_context(nc.sbuf_tensor(f"{name}_{l}", shape, dt))
                g2 = sbC("g2", [128, 1024]); b2 = sbC("b2", [128, 1024])
                K.dma(g2[:], T["ln2_g"][l:l + 1, :].partition_broadcast(128))
                K.dma(b2[:], T["ln2_b"][l:l + 1, :].partition_broadcast(128))
                yg = [sbC(f"yg{i}", [128, 1024]) for i in range(8)]
                for y_ in yg:
                    K.memset("pool", y_[:], 0.0)
                hr2 = [sbC(f"hr2{i}", [128, 1024]) for i in range(2)]
                macc = [sbC(f"macc{i}", [128, 1024]) for i in range(2)]
                zt2 = [sbC(f"zt2{i}", [128, 1024]) for i in range(2)]
                h2t = [sbC(f"h2t{i}", [128, 1024]) for i in range(2)]
                hT2 = [sbC(f"hT2{i}", [128, 8, 128], F32R) for i in range(2)]
                stats = sbC("stats2", [128, 2, 6]); mv = sbC("mv2", [128, 2]); rstd = sbC("rstd2", [128, 1])
                HTv = T["HT"].ap().rearrange("(c p) t -> p c t", p=128)
                for ti in range(NT):
                    hr = hr2[ti % 2]; m_ = macc[ti % 2]; z = zt2[ti % 2]; h2 = h2t[ti % 2]
                    K.dma(hr[:], (T["H1"][ti * 128:(ti + 1) * 128, :], ti))
                    for k in range(4):
                        y_ = yg[(ti % 2) * 4 + k]

                        def ga(e, ti=ti, k=k, y_=y_):
                            return e.indirect_dma_start(
                                out=y_[:], out_offset=None, in_=T["Ys"].ap(),
                                in_offset=bass.IndirectOffsetOnAxis(ap=dest_all[:, ti, k:k + 1], axis=0),
                                bounds_check=REG["bc"], oob_is_err=False)
                        K.S.add("pool", ga, [("Ys", None), _rk((dest_all[:, ti, :], ti))], [_rk(y_[:])], [], dma=True)
                        if k == 0:
                            K.ts("dve", m_[:], y_[:], (gates_all[:, ti, 0:1], ti), None, ALU.mult)
                        else:
                            K.stt(m_[:], y_[:], (gates_all[:, ti, k:k + 1], ti), m_[:], ALU.mult, ALU.add)
                    K.stt(z[:], hr[:], ALPHA, m_[:], ALU.mult, ALU.add)
                    layer_norm(K, z, h2, g2, b2, stats, mv, rstd, keyed=False)
                    dst = out_t if last else T["H2"]
                    K.dma((dst[ti * 128:(ti + 1) * 128, :], ti), h2[:], q="act")
                    if not last:
                        hx = hT2[ti % 2]
                        tr8(h2, lambda hb, hx=hx: hx[:, hb * 4:(hb + 1) * 4, :])
                        K.dma((HTv[:, :, ti * 128:(ti + 1) * 128], ti // TPB), hx[:], q="act")

        K.S.stopped = False
        K.S.barrier()
        for n, t_ in dbg_t.items():
            for ti in range(NT):
                K.dma((t_[ti * 128:(ti + 1) * 128, :], ti), (T[n][ti * 128:(ti + 1) * 128, :], ti))
        K.S.emit()
    return nc


def make_in_maps(inputs, nlayers=DEPTH, cores=range(8)):
    f = lambda a: np.ascontiguousarray(a, dtype=np.float32)
    shared = {}
    shared["mem_w_k"] = f(inputs["mem_w_k"]); shared["mem_w_v"] = f(inputs["mem_w_v"])
    for k, v in host_consts().items():
        shared["c_" + k] = v
    for l in range(nlayers):
        shared[f"w_in{l}"] = f(inputs[f"l{l}_w_in"])
        if KINDS[l] == 0:
            p = {n: np.asarray(inputs[f"l{l}_s5_{n}"]) for n in
                 ("a_re", "a_im", "log_dt", "b_re", "b_im", "c_re", "c_im", "d", "w_glu", "b_glu")}
            for k, v in s5_layouts(p).items():
                shared[f"{k}{l}"] = f(v)
        if KINDS[l] == 1:
            cq = np.asarray(inputs[f"l{l}_ml_conv_q"]).reshape(4, 4, 96)
            ck = np.asarray(inputs[f"l{l}_ml_conv_k"]).reshape(4, 4, 96)
            shared[f"convq{l}"] = f(cq.transpose(2, 1, 0)); shared[f"convk{l}"] = f(ck.transpose(2, 1, 0))
            shared[f"bi{l}"] = f(np.asarray(inputs[f"l{l}_ml_b_i"]).reshape(1, 4))
            shared[f"bf{l}"] = f(np.asarray(inputs[f"l{l}_ml_b_f"]).reshape(1, 4))
            shared[f"ng{l}"] = f(np.asarray(inputs[f"l{l}_ml_norm_g"]).reshape(1, 768))
        if KINDS[l] == 2:
            shared[f"ng{l}"] = f(np.asarray(inputs[f"l{l}_ret_norm_g"]).reshape(1, 768))
    for n in ("w_out", "ln1_g", "ln1_b", "ln2_g", "ln2_b", "router_w", "router_b"):
        shared[n] = f(inputs[n])
    for n in ("exp_w_gu", "exp_w_down", "exp_b_down"):
        shared[n] = f(inputs[n][:nlayers])
    bgu = np.asarray(inputs["exp_b_gu"]).reshape(DEPTH, NE, 16, 128)
    shared["exp_b_gu_l"] = f(bgu.transpose(0, 3, 1, 2)[:nlayers])
    maps = []
    for c in cores:
        m = dict(shared)
        m["x"] = f(inputs["x"][c]); m["mem"] = f(inputs["mem"][c])
        m["pos"] = np.ascontiguousarray(np.asarray(inputs["positions"][c]).reshape(1, L).astype(np.int32))
        maps.append(m)
    return maps


def kernel(**inputs):
    nc = build()
    maps = make_in_maps(inputs)
    res = run_bass_kernel_spmd(nc, maps, core_ids=list(range(8)))
    return np.stack([np.asarray(r["out"]) for r in res.results], axis=0).astype(np.float32)
```

```python
import math
import numpy as np
import concourse.bass as bass
import concourse.mybir as mybir
from concourse.bass_utils import run_bass_kernel_spmd
from contextlib import ExitStack

F32 = mybir.dt.float32
F32R = mybir.dt.float32r
I32 = mybir.dt.int32
U32 = mybir.dt.uint32
ALU = mybir.AluOpType
AF = mybir.ActivationFunctionType
AX = mybir.AxisListType

L = 2048
D = 1024
NT = 16
NB = 4
DEPTH = 4
NE = 32
CAP = 384
NSLOT = NE * CAP
ALPHA = (2.0 * DEPTH) ** 0.25
EPS = 1e-5
TWO_PI = 2.0 * math.pi
C1 = 6.28125
C2 = TWO_PI - C1
MAGIC = 12582912.0
PI_LO = 3.1415925

SAME_ENGINE_SYNC = True
EPOCH = 20000
REG = {}
DMA_RING = {"sp": 16, "pool": 8, "act": 6}


class Sched:
    def __init__(self, nc):
        self.nc = nc
        self.ops = []
        self.n_eng = {e: 0 for e in ("pe", "act", "dve", "pool", "sp")}
        self.n_dma = {}
        self.dma_rr = {q: 0 for q in DMA_RING}
        self.W = {}
        self.R = {}
        self.seen = {e: {} for e in self.n_eng}
        self.sig = set()
        self.floor = {}
        self.last_c = {}
        self.stopped = False

    def barrier(self):
        for e, o in self.last_c.items():
            self.floor[e] = o
        for s, o in self.n_dma.items():
            self.floor[s] = o

    @staticmethod
    def _dep(deps, so):
        for s, o in so.items():
            if o > deps.get(s, 0):
                deps[s] = o

    def _gather(self, table, res, deps):
        name, key = res
        d = table.get(name)
        if not d:
            return
        if key is None:
            for so in d.values():
                self._dep(deps, so)
        else:
            if key in d:
                self._dep(deps, d[key])
            if None in d:
                self._dep(deps, d[None])

    def add(self, eng, fn, reads=(), writes=(), acc=(), dma=False):
        if self.stopped:
            return None
        deps = {}
        for r in reads:
            self._gather(self.W, r, deps)
        for w in writes:
            self._gather(self.W, w, deps)
            self._gather(self.R, w, deps)
        for a in acc:
            self._gather(self.R, a, deps)
        self._dep(deps, self.floor)
        self.n_eng[eng] += 1
        if dma:
            slot = self.dma_rr[eng] % DMA_RING[eng]
            self.dma_rr[eng] += 1
            stream = ("dma", eng, slot)
            prev = self.n_dma.get(stream, 0)
            if prev:
                self._dep(deps, {stream: prev})
            self.n_dma[stream] = prev + 1
            ev = (stream, prev + 1)
        else:
            ev = (eng, self.n_eng[eng])
            self.last_c[eng] = self.n_eng[eng]
        waits = []
        seen = self.seen[eng]
        for s, o in deps.items():
            if s == eng and not dma and (eng == "pe" or not SAME_ENGINE_SYNC):
                continue
            if seen.get(s, 0) >= o:
                continue
            seen[s] = o
            waits.append((s, o))
            self.sig.add((s, o))
        self.ops.append((eng, fn, waits, ev, dma))
        for (name, key) in reads:
            self.R.setdefault(name, {}).setdefault(key, {})[ev[0]] = ev[1]
        for (name, key) in writes:
            if key is None:
                self.W[name] = {None: {ev[0]: ev[1]}}
                self.R[name] = {}
            else:
                self.W.setdefault(name, {})[key] = {ev[0]: ev[1]}
                self.R.setdefault(name, {})[key] = {}
        for (name, key) in acc:
            self.W.setdefault(name, {}).setdefault(key, {})[ev[0]] = ev[1]
        return ev

    def emit(self):
        nc = self.nc
        waits = []
        for s, o in self.n_dma.items():
            if self.seen["sp"].get(s, 0) < o:
                waits.append((s, o))
        self.ops.append(("sp", None, waits, None, False))
        sigmap = {}
        per = {e: sorted(o for (s, o) in self.sig if s == e) for e in self.n_eng}
        for e, lst in per.items():
            for i, o in enumerate(lst):
                sigmap[(e, o)] = (i // EPOCH, i % EPOCH + 1)
        with ExitStack() as es:
            esem = {}
            for e in self.n_eng:
                for k in range(max(1, (len(per[e]) + EPOCH - 1) // EPOCH)):
                    esem[(e, k)] = es.enter_context(nc.semaphore(f"s_{e}_{k}"))
            dsem = {}
            for s in self.n_dma:
                dsem[s] = es.enter_context(nc.semaphore(f"d_{s[1]}_{s[2]}"))
            block = es.enter_context(nc.Block())
            streams = {e: [] for e in self.n_eng}
            for (eng, fn, w, ev, dma) in self.ops:
                streams[eng].append((fn, w, ev, dma))
            sig = self.sig

            def lower(s, o):
                if isinstance(s, tuple):
                    return dsem[s], 16 * o
                k, v = sigmap[(s, o)]
                return esem[(s, k)], v

            def run(name, eng):
                if name == "pool":
                    REG["bc"] = eng.to_reg(NSLOT - 1)
                for (fn, w, ev, dma) in streams[name]:
                    for (s, o) in w:
                        sem, v = lower(s, o)
                        eng.wait_ge(sem, v)
                    if fn is None:
                        continue
                    ins = fn(eng)
                    if dma:
                        ins.then_inc(dsem[ev[0]], 16)
                    elif ev in sig:
                        k, v = sigmap[ev]
                        ins.then_inc(esem[(name, k)], 1)

            @block.tensor
            def _(e):
                run("pe", e)

            @block.scalar
            def _(e):
                run("act", e)

            @block.vector
            def _(e):
                run("dve", e)

            @block.gpsimd
            def _(e):
                run("pool", e)

            @block.sync
            def _(e):
                run("sp", e)


def _ap(x):
    return x[0] if isinstance(x, tuple) else x


def _rk(x):
    if isinstance(x, tuple):
        return (x[0].tensor.name, x[1])
    return (x.tensor.name, None)


class KB:
    def __init__(self, nc, es):
        self.nc = nc
        self.es = es
        self.S = Sched(nc)

    def sb(self, name, shape, dt=F32):
        return self.es.enter_context(self.nc.sbuf_tensor(name, shape, dt))

    def _add(self, eng, fn, ins, outs, acc=(), dma=False):
        self.S.add(eng, fn, [_rk(i) for i in ins if i is not None and not isinstance(i, (int, float))],
                   [_rk(o) for o in outs], [_rk(a) for a in acc], dma)

    def dma(self, out, in_, q="sp", acc=False):
        o, i = _ap(out), _ap(in_)
        self._add(q, lambda e: e.dma_start(out=o, in_=i), [in_], [] if acc else [out], [out] if acc else [], dma=True)

    def mm(self, out, lhsT, rhs, start=True, stop=True, sgc=False):
        o, l, r = _ap(out), _ap(lhsT), _ap(rhs)
        self._add("pe", lambda e: e.matmul(o, lhsT=l, rhs=r, start=start, stop=stop, skip_group_check=sgc), [lhsT, rhs], [out])

    def tr(self, out, in_, ident):
        o, i, d = _ap(out), _ap(in_), _ap(ident)
        self._add("pe", lambda e: e.transpose(o, i, d), [in_, ident], [out])

    def act(self, out, in_, func, bias=0.0, scale=1.0, accum_out=None, extra_ins=()):
        o, i = _ap(out), _ap(in_)
        b = _ap(bias) if not isinstance(bias, (int, float)) else float(bias)
        sc = _ap(scale) if not isinstance(scale, (int, float)) else float(scale)
        ac = _ap(accum_out) if accum_out is not None else None
        outs = [out] + ([accum_out] if accum_out is not None else [])

        def fn(e):
            kw = {}
            if ac is not None:
                kw["accum_out"] = ac
            return e.activation(out=o, in_=i, func=func, bias=b, scale=sc, **kw)
        self._add("act", fn, [in_, bias, scale] + list(extra_ins), outs)

    def copy(self, eng, out, in_):
        o, i = _ap(out), _ap(in_)
        if eng == "act":
            self._add("act", lambda e: e.copy(out=o, in_=i), [in_], [out])
        else:
            self._add(eng, lambda e: e.tensor_copy(out=o, in_=i), [in_], [out])

    def tt(self, eng, out, in0, in1, op):
        o, a, b = _ap(out), _ap(in0), _ap(in1)
        self._add(eng, lambda e: e.tensor_tensor(out=o, in0=a, in1=b, op=op), [in0, in1], [out])

    def ts(self, eng, out, in0, s1, s2, op0, op1=None, accum_out=None):
        o, a = _ap(out), _ap(in0)
        x1 = _ap(s1) if not isinstance(s1, (int, float)) else float(s1)
        x2 = None if s2 is None else (_ap(s2) if not isinstance(s2, (int, float)) else float(s2))
        ac = _ap(accum_out) if accum_out is not None else None
        outs = [out] + ([accum_out] if accum_out is not None else [])

        def fn(e):
            kw = {}
            if op1 is not None:
                kw["op1"] = op1
            if ac is not None:
                kw["accum_out"] = ac
            return e.tensor_scalar(out=o, in0=a, scalar1=x1, scalar2=x2, op0=op0, **kw)
        self._add(eng, fn, [in0, s1, s2], outs)

    def stt(self, out, in0, scalar, in1, op0, op1, accum_out=None):
        o, a, b = _ap(out), _ap(in0), _ap(in1)
        sc = _ap(scalar) if not isinstance(scalar, (int, float)) else float(scalar)
        ac = _ap(accum_out) if accum_out is not None else None
        outs = [out] + ([accum_out] if accum_out is not None else [])

        def fn(e):
            kw = {}
            if ac is not None:
                kw["accum_out"] = ac
            return e.scalar_tensor_tensor(out=o, in0=a, scalar=sc, in1=b, op0=op0, op1=op1, **kw)
        self._add("dve", fn, [in0, scalar, in1], outs)

    def scan(self, out, data0, data1, initial):
        o, a, b = _ap(out), _ap(data0), _ap(data1)
        ini = _ap(initial) if not isinstance(initial, (int, float)) else float(initial)
        self._add("dve", lambda e: e.tensor_tensor_scan(out=o, data0=a, data1=b, initial=ini, op0=ALU.mult, op1=ALU.add),
                  [data0, data1, initial], [out])

    def memset(self, eng, out, val):
        o = _ap(out)
        self._add(eng, lambda e: e.memset(o, val), [], [out])

    def recip(self, out, in_):
        o, i = _ap(out), _ap(in_)
        self._add("dve", lambda e: e.reciprocal(out=o, in_=i), [in_], [out])

    def generic(self, eng, fn, ins, outs):
        self._add(eng, fn, ins, outs)


def host_consts():
    c = {}
    c["ident"] = np.eye(128, dtype=np.float32)
    i = np.arange(128)
    c["stri"] = (i[:, None] < i[None, :]).astype(np.float32)
    c["ones"] = np.ones((128, 128), np.float32)
    c["iota32"] = np.tile(np.arange(32, dtype=np.float32)[None, :], (128, 1))
    c["ebase"] = np.tile((np.arange(32, dtype=np.float32) * CAP)[None, :], (128, 1))
    c["jota"] = np.tile(np.arange(1, 129, dtype=np.float32)[None, :], (128, 1))
    c["tri"] = (i[:, None] <= i[None, :]).astype(np.float32)
    lg_ = np.log(1.0 - np.power(2.0, -5.0 - np.arange(4, dtype=np.float32))).astype(np.float32)
    c["lfc"] = np.tile(lg_[None, :], (128, 1)).astype(np.float32)
    inv = (10000.0 ** (-np.arange(0, 96, 2, dtype=np.float32) / 96.0)).astype(np.float32)
    c["invf"] = np.concatenate([inv, inv])[:, None].astype(np.float32)
    c["sgn"] = np.concatenate([-np.ones(48), np.ones(48)])[:, None].astype(np.float32)
    return c


def s5_layouts(p):
    o = {}
    bblk = np.zeros((128, 6, 2, 2, 128), np.float32)
    cblk = np.zeros((128, 2, 24, 64), np.float32)
    are = np.zeros((128, 24), np.float32)
    aim = np.zeros((128, 24), np.float32)
    ldt = np.zeros((128, 24), np.float32)
    for q in range(24):
        for gi in range(2):
            g = 2 * q + gi
            r0 = 32 * (q % 4) + 16 * gi
            bblk[r0:r0 + 16, q // 4, 0, q % 2, 64 * gi:64 * gi + 64] = p["b_re"][g].T
            bblk[r0:r0 + 16, q // 4, 1, q % 2, 64 * gi:64 * gi + 64] = p["b_im"][g].T
            co = 32 * (q % 2) + 16 * gi
            cblk[64 * gi:64 * gi + 64, 0, q, co:co + 16] = p["c_re"][g].T
            cblk[64 * gi:64 * gi + 64, 1, q, co:co + 16] = p["c_im"][g].T
            are[64 * gi:64 * gi + 64, q] = p["a_re"][g]
            aim[64 * gi:64 * gi + 64, q] = p["a_im"][g]
            ldt[64 * gi:64 * gi + 64, q] = p["log_dt"][g]
    o["bblk"] = bblk
    o["cblk"] = cblk
    o["are"] = are
    o["aim"] = aim
    o["ldt"] = ldt
    o["dsk"] = np.ascontiguousarray(p["d"].reshape(6, 128).T)
    o["bglu"] = np.ascontiguousarray(p["b_glu"].reshape(6, 128).T)
    o["wglu"] = np.ascontiguousarray(p["w_glu"])
    return o


KINDS = [0, 1, 2, 0]
N_IN = [1024, 2568, 2560, 1024]
BT = 256
NBLK = L // BT
TPB = BT // 128


def layer_norm(K, z, out, g, b, stats, mv, rstd, keyed=True):
    zin = [(z[:, 0:512], 0), (z[:, 512:1024], 1)] if keyed else [z[:], z[:]]
    for hh in range(2):
        K.generic("dve", (lambda e, hh=hh: e.bn_stats(out=stats[:, hh, :], in_=z[:, hh * 512:(hh + 1) * 512])),
                  [zin[hh]], [(stats[:, hh, :], hh)])
    K.generic("dve", lambda e: e.bn_aggr(out=mv[:], in_=stats[:, :, :].rearrange("p a b -> p (a b)")),
              [(stats[:, 0, :], 0), (stats[:, 1, :], 1)], [mv[:]])
    K.ts("dve", rstd[:], mv[:, 1:2], EPS, None, ALU.add)
    K.act(rstd[:], rstd[:], AF.Sqrt)
    K.recip(rstd[:], rstd[:])
    K.generic("dve", lambda e: e.tensor_scalar(out=out[:], in0=z[:], scalar1=mv[:, 0:1], scalar2=rstd[:, 0:1],
                                               op0=ALU.subtract, op1=ALU.mult),
              zin + [mv[:], rstd[:]], [out[:]])
    K.tt("pool", out[:], out[:], g[:], ALU.mult)
    K.tt("pool", out[:], out[:], b[:], ALU.add)


class StopBuild(Exception):
    pass


def build(nlayers=DEPTH, dbg=(), stop=None):
    nc = bass.Bass("TRN2", target_bir_lowering=False)
    nc.dge_precook = False
    T = {}

    def din(name, shape, dt=F32):
        T[name] = nc.dram_tensor(name, list(shape), dt, kind="ExternalInput")
        return T[name]

    def dscr(name, shape, dt=F32):
        T[name] = nc.dram_tensor(name, list(shape), dt, kind="Internal")
        return T[name]

    din("x", [L, D]); din("mem", [256, D]); din("pos", [1, L], I32)
    din("mem_w_k", [D, 256], F32R); din("mem_w_v", [D, 256], F32R)
    for k, v in host_consts().items():
        din("c_" + k, v.shape)
    for l in range(nlayers):
        din(f"w_in{l}", [D, N_IN[l]], F32R)
        if KINDS[l] == 0:
            din(f"bblk{l}", [128, 6, 2, 2, 128], F32R); din(f"cblk{l}", [128, 2, 24, 64])
            din(f"are{l}", [128, 24]); din(f"aim{l}", [128, 24]); din(f"ldt{l}", [128, 24])
            din(f"dsk{l}", [128, 6]); din(f"bglu{l}", [128, 6]); din(f"wglu{l}", [768, 768], F32R)
        if KINDS[l] == 1:
            din(f"convq{l}", [96, 4, 4]); din(f"convk{l}", [96, 4, 4]); din(f"bi{l}", [1, 4]); din(f"bf{l}", [1, 4])
        if KINDS[l] in (1, 2):
            din(f"ng{l}", [1, 768])
    din("w_out", [DEPTH, D, D], F32R)
    for n in ("ln1_g", "ln1_b", "ln2_g", "ln2_b"):
        din(n, [DEPTH, D])
    din("router_w", [DEPTH, D, NE]); din("router_b", [DEPTH, NE])
    din("exp_w_gu", [nlayers, NE, D, 2 * D], F32R)
    din("exp_b_gu_l", [nlayers, 128, NE, 16])
    din("exp_w_down", [nlayers, NE, D, D], F32R)
    din("exp_b_down", [nlayers, NE, D], F32R)
    out_t = nc.dram_tensor("out", [L, D], F32, kind="ExternalOutput")
    dscr("H1", [L, D]); dscr("H2", [L, D]); dscr("HT", [D, L], F32R)
    dscr("Xs", [NSLOT, D]); dscr("Ys", [NSLOT, D])
    dbg_t = {n: nc.dram_tensor("dbg_" + n, [L, D], F32, kind="ExternalOutput") for n in dbg}

    with ExitStack() as es:
        K = KB(nc, es)
        sb = K.sb
        ps = [es.enter_context(nc.psum_tensor(f"ps{i}", [128, 512], F32)) for i in range(6)]
        psY = es.enter_context(nc.psum_tensor("psY", [128, 1024], F32))

        def tr8(src, dst_of_hb, engs=("dve", "act")):
            for hb in range(2):
                for c4 in range(4):
                    c = hb * 4 + c4
                    K.tr(ps[hb][:, c4 * 128:(c4 + 1) * 128], src[:, c * 128:(c + 1) * 128], ident[:])
                K.copy(engs[hb], dst_of_hb(hb), ps[hb][:, :].rearrange("p (c t) -> p c t", c=4))

        ident = sb("ident", [128, 128]); stri = sb("stri", [128, 128])
        ones = sb("ones", [128, 128]); iota32 = sb("iota32", [128, 32]); ebase = sb("ebase", [128, 32])
        jota = sb("jota", [128, 128]); tri = sb("tri", [128, 128]); lfc = sb("lfc", [128, 4])
        invf = sb("invf", [96, 1]); sgn = sb("sgn", [96, 1])
        for t_, n_ in ((ident, "ident"), (stri, "stri"), (ones, "ones"), (iota32, "iota32"),
                       (ebase, "ebase"), (jota, "jota"), (tri, "tri"), (lfc, "lfc"), (invf, "invf"), (sgn, "sgn")):
            K.dma(t_[:], T["c_" + n_].ap())
        ones_r = sb("ones_r", [128, 128], F32R)
        K.copy("dve", ones_r[:], ones[:])
        kT = sb("kT", [64, 4, 256], F32R)
        vv = sb("vv", [128, 2, 256], F32R)
        gates_all = sb("gates_all", [128, NT, 4])
        dest_all = sb("dest_all", [128, NT, 4], I32)
        mask_all = sb("mask_all", [128, NT, 32])

        with ExitStack() as es2:
            def sb2(name, shape, dt=F32):
                return es2.enter_context(nc.sbuf_tensor(name, shape, dt))
            zrow = sb2("zrow", [128, 1024])
            K.memset("pool", zrow[:], 0.0)
            for r in range(0, NSLOT, 128):
                K.dma((T["Xs"][r:r + 128, :], r // CAP), zrow[:])
            memT = sb2("memT", [128, 8, 256], F32R)
            wk = sb2("wk", [128, 8, 256], F32R)
            wv = sb2("wv", [128, 8, 256], F32R)
            mt = [sb2(f"mt{i}", [128, 1024]) for i in range(2)]
            K.dma(wk[:], T["mem_w_k"].ap().rearrange("(c p) n -> p c n", p=128))
            K.dma(wv[:], T["mem_w_v"].ap().rearrange("(c p) n -> p c n", p=128))
            for m in range(2):
                K.dma(mt[m][:], T["mem"][m * 128:(m + 1) * 128, :])
                tr8(mt[m], lambda hb, m=m: memT[:, hb * 4:(hb + 1) * 4, m * 128:(m + 1) * 128])
            for h in range(4):
                for c in range(8):
                    K.mm(ps[2][0:64, 0:256], wk[:, c, 64 * h:64 * h + 64], memT[:, c, :], start=(c == 0), stop=(c == 7))
                K.copy("dve", kT[:, h, :], ps[2][0:64, 0:256])
            for m in range(2):
                for c in range(8):
                    K.mm(ps[3][:, 0:256], memT[:, c, m * 128:(m + 1) * 128], wv[:, c, :], start=(c == 0), stop=(c == 7))
                K.copy("act", vv[:, m, :], ps[3][:, 0:256])
            HTv = T["HT"].ap().rearrange("(c p) t -> p c t", p=128)
            xts = [sb2(f"xts{i}", [128, 8, 128], F32R) for i in range(2)]
            for t in range(NT):
                xt = mt[t % 2]
                K.dma(xt[:], T["x"][t * 128:(t + 1) * 128, :])
                tr8(xt, lambda hb, t=t: xts[t % 2][:, hb * 4:(hb + 1) * 4, :])
                K.dma((HTv[:, :, t * 128:(t + 1) * 128], t // TPB), xts[t % 2][:])

        if stop == "setup":
            K.S.stopped = True
        for l in range(nlayers):
            kind = KINDS[l]
            Hres = T["x"] if l == 0 else T["H2"]
            last = (l == nlayers - 1)
            with ExitStack() as esA:
                K.S.barrier()
                def sbA(name, shape, dt=F32):
                    return esA.enter_context(nc.sbuf_tensor(f"{name}_{l}", shape, dt))
                g1 = sbA("g1", [128, 1024]); b1 = sbA("b1", [128, 1024])
                K.dma(g1[:], T["ln1_g"][l:l + 1, :].partition_broadcast(128))
                K.dma(b1[:], T["ln1_b"][l:l + 1, :].partition_broadcast(128))
                rw = sbA("rw", [128, 8, 32]); rb = sbA("rb", [1, 32])
                K.dma(rw[:], T["router_w"][l].rearrange("(c p) n -> p c n", p=128))
                K.dma(rb[:], T["router_b"][l:l + 1, :])
                hTb = sbA("hTb", [128, 8, BT], F32R)
                wp = [sbA(f"wp{i}", [128, 8, 128], F32R) for i in range(3)]
                wpi = [0]
                xqT = sbA("xqT", [64, 4, BT], F32R)
                catT = sbA("catT", [128, 6, BT], F32R)
                Eb = [sbA(f"E{i}", [128, BT], F32R) for i in range(2)]
                rec = sbA("rec", [64, BT])
                hres = [sbA(f"hres{i}", [128, 1024]) for i in range(2)]
                zt = sbA("zt", [128, 1024])
                h1t = [sbA(f"h1t{i}", [128, 1024]) for i in range(2)]
                h1T = sbA("h1T", [128, 8, 128])
                stats = sbA("stats", [128, 2, 6]); mv = sbA("mv", [128, 2]); rstd = sbA("rstd", [128, 1])
                lg = sbA("lg", [128, 32]); m8 = sbA("m8", [128, 8]); i8 = sbA("i8", [128, 8], U32)
                negm = sbA("negm", [128, 1]); e4 = sbA("e4", [128, 4]); ssum = sbA("ssum", [128, 1])
                idxf = sbA("idxf", [128, 4]); ovf = sbA("ovf", [128, 32]); slot = sbA("slot", [128, 32])
                junk = sbA("junk", [128, 32]); destf = sbA("destf", [128, 4])
                w_in = T[f"w_in{l}"].ap().rearrange("(c p) n -> p c n", p=128)
                HTv = T["HT"].ap().rearrange("(c p) t -> p c t", p=128)

                def next_wp():
                    w = wp[wpi[0] % 3]
                    wpi[0] += 1
                    return w

                def proj_fm(col0, ncols, evac):
                    w = next_wp()
                    pb = ps[wpi[0] % 2]
                    K.dma(w[:, :, 0:ncols], w_in[:, :, col0:col0 + ncols])
                    for c in range(8):
                        K.mm(pb[0:ncols, 0:BT], w[:, c, 0:ncols], hTb[:, c, :], start=(c == 0), stop=(c == 7))
                    evac(pb[0:ncols, 0:BT])

                if kind == 0:
                    bblk = sbA("bblk", [128, 6, 2, 2, 128], F32R)
                    K.dma(bblk[:], T[f"bblk{l}"].ap())
                    dsk = sbA("dsk", [128, 6])
                    K.dma(dsk[:], T[f"dsk{l}"].ap())
                    bglu = sbA("bglu", [128, 6])
                    K.dma(bglu[:], T[f"bglu{l}"].ap())
                    CSC = sbA("CSC", [128, 24, 384])
                    COS = CSC[:, :, 0:128]; SIN = CSC[:, :, 128:256]
                    rmag = sbA("rmag", [128, 24]); theta = sbA("theta", [128, 24])
                    Cp = sbA("Cp", [128, 24, 64]); Cin = sbA("Cin", [128, 24, 64])
                    with ExitStack() as esP:
                        def sbP(name, shape):
                            return esP.enter_context(nc.sbuf_tensor(f"{name}_{l}", shape, F32))
                        cblk = sbP("cblk", [128, 2, 24, 64])
                        K.dma(cblk[:], T[f"cblk{l}"].ap())
                        are = sbP("are", [128, 24]); aim = sbP("aim", [128, 24]); ldt = sbP("ldt", [128, 24])
                        K.dma(are[:], T[f"are{l}"].ap()); K.dma(aim[:], T[f"aim{l}"].ap()); K.dma(ldt[:], T[f"ldt{l}"].ap())
                        lam = sbP("lam", [128, 24]); dtt = sbP("dtt", [128, 24]); tmp = sbP("tmp", [128, 24])
                        tmp2 = sbP("tmp2", [128, 24]); sn = sbP("sn", [128, 24]); cs = sbP("cs", [128, 24])
                        abr = sbP("abr", [128, 24]); abi = sbP("abi", [128, 24]); den = sbP("den", [128, 24])
                        cre = sbP("cre", [128, 24]); cim = sbP("cim", [128, 24])
                        ANG = sbP("ANG", [128, 24, 128]); KK = sbP("KK", [128, 24, 128])
                        t32a = sbP("t32a", [128, 24, 64]); t32b = sbP("t32b", [128, 24, 64])

                        def range_sin(out, ang, kk, shift):
                            K.ts("dve", kk, ang, 1.0 / TWO_PI, shift / TWO_PI, ALU.mult, ALU.add)
                            K.ts("dve", kk, kk, MAGIC, MAGIC, ALU.add, ALU.subtract)
                            K.stt(out, kk, -C1, ang, ALU.mult, ALU.add)
                            K.stt(out, kk, -C2, out, ALU.mult, ALU.add)
                            K.ts("dve", out, out, shift, None, ALU.add)
                            K.ts("dve", out, out, -PI_LO, PI_LO, ALU.max, ALU.min)
                            K.act(out, out, AF.Sin)

                        K.ts("dve", lam[:], are[:], -1e-4, None, ALU.min)
                        K.act(dtt[:], ldt[:], AF.Exp)
                        K.tt("dve", tmp[:], dtt[:], lam[:], ALU.mult)
                        K.act(rmag[:], tmp[:], AF.Exp)
                        K.tt("dve", theta[:], dtt[:], aim[:], ALU.mult)
                        range_sin(sn[:], theta[:], tmp[:], 0.0)
                        range_sin(cs[:], theta[:], tmp[:], math.pi / 2)
                        K.tt("dve", abr[:], rmag[:], cs[:], ALU.mult)
                        K.tt("dve", abi[:], rmag[:], sn[:], ALU.mult)
                        K.ts("dve", abr[:], abr[:], -1.0, None, ALU.add)
                        K.tt("dve", den[:], lam[:], lam[:], ALU.mult)
                        K.tt("dve", tmp[:], aim[:], aim[:], ALU.mult)
                        K.tt("dve", den[:], den[:], tmp[:], ALU.add)
                        K.recip(den[:], den[:])
                        K.tt("dve", tmp[:], abr[:], lam[:], ALU.mult)
                        K.tt("dve", tmp2[:], abi[:], aim[:], ALU.mult)
                        K.tt("dve", tmp[:], tmp[:], tmp2[:], ALU.add)
                        K.tt("dve", cre[:], tmp[:], den[:], ALU.mult)
                        K.tt("dve", tmp[:], abi[:], lam[:], ALU.mult)
                        K.tt("dve", tmp2[:], abr[:], aim[:], ALU.mult)
                        K.tt("dve", tmp[:], tmp[:], tmp2[:], ALU.subtract)
                        K.tt("dve", cim[:], tmp[:], den[:], ALU.mult)
                        creb = cre[:].unsqueeze(2).to_broadcast([128, 24, 64])
                        cimb = cim[:].unsqueeze(2).to_broadcast([128, 24, 64])
                        K.tt("dve", t32a[:], cblk[:, 0, :, :], creb, ALU.mult)
                        K.tt("dve", t32b[:], cblk[:, 1, :, :], cimb, ALU.mult)
                        K.tt("dve", Cp[:], t32a[:], t32b[:], ALU.subtract)
                        K.tt("dve", t32a[:], cblk[:, 0, :, :], cimb, ALU.mult)
                        K.tt("dve", t32b[:], cblk[:, 1, :, :], creb, ALU.mult)
                        K.tt("dve", t32a[:], t32a[:], t32b[:], ALU.add)
                        K.ts("dve", Cin[:], t32a[:], -1.0, None, ALU.mult)
                        for q in range(24):
                            K.ts("dve", ANG[:, q, :], jota[:], theta[:, q:q + 1], None, ALU.mult)
                        range_sin(SIN, ANG[:, :, :], KK[:, :, :], 0.0)
                        range_sin(COS, ANG[:, :, :], KK[:, :, :], math.pi / 2)
                        K.copy("dve", CSC[:, :, 256:384], COS)

                    K.S.barrier()
                    if stop == f"P{l}":
                        K.S.stopped = True
                    uT = catT
                    ygT = sbA("ygT", [128, 6, BT], F32R)
                    ygT_f = ygT.bitcast(F32)
                    car_re = sbA("car_re", [128, 24]); car_im = sbA("car_im", [128, 24])
                    K.memset("pool", car_re[:], 0.0); K.memset("pool", car_im[:], 0.0)
                    NBUF = 4
                    mk = lambda n, w_: [sbA(f"{n}_{i}", [128, w_]) for i in range(NBUF)]
                    T12, T43, RR, WW, VAC, VBD, XX = [mk(n, 256) for n in ("T12", "T43", "RR", "WW", "VAC", "VBD", "XX")]
                    ytok = sbA("ytok", [128, 768]); yx2 = sbA("yx2", [128, 768])
                    sgl = [sbA(f"sgl{i}", [128, BT]) for i in range(2)]
                    wgluv = T[f"wglu{l}"].ap().rearrange("(c p) n -> p c n", p=128)


                if kind in (1, 2):
                    LNS = math.log(96.0 ** -0.5)
                    qT = sbA("qT", [96, 4, BT], F32R); kTt = sbA("kTt", [96, 4, BT], F32R)
                    qT_f = qT.bitcast(F32); kTt_f = kTt.bitcast(F32)
                    vt = [sbA(f"vt{i}", [128, 4, 194], F32R) for i in range(2)]
                    for v_ in vt:
                        K.memset("pool", v_.bitcast(F32)[:], 0.0)
                        K.copy("dve", v_[:, :, 192:193], ones[:, 0:4].unsqueeze(2))
                    clns = sbA("clns", [128, 1])
                    K.memset("pool", clns[:], LNS)
                    gt = [sbA(f"gt{i}", [128, 768]) for i in range(2)]
                    gps = sbA("gps", [128, 8]); lf = sbA("lf", [128, 4]); igb = sbA("igb", [128, 4]); bcol = sbA("bcol", [128, 4])
                    bias1 = sbA("bias1", [128, 4]); bias2 = sbA("bias2", [128, 4])
                    rhsB = [sbA(f"rhsB{i}", [128, 128]) for i in range(4)]
                    DT = [sbA(f"DT{i}", [128, 128]) for i in range(4)]
                    SD = [sbA(f"SD{i}", [128, 128], F32R) for i in range(4)]
                    EBt = [sbA(f"EBt{i}", [96, 128]) for i in range(4)]
                    qs = [sbA(f"qs{i}", [96, 128], F32R) for i in range(4)]
                    kw = [sbA(f"kw{i}", [128, 96], F32R) for i in range(4)]
                    wvv = [sbA(f"wvv{i}", [128, 1]) for i in range(4)]
                    ebt = [sbA(f"ebt{i}", [96, 1]) for i in range(4)]
                    Cst = [sbA(f"Cst{i}", [96, 4, 194], F32R) for i in range(2)]
                    Cst_f = [c_.bitcast(F32) for c_ in Cst]
                    K.memset("pool", Cst_f[0][:], 0.0); K.memset("pool", Cst_f[1][:], 0.0)
                    hN = [sbA(f"hN{i}", [128, 192]) for i in range(4)]; dn = [sbA(f"dn{i}", [128, 1]) for i in range(4)]
                    hst = [sbA(f"hst{i}", [128, 6]) for i in range(4)]; hmv = [sbA(f"hmv{i}", [128, 2]) for i in range(4)]
                    hrs = [sbA(f"hrs{i}", [128, 1]) for i in range(4)]
                    ymx = sbA("ymx", [128, 768]); gsg = sbA("gsg", [128, 768])
                    ngb = sbA("ngb", [128, 768])
                    K.dma(ngb[:], T[f"ng{l}"].ap().partition_broadcast(128))
                    if kind == 1:
                        qpre = sbA("qpre", [96, 4, 3 + BT]); kpre = sbA("kpre", [96, 4, 3 + BT])
                        K.memset("pool", qpre[:], 0.0); K.memset("pool", kpre[:], 0.0)
                        cacc = sbA("cacc", [96, BT])
                        convq = sbA("convq", [96, 4, 4]); convk = sbA("convk", [96, 4, 4])
                        K.dma(convq[:], T[f"convq{l}"].ap()); K.dma(convk[:], T[f"convk{l}"].ap())
                        bib = sbA("bib", [128, 4]); bfb = sbA("bfb", [128, 4])
                        K.dma(bib[:], T[f"bi{l}"].ap().partition_broadcast(128))
                        K.dma(bfb[:], T[f"bf{l}"].ap().partition_broadcast(128))
                    else:
                        RC = sbA("RC", [96, L]); RS = sbA("RS", [96, L])
                        qraw = sbA("qraw", [96, BT]); qsw = sbA("qsw", [96, BT])
                        with ExitStack() as esR:
                            posb = esR.enter_context(nc.sbuf_tensor(f"posb_{l}", [96, L], I32))
                            posf = esR.enter_context(nc.sbuf_tensor(f"posf_{l}", [96, L], F32))
                            kkr = esR.enter_context(nc.sbuf_tensor(f"kkr_{l}", [96, L], F32))
                            K.dma(posb[:], T["pos"].ap().partition_broadcast(96))
                            K.copy("dve", posf[:], posb[:])
                            K.ts("dve", posf[:], posf[:], invf[:, 0:1], None, ALU.mult)

                            def range_sin2(out, ang, kk, shift):
                                K.ts("dve", kk, ang, 1.0 / TWO_PI, shift / TWO_PI, ALU.mult, ALU.add)
                                K.ts("dve", kk, kk, MAGIC, MAGIC, ALU.add, ALU.subtract)
                                K.stt(out, kk, -C1, ang, ALU.mult, ALU.add)
                                K.stt(out, kk, -C2, out, ALU.mult, ALU.add)
                                K.ts("dve", out, out, shift, None, ALU.add)
                                K.ts("dve", out, out, -PI_LO, PI_LO, ALU.max, ALU.min)
                                K.act(out, out, AF.Sin)
                            range_sin2(RS[:], posf[:], kkr[:], 0.0)
                            range_sin2(RC[:], posf[:], kkr[:], math.pi / 2)
                            K.ts("dve", RS[:], RS[:], sgn[:, 0:1], None, ALU.mult)
                        K.S.barrier()

                for tb in range(NBLK):
                    tsl = slice(tb * BT, (tb + 1) * BT)
                    K.dma(hTb[:], HTv[:, :, tsl])
                    xq0 = N_IN[l] - 256
                    for h in range(4):
                        proj_fm(xq0 + 64 * h, 64, lambda p, h=h: K.copy("act", xqT[:, h, :], p))
                    if stop == "J0":
                        K.S.stopped = True
                    if kind == 0:
                        for c in range(6):
                            proj_fm(128 * c, 128, lambda p, c=c: K.copy("dve" if c % 2 else "act", uT[:, c, :], p))
                        for s in range(TPB):
                            ssl = slice(s * 128, (s + 1) * 128)

                            banks = [ps[2], ps[5], ps[3], ps[4]]

                            def bu(q):
                                c, r0 = q // 4, 64 * ((q % 4) // 2)
                                pq = banks[q % 4]
                                K.mm(pq[:, 0:128], bblk[r0:r0 + 64, c, 0, q % 2, :], uT[r0:r0 + 64, c, ssl])
                                K.mm(pq[:, 128:256], bblk[r0:r0 + 64, c, 1, q % 2, :], uT[r0:r0 + 64, c, ssl])
                            for q in range(4):
                                bu(q)
                            for g in range(6):
                                qs_ = range(4 * g, 4 * g + 4)
                                c = g
                                for q in qs_:
                                    K.tt("dve", T12[q % 4][:], banks[q % 4][:, 0:256], CSC[:, q, 0:256], ALU.mult)
                                for q in qs_:
                                    K.tt("dve", T43[q % 4][:], banks[q % 4][:, 0:256], CSC[:, q, 128:384], ALU.mult)
                                if g + 1 < 6:
                                    for q in range(4 * g + 4, 4 * g + 8):
                                        bu(q)
                                for q in qs_:
                                    b = q % 4
                                    K.tt("dve", RR[b][:, 0:128], T12[b][:, 0:128], T12[b][:, 128:256], ALU.add)
                                for q in qs_:
                                    b = q % 4
                                    K.tt("dve", RR[b][:, 128:256], T43[b][:, 128:256], T43[b][:, 0:128], ALU.subtract)
                                for q in qs_:
                                    b = q % 4
                                    K.scan(WW[b][:, 0:128], rmag[:, q:q + 1].to_broadcast([128, 128]), RR[b][:, 0:128], (car_re[:, q:q + 1], q))
                                for q in qs_:
                                    b = q % 4
                                    K.scan(WW[b][:, 128:256], rmag[:, q:q + 1].to_broadcast([128, 128]), RR[b][:, 128:256], (car_im[:, q:q + 1], q))
                                for q in qs_:
                                    b = q % 4
                                    K.tt("dve", VAC[b][:].rearrange("p (a b) -> p a b", a=2),
                                         WW[b][:, 0:128].unsqueeze(1).to_broadcast([128, 2, 128]),
                                         CSC[:, q, 0:256].rearrange("p (a b) -> p a b", a=2), ALU.mult)
                                for q in qs_:
                                    b = q % 4
                                    K.tt("dve", VBD[b][:].rearrange("p (a b) -> p a b", a=2),
                                         WW[b][:, 128:256].unsqueeze(1).to_broadcast([128, 2, 128]),
                                         CSC[:, q, 128:384].rearrange("p (a b) -> p a b", a=2), ALU.mult)
                                for q in qs_:
                                    b = q % 4
                                    K.tt("pool", XX[b][:, 0:128], VAC[b][:, 0:128], VBD[b][:, 0:128], ALU.subtract)
                                for q in qs_:
                                    b = q % 4
                                    K.tt("pool", XX[b][:, 128:256], VAC[b][:, 128:256], VBD[b][:, 128:256], ALU.add)
                                for q in qs_:
                                    b = q % 4
                                    K.copy("pool", (car_re[:, q:q + 1], q), XX[b][:, 127:128])
                                    K.copy("pool", (car_im[:, q:q + 1], q), XX[b][:, 255:256])
                                for q in qs_:
                                    b = q % 4
                                    hq = (q % 4) // 2
                                    yo = (psY[64 * hq:64 * hq + 64, 128 * c:128 * c + 128], c // 4)
                                    K.mm(yo, Cp[:, q, :], XX[b][:, 0:128], start=(q % 2 == 0), stop=False, sgc=True)
                                    K.mm(yo, Cin[:, q, :], XX[b][:, 128:256], start=False, stop=(q % 2 == 1), sgc=True)
                            uT_f = uT.bitcast(F32)
                            for c in range(6):
                                K.stt((ytok[:, 128 * c:128 * c + 128], c), uT_f[:, c, ssl], dsk[:, c:c + 1],
                                      (psY[:, 128 * c:128 * c + 128], c // 4), ALU.mult, ALU.add)
                            K.act(yx2[:], ytok[:], AF.Square)
                            K.ts("dve", yx2[:], yx2[:], 0.044715, 1.0, ALU.mult, ALU.add)
                            K.tt("dve", yx2[:], yx2[:], ytok[:], ALU.mult)
                            K.act(yx2[:], yx2[:], AF.Sigmoid, scale=1.5957691216057308)
                            K.tt("dve", ygT[:, :, ssl], ytok[:].rearrange("p (c t) -> p c t", c=6),
                                 yx2[:].rearrange("p (c t) -> p c t", c=6), ALU.mult)
                        if stop == "S0":
                            K.S.stopped = True
                        for j in range(6):
                            w = next_wp()
                            pb = ps[j % 2]
                            K.dma(w[:, 0:6, :], wgluv[:, :, 128 * j:128 * j + 128])
                            for c in range(6):
                                K.mm(pb[:, 0:BT], w[:, c, :], ygT[:, c, :], start=(c == 0), stop=(c == 5))
                            K.act(sgl[j % 2][:], pb[:, 0:BT], AF.Sigmoid, bias=bglu[:, j:j + 1])
                            K.tt("dve", catT[:, j, :], ygT_f[:, j, :], sgl[j % 2][:], ALU.mult)
                    else:
                        for h in range(4):
                            if kind == 1:
                                proj_fm(96 * h, 96, lambda p, h=h: K.copy("act", qpre[:, h, 3:3 + BT], p))
                                proj_fm(384 + 96 * h, 96, lambda p, h=h: K.copy("act", kpre[:, h, 3:3 + BT], p))
                                for (pre, cw, dstT) in ((qpre, convq, qT), (kpre, convk, kTt)):
                                    K.ts("dve", cacc[:], pre[:, h, 3:3 + BT], cw[:, h, 3:4], None, ALU.mult)
                                    for w_ in (2, 1, 0):
                                        K.stt(cacc[:], pre[:, h, w_:w_ + BT], cw[:, h, w_:w_ + 1], cacc[:], ALU.mult, ALU.add)
                                    K.act(dstT[:, h, :], cacc[:], AF.Silu)
                            else:
                                for (c0_, dstT, dst_f) in ((0, qT, qT_f), (384, kTt, kTt_f)):
                                    proj_fm(c0_ + 96 * h, 96, lambda p: K.copy("act", qraw[:], p))
                                    w = next_wp()
                                    pb = ps[wpi[0] % 2]
                                    K.dma(w[:, :, 0:48], w_in[:, :, c0_ + 96 * h + 48:c0_ + 96 * h + 96])
                                    K.dma(w[:, :, 48:96], w_in[:, :, c0_ + 96 * h:c0_ + 96 * h + 48])
                                    for c in range(8):
                                        K.mm(pb[0:96, 0:BT], w[:, c, 0:96], hTb[:, c, :], start=(c == 0), stop=(c == 7))
                                    K.tt("dve", qsw[:], pb[0:96, 0:BT], RS[:, tsl], ALU.mult)
                                    K.tt("pool", qraw[:], qraw[:], RC[:, tsl], ALU.mult)
                                    K.tt("dve", dstT[:, h, :], qraw[:], qsw[:], ALU.add)
                        if kind == 1:
                            K.copy("pool", qpre[:, :, 0:3], qpre[:, :, BT:BT + 3])
                            K.copy("pool", kpre[:, :, 0:3], kpre[:, :, BT:BT + 3])
                        for pc in range(12):
                            w = next_wp()
                            K.dma(w[:], w_in[:, :, 768 + 128 * pc:768 + 128 * pc + 128])
                            for j in range(TPB):
                                pb = ps[(pc * TPB + j) % 2]
                                for c in range(8):
                                    K.mm(pb[:, 0:128], hTb[:, c, j * 128:(j + 1) * 128], w[:, c, :], start=(c == 0), stop=(c == 7))
                                if pc < 6:
                                    n0 = 128 * pc
                                    while n0 < 128 * pc + 128:
                                        hh_ = n0 // 192
                                        n1 = min(128 * pc + 128, 192 * (hh_ + 1))
                                        K.copy("act" if (n0 // 64) % 2 else "dve", vt[j][:, hh_, n0 - 192 * hh_:n1 - 192 * hh_],
                                               pb[:, n0 - 128 * pc:n1 - 128 * pc])
                                        n0 = n1
                                else:
                                    K.copy("act", gt[j][:, 128 * (pc - 6):128 * (pc - 6) + 128], pb[:, 0:128])
                        if kind == 1:
                            wg_ = next_wp()
                            K.dma(wg_[:, :, 0:8], w_in[:, :, 2304:2312])
                        for j in range(TPB):
                            csl = slice(j * 128, (j + 1) * 128)
                            if kind == 1:
                                for c in range(8):
                                    K.mm(ps[2][:, 0:8], hTb[:, c, csl], wg_[:, c, 0:8], start=(c == 0), stop=(c == 7))
                                K.copy("dve", gps[:], ps[2][:, 0:8])
                                K.tt("dve", igb[:], gps[:, 0:4], bib[:], ALU.add)
                                K.tt("dve", lf[:], gps[:, 4:8], bfb[:], ALU.add)
                                K.act(lf[:], lf[:], AF.Exp, scale=-1.0)
                                K.act(lf[:], lf[:], AF.Ln, bias=ones[:, 0:1])
                                K.ts("dve", lf[:], lf[:], -1.0, None, ALU.mult)
                                lfx = lf
                            else:
                                lfx = lfc
                            K.mm(ps[2][:, 0:4], tri[:], lfx[:], start=True, stop=True)
                            if kind == 1:
                                K.tt("dve", bias2[:], igb[:], ps[2][:, 0:4], ALU.subtract)
                                K.ts("dve", bias1[:], bias2[:], LNS, None, ALU.add)
                            else:
                                K.ts("dve", bias1[:], ps[2][:, 0:4], -1.0, LNS, ALU.mult, ALU.add)
                                K.copy("dve", bias2[:], bias1[:])
                            cur = Cst[(tb * TPB + j) % 2]; nxt = Cst[(tb * TPB + j + 1) % 2]
                            cur_f = Cst_f[(tb * TPB + j) % 2]
                            H4 = range(4)
                            for h in H4:
                                K.ts("dve", rhsB[h][:], tri[:], lfx[:, h:h + 1], None, ALU.mult)
                            for h in H4:
                                K.mm(ps[3][:, 128 * h:128 * h + 128], ones[:], rhsB[h][:])
                            for h in H4:
                                K.act(DT[h][:], ps[3][:, 128 * h:128 * h + 128], AF.Exp, bias=bias1[:, h:h + 1])
                            for h in H4:
                                K.act(EBt[h][:], ps[3][0:96, 128 * h:128 * h + 128], AF.Exp, bias=(clns[0:96, 0:1] if kind == 1 else 0.0))
                            for h in H4:
                                K.act(wvv[h][:], ps[3][:, 128 * h + 127:128 * h + 128], AF.Exp, bias=bias2[:, h:h + 1])
                                K.act(ebt[h][:], ps[3][0:96, 128 * h + 127:128 * h + 128], AF.Exp)
                            for h in H4:
                                K.tr(ps[4][:, 96 * h:96 * h + 96], kTt_f[:, h, csl], ident[0:96, 0:96])
                            for h in H4:
                                K.mm(ps[5][:, 128 * h:128 * h + 128], kTt[:, h, csl], qT[:, h, csl])
                            for h in H4:
                                K.tt("dve", DT[h][:], DT[h][:], tri[:], ALU.mult)
                            for h in H4:
                                K.tt("dve", qs[h][:], qT_f[:, h, csl], EBt[h][:], ALU.mult)
                            for h in H4:
                                K.ts("dve", kw[h][:], ps[4][:, 96 * h:96 * h + 96], wvv[h][:, 0:1], None, ALU.mult)
                            for h in H4:
                                K.tt("dve", SD[h][:], ps[5][:, 128 * h:128 * h + 128], DT[h][:], ALU.mult)
                            psN = [(psY[:, 512 * (h // 2) + 256 * (h % 2):512 * (h // 2) + 256 * (h % 2) + 194], h // 2) for h in H4]
                            psS = [ps[h // 2][0:96, 256 * (h % 2):256 * (h % 2) + 194] for h in H4]
                            for h in H4:
                                K.mm(psN[h], SD[h][:], vt[j][:, h, :], start=True, stop=False, sgc=True)
                                K.mm(psN[h], qs[h][:], (cur[:, h, :], h), start=False, stop=True, sgc=True)
                            for h in H4:
                                K.mm(psS[h], kw[h][:], vt[j][:, h, :], start=True, stop=True, sgc=True)
                            for h in H4:
                                K.stt((nxt[:, h, :], h), (cur_f[:, h, :], h), ebt[h][:, 0:1], psS[h], ALU.mult, ALU.add)
                            if kind == 1:
                                for h in H4:
                                    K.act(dn[h][:], (psN[h][0][:, 192:193], h // 2), AF.Abs)
                                for h in H4:
                                    K.ts("dve", dn[h][:], dn[h][:], 1.0, None, ALU.max)
                                for h in H4:
                                    K.recip(dn[h][:], dn[h][:])
                                for h in H4:
                                    K.ts("dve", hN[h][:], (psN[h][0][:, 0:192], h // 2), dn[h][:, 0:1], None, ALU.mult)
                            else:
                                for h in H4:
                                    K.copy("act" if h % 2 else "dve", hN[h][:], (psN[h][0][:, 0:192], h // 2))
                            for h in H4:
                                K.generic("dve", lambda e, a=hst[h], b_=hN[h]: e.bn_stats(out=a[:], in_=b_[:]), [hN[h][:]], [hst[h][:]])
                            for h in H4:
                                K.generic("dve", lambda e, a=hst[h], b_=hmv[h]: e.bn_aggr(out=b_[:], in_=a[:]), [hst[h][:]], [hmv[h][:]])
                            for h in H4:
                                K.ts("dve", hrs[h][:], hmv[h][:, 1:2], EPS, None, ALU.add)
                            for h in H4:
                                K.act(hrs[h][:], hrs[h][:], AF.Sqrt)
                            for h in H4:
                                K.recip(hrs[h][:], hrs[h][:])
                            for h in H4:
                                K.ts("dve", (ymx[:, 192 * h:192 * h + 192], h), hN[h][:], hmv[h][:, 0:1], hrs[h][:, 0:1], ALU.subtract, ALU.mult)
                            K.tt("dve", ymx[:], ymx[:], ngb[:], ALU.mult)
                            K.act(gsg[:], gt[j][:], AF.Sigmoid if kind == 1 else AF.Silu)
                            K.tt("dve", ymx[:], ymx[:], gsg[:], ALU.mult)
                            for c in range(6):
                                pb = ps[c // 4]
                                K.tr(pb[:, (c % 4) * 128:(c % 4) * 128 + 128], ymx[:, 128 * c:128 * c + 128], ident[:])
                            K.copy("act", catT[:, 0:4, csl], ps[0][:, :].rearrange("p (c t) -> p c t", c=4))
                            K.copy("dve", catT[:, 4:6, csl], ps[1][:, 0:256].rearrange("p (c t) -> p c t", c=2))

                    if stop == "G0":
                        K.S.stopped = True
                    for h in range(4):
                        for m in range(2):
                            K.mm(ps[3 + m][:, 0:BT], kT[:, h, m * 128:(m + 1) * 128], xqT[:, h, :])
                            K.act(Eb[m][:], ps[3 + m][:, 0:BT], AF.Exp, scale=0.125)
                        for m in range(2):
                            K.mm(ps[5][0:64, 0:BT], vv[:, m, 64 * h:64 * h + 64], Eb[m][:], start=(m == 0), stop=(m == 1))
                        for m in range(2):
                            K.mm(ps[2][0:64, 0:BT], ones_r[:, 0:64], Eb[m][:], start=(m == 0), stop=(m == 1))
                        K.recip(rec[:], ps[2][0:64, 0:BT])
                        K.tt("dve", xqT[:, h, :], ps[5][0:64, 0:BT], rec[:], ALU.mult)
                    ymT = xqT

                    if stop == "X0":
                        K.S.stopped = True
                    accs = [(psY[:, 0:512], 0), (psY[:, 512:1024], 1), ps[3][:, :], ps[4][:, :]]
                    for c in range(10):
                        w = next_wp()
                        wv_ = w[:, :, :].rearrange("p a b -> p (a b)")
                        if c < 6:
                            K.dma(w[:], T["w_out"][l, 128 * c:128 * c + 128, :].rearrange("p (a b) -> p a b", b=128))
                        else:
                            hq = c - 6
                            K.generic_dma = None
                            K.dma((w[0:64, :, :], None), T["w_out"][l, 768 + 64 * hq:768 + 64 * hq + 64, :].rearrange("p (a b) -> p a b", b=128))
                        for j in range(TPB):
                            jsl = slice(j * 128, (j + 1) * 128)
                            for hh in range(2):
                                a = accs[j * 2 + hh]
                                if c < 6:
                                    K.mm(a, catT[:, c, jsl], wv_[:, hh * 512:(hh + 1) * 512], start=(c == 0), stop=False)
                                else:
                                    K.mm(a, ymT[:, c - 6, jsl], wv_[0:64, hh * 512:(hh + 1) * 512], start=False, stop=(c == 9))
                    if stop == "O0":
                        K.S.stopped = True
                    for j in range(TPB):
                        ti = tb * TPB + j
                        hr = hres[ti % 2]; z = zt; h1 = h1t[ti % 2]
                        K.dma(hr[:], (Hres[ti * 128:(ti + 1) * 128, :], ti))
                        for hh in range(2):
                            K.stt((z[:, hh * 512:(hh + 1) * 512], hh), hr[:, hh * 512:(hh + 1) * 512], ALPHA, accs[j * 2 + hh], ALU.mult, ALU.add)
                        layer_norm(K, z, h1, g1, b1, stats, mv, rstd)
                        K.dma((T["H1"][ti * 128:(ti + 1) * 128, :], ti), h1[:], q="act")
                        if stop == "L0":
                            K.S.stopped = True
                        tr8(h1, lambda hb: h1T[:, hb * 4:(hb + 1) * 4, :])
                        for c in range(8):
                            K.mm(ps[5][:, 0:32], h1T[:, c, :], rw[:, c, :], start=(c == 0), stop=False)
                        K.mm(ps[5][:, 0:32], ones[0:1, :], rb[0:1, :], start=False, stop=True)
                        K.copy("dve", lg[:], ps[5][:, 0:32])
                        K.generic("dve", lambda e, m8=m8, lg=lg: e.max(out=m8[:], in_=lg[:]), [lg[:]], [m8[:]])
                        K.generic("dve", lambda e, m8=m8, lg=lg, i8=i8: e.max_index(out=i8[:], in_max=m8[:], in_values=lg[:]), [lg[:], m8[:]], [i8[:]])
                        K.ts("dve", negm[:], m8[:, 0:1], -1.0, None, ALU.mult)
                        K.act(e4[:], m8[:, 0:4], AF.Exp, bias=negm[:], accum_out=ssum[:])
                        K.recip(ssum[:], ssum[:])
                        K.ts("dve", (gates_all[:, ti, :], ti), e4[:], ssum[:], None, ALU.mult)
                        K.copy("dve", idxf[:], i8[:, 0:4])
                        K.ts("dve", (mask_all[:, ti, :], ti), lg[:], m8[:, 3:4], None, ALU.is_ge)
                        for t2_ in range(ti):
                            K.mm(ps[2][:, 0:32], ones[:], (mask_all[:, t2_, :], t2_), start=(t2_ == 0), stop=False)
                        K.mm(ps[2][:, 0:32], stri[:], (mask_all[:, ti, :], ti), start=(ti == 0), stop=True)
                        K.ts("dve", ovf[:], ps[2][:, 0:32], float(CAP), 1.0e6, ALU.is_ge, ALU.mult)
                        K.tt("dve", slot[:], ps[2][:, 0:32], ebase[:], ALU.add)
                        K.tt("dve", slot[:], slot[:], ovf[:], ALU.add)
                        for k in range(4):
                            K.stt(junk[:], iota32[:], idxf[:, k:k + 1], slot[:], ALU.is_equal, ALU.mult, accum_out=(destf[:, k:k + 1], k))
                        K.generic("dve", lambda e, ti=ti, destf=destf: e.tensor_copy(out=dest_all[:, ti, :], in_=destf[:]),
                                  [(destf[:, k:k + 1], k) for k in range(4)], [(dest_all[:, ti, :], ti)])
                        if stop == "R0":
                            K.S.stopped = True
                        for k in range(4):
                            def sc(e, ti=ti, k=k, h1=h1):
                                return e.indirect_dma_start(
                                    out=T["Xs"].ap(), out_offset=bass.IndirectOffsetOnAxis(ap=dest_all[:, ti, k:k + 1], axis=0),
                                    in_=h1[:], in_offset=None, bounds_check=REG["bc"], oob_is_err=False)
                            K.S.add("pool", sc, [_rk(h1[:]), _rk((dest_all[:, ti, :], ti))], [],
                                    [("Xs", e_) for e_ in range(NE)], dma=True)
            if stop == f"A{l}":
                K.S.stopped = True

            with ExitStack() as esB:
                K.S.barrier()
                def sbB(name, shape, dt=F32):
                    return esB.enter_context(nc.sbuf_tensor(f"{name}_{l}", shape, dt))
                XT = [sbB(f"XT{i}", [128, 8, CAP], F32R) for i in range(2)]
                xs = [sbB(f"xs{i}", [128, 1024]) for i in range(2)]
                NWG = 6
                wg = [sbB(f"wg{i}", [128, 8, 256], F32R) for i in range(NWG)]
                NWD = 10
                wd = [sbB(f"wd{i}", [128, 1024], F32R) for i in range(NWD)]
                bd = [sbB(f"bd{i}", [1, 1024], F32R) for i in range(2)]
                actT = sbB("actT", [128, 8, CAP], F32R)
                bgu = sbB("bgu", [128, NE, 16])
                K.dma(bgu[:], T["exp_b_gu_l"][l])
                K.ts("dve", bgu[:, :, 8:16], bgu[:, :, 8:16], 1.0, None, ALU.add)
                gg, sg, ll = [[sbB(f"{n}{i}", [128, CAP]) for i in range(2)] for n in ("gg", "sg", "ll")]
                yt = [sbB(f"yt{i}", [128, 1024]) for i in range(2)]
                xi = 0; gi_ = 0; yi = 0; wdi = 0
                def load_xt(e2):
                    nonlocal_xi = xi_box
                    X2 = XT[e2 % 2]
                    for t in range(3):
                        x_ = xs[nonlocal_xi[0] % 2]; nonlocal_xi[0] += 1
                        r0 = e2 * CAP + t * 128
                        K.dma(x_[:], (T["Xs"][r0:r0 + 128, :], e2))
                        tr8(x_, lambda hb, X2=X2, t=t: X2[:, hb * 4:(hb + 1) * 4, t * 128:(t + 1) * 128])
                xi_box = [0]
                load_xt(0)
                for e_ in range(NE):
                    X = XT[e_ % 2]
                    wgu = T["exp_w_gu"][l, e_].rearrange("(c p) n -> p c n", p=128)
                    for jj in range(4):
                        wG = wg[gi_ % NWG]; gi_ += 1
                        wL = wg[gi_ % NWG]; gi_ += 1
                        K.dma(wG[:], wgu[:, :, 256 * jj:256 * jj + 256])
                        K.dma(wL[:], wgu[:, :, 1024 + 256 * jj:1024 + 256 * jj + 256])
                        for j2 in range(2):
                            j = 2 * jj + j2
                            pG = ps[2 + 2 * j2]; pL = ps[3 + 2 * j2]
                            for c in range(8):
                                K.mm(pG[:, 0:CAP], wG[:, c, 128 * j2:128 * j2 + 128], X[:, c, :], start=(c == 0), stop=(c == 7))
                            for c in range(8):
                                K.mm(pL[:, 0:CAP], wL[:, c, 128 * j2:128 * j2 + 128], X[:, c, :], start=(c == 0), stop=(c == 7))
                            b = j2
                            K.ts("dve", gg[b][:], pG[:, 0:CAP], bgu[:, e_, j:j + 1], 7.0, ALU.add, ALU.min)
                            K.act(sg[b][:], gg[b][:], AF.Silu, scale=1.702)
                            K.act(ll[b][:], pL[:, 0:CAP], AF.Identity, bias=bgu[:, e_, 8 + j:9 + j])
                            K.ts("dve", ll[b][:], ll[b][:], 8.0, -6.0, ALU.min, ALU.max)
                            K.stt((actT[:, j, :], j), sg[b][:], 1.0 / 1.702, ll[b][:], ALU.mult, ALU.mult)
                    wds = []
                    for j in range(8):
                        w = wd[wdi % NWD]; wdi += 1
                        wds.append(w)
                        K.dma(w[:], T["exp_w_down"][l, e_, 128 * j:128 * j + 128, :])
                    K.dma(bd[e_ % 2][:], T["exp_b_down"][l, e_:e_ + 1, :])
                    if e_ + 1 < NE:
                        load_xt(e_ + 1)
                    for t in range(3):
                        y_ = yt[yi % 2]; yi += 1
                        for hh in range(2):
                            pb = (psY[:, hh * 512:(hh + 1) * 512], hh)
                            for j in range(8):
                                K.mm(pb, (actT[:, j, t * 128:(t + 1) * 128], j), wds[j][:, hh * 512:(hh + 1) * 512],
                                     start=(j == 0), stop=False)
                            K.mm(pb, ones_r[0:1, :], bd[e_ % 2][0:1, hh * 512:(hh + 1) * 512], start=False, stop=True)
                            K.copy("act" if hh else "dve", (y_[:, hh * 512:(hh + 1) * 512], hh), pb)
                        r0 = e_ * CAP + t * 128
                        K.S.add("act", (lambda e, r0=r0, y_=y_: e.dma_start(out=T["Ys"][r0:r0 + 128, :], in_=y_[:])),
                                [_rk(y_[:])], [], [("Ys", None)], dma=True)
            if stop == f"B{l}":
                K.S.stopped = True

            with ExitStack() as esC:
                K.S.barrier()
                def sbC(name, shape, dt=F32):
                    return esC.enter_context(nc.sbuf_tensor(f"{name}_{l}", shape, dt))
                g2 = sbC("g2", [128, 1024]); b2 = sbC("b2", [128, 1024])
                K.dma(g2[:], T["ln2_g"][l:l + 1, :].partition_broadcast(128))
                K.dma(b2[:], T["ln2_b"][l:l + 1, :].partition_broadcast(128))
                yg = [sbC(f"yg{i}", [128, 1024]) for i in range(8)]
                for y_ in yg:
                    K.memset("pool", y_[:], 0.0)
                hr2 = [sbC(f"hr2{i}", [128, 1024]) for i in range(2)]
                macc = [sbC(f"macc{i}", [128, 1024]) for i in range(2)]
                zt2 = [sbC(f"zt2{i}", [128, 1024]) for i in range(2)]
                h2t = [sbC(f"h2t{i}", [128, 1024]) for i in range(2)]
                hT2 = [sbC(f"hT2{i}", [128, 8, 128], F32R) for i in range(2)]
                stats = sbC("stats2", [128, 2, 6]); mv = sbC("mv2", [128, 2]); rstd = sbC("rstd2", [128, 1])
                HTv = T["HT"].ap().rearrange("(c p) t -> p c t", p=128)
                for ti in range(NT):
                    hr = hr2[ti % 2]; m_ = macc[ti % 2]; z = zt2[ti % 2]; h2 = h2t[ti % 2]
                    K.dma(hr[:], (T["H1"][ti * 128:(ti + 1) * 128, :], ti))
                    for k in range(4):
                        y_ = yg[(ti % 2) * 4 + k]

                        def ga(e, ti=ti, k=k, y_=y_):
                            return e.indirect_dma_start(
                                out=y_[:], out_offset=None, in_=T["Ys"].ap(),
                                in_offset=bass.IndirectOffsetOnAxis(ap=dest_all[:, ti, k:k + 1], axis=0),
                                bounds_check=REG["bc"], oob_is_err=False)
                        K.S.add("pool", ga, [("Ys", None), _rk((dest_all[:, ti, :], ti))], [_rk(y_[:])], [], dma=True)
                        if k == 0:
                            K.ts("dve", m_[:], y_[:], (gates_all[:, ti, 0:1], ti), None, ALU.mult)
                        else:
                            K.stt(m_[:], y_[:], (gates_all[:, ti, k:k + 1], ti), m_[:], ALU.mult, ALU.add)
                    K.stt(z[:], hr[:], ALPHA, m_[:], ALU.mult, ALU.add)
                    layer_norm(K, z, h2, g2, b2, stats, mv, rstd, keyed=False)
                    dst = out_t if last else T["H2"]
                    K.dma((dst[ti * 128:(ti + 1) * 128, :], ti), h2[:], q="act")
                    if not last:
                        hx = hT2[ti % 2]
                        tr8(h2, lambda hb, hx=hx: hx[:, hb * 4:(hb + 1) * 4, :])
                        K.dma((HTv[:, :, ti * 128:(ti + 1) * 128], ti // TPB), hx[:], q="act")

        K.S.stopped = False
        K.S.barrier()
        for n, t_ in dbg_t.items():
            for ti in range(NT):
                K.dma((t_[ti * 128:(ti + 1) * 128, :], ti), (T[n][ti * 128:(ti + 1) * 128, :], ti))
        K.S.emit()
    return nc


def make_in_maps(inputs, nlayers=DEPTH, cores=range(8)):
    f = lambda a: np.ascontiguousarray(a, dtype=np.float32)
    shared = {}
    shared["mem_w_k"] = f(inputs["mem_w_k"]); shared["mem_w_v"] = f(inputs["mem_w_v"])
    for k, v in host_consts().items():
        shared["c_" + k] = v
    for l in range(nlayers):
        shared[f"w_in{l}"] = f(inputs[f"l{l}_w_in"])
        if KINDS[l] == 0:
            p = {n: np.asarray(inputs[f"l{l}_s5_{n}"]) for n in
                 ("a_re", "a_im", "log_dt", "b_re", "b_im", "c_re", "c_im", "d", "w_glu", "b_glu")}
            for k, v in s5_layouts(p).items():
                shared[f"{k}{l}"] = f(v)
        if KINDS[l] == 1:
            cq = np.asarray(inputs[f"l{l}_ml_conv_q"]).reshape(4, 4, 96)
            ck = np.asarray(inputs[f"l{l}_ml_conv_k"]).reshape(4, 4, 96)
            shared[f"convq{l}"] = f(cq.transpose(2, 1, 0)); shared[f"convk{l}"] = f(ck.transpose(2, 1, 0))
            shared[f"bi{l}"] = f(np.asarray(inputs[f"l{l}_ml_b_i"]).reshape(1, 4))
            shared[f"bf{l}"] = f(np.asarray(inputs[f"l{l}_ml_b_f"]).reshape(1, 4))
            shared[f"ng{l}"] = f(np.asarray(inputs[f"l{l}_ml_norm_g"]).reshape(1, 768))
        if KINDS[l] == 2:
            shared[f"ng{l}"] = f(np.asarray(inputs[f"l{l}_ret_norm_g"]).reshape(1, 768))
    for n in ("w_out", "ln1_g", "ln1_b", "ln2_g", "ln2_b", "router_w", "router_b"):
        shared[n] = f(inputs[n])
    for n in ("exp_w_gu", "exp_w_down", "exp_b_down"):
        shared[n] = f(inputs[n][:nlayers])
    bgu = np.asarray(inputs["exp_b_gu"]).reshape(DEPTH, NE, 16, 128)
    shared["exp_b_gu_l"] = f(bgu.transpose(0, 3, 1, 2)[:nlayers])
    maps = []
    for c in cores:
        m = dict(shared)
        m["x"] = f(inputs["x"][c]); m["mem"] = f(inputs["mem"][c])
        m["pos"] = np.ascontiguousarray(np.asarray(inputs["positions"][c]).reshape(1, L).astype(np.int32))
        maps.append(m)
    return maps


def kernel(**inputs):
    nc = build()
    maps = make_in_maps(inputs)
    res = run_bass_kernel_spmd(nc, maps, core_ids=list(range(8)))
    return np.stack([np.asarray(r["out"]) for r in res.results], axis=0).astype(np.float32)
```

```python
import math
import numpy as np
import concourse.bass as bass
import concourse.mybir as mybir
from concourse.bass_utils import run_bass_kernel_spmd
from contextlib import ExitStack

F32 = mybir.dt.float32
F32R = mybir.dt.float32r
I32 = mybir.dt.int32
U32 = mybir.dt.uint32
ALU = mybir.AluOpType
AF = mybir.ActivationFunctionType
AX = mybir.AxisListType

L = 2048
D = 1024
NT = 16
NB = 4
DEPTH = 4
NE = 32
CAP = 384
NSLOT = NE * CAP
ALPHA = (2.0 * DEPTH) ** 0.25
EPS = 1e-5
TWO_PI = 2.0 * math.pi
C1 = 6.28125
C2 = TWO_PI - C1
MAGIC = 12582912.0
PI_LO = 3.1415925

SAME_ENGINE_SYNC = True
EPOCH = 20000
REG = {}
DMA_RING = {"sp": 16, "pool": 8, "act": 6}


class Sched:
    def __init__(self, nc):
        self.nc = nc
        self.ops = []
        self.n_eng = {e: 0 for e in ("pe", "act", "dve", "pool", "sp")}
        self.n_dma = {}
        self.dma_rr = {q: 0 for q in DMA_RING}
        self.W = {}
        self.R = {}
        self.seen = {e: {} for e in self.n_eng}
        self.sig = set()
        self.floor = {}
        self.last_c = {}
        self.stopped = False

    def barrier(self):
        for e, o in self.last_c.items():
            self.floor[e] = o
        for s, o in self.n_dma.items():
            self.floor[s] = o

    @staticmethod
    def _dep(deps, so):
        for s, o in so.items():
            if o > deps.get(s, 0):
                deps[s] = o

    def _gather(self, table, res, deps):
        name, key = res
        d = table.get(name)
        if not d:
            return
        if key is None:
            for so in d.values():
                self._dep(deps, so)
        else:
            if key in d:
                self._dep(deps, d[key])
            if None in d:
                self._dep(deps, d[None])

    def add(self, eng, fn, reads=(), writes=(), acc=(), dma=False):
        if self.stopped:
            return None
        deps = {}
        for r in reads:
            self._gather(self.W, r, deps)
        for w in writes:
            self._gather(self.W, w, deps)
            self._gather(self.R, w, deps)
        for a in acc:
            self._gather(self.R, a, deps)
        self._dep(deps, self.floor)
        self.n_eng[eng] += 1
        if dma:
            slot = self.dma_rr[eng] % DMA_RING[eng]
            self.dma_rr[eng] += 1
            stream = ("dma", eng, slot)
            prev = self.n_dma.get(stream, 0)
            if prev:
                self._dep(deps, {stream: prev})
            self.n_dma[stream] = prev + 1
            ev = (stream, prev + 1)
        else:
            ev = (eng, self.n_eng[eng])
            self.last_c[eng] = self.n_eng[eng]
        waits = []
        seen = self.seen[eng]
        for s, o in deps.items():
            if s == eng and not dma and (eng == "pe" or not SAME_ENGINE_SYNC):
                continue
            if seen.get(s, 0) >= o:
                continue
            seen[s] = o
            waits.append((s, o))
            self.sig.add((s, o))
        self.ops.append((eng, fn, waits, ev, dma))
        for (name, key) in reads:
            self.R.setdefault(name, {}).setdefault(key, {})[ev[0]] = ev[1]
        for (name, key) in writes:
            if key is None:
                self.W[name] = {None: {ev[0]: ev[1]}}
                self.R[name] = {}
            else:
                self.W.setdefault(name, {})[key] = {ev[0]: ev[1]}
                self.R.setdefault(name, {})[key] = {}
        for (name, key) in acc:
            self.W.setdefault(name, {}).setdefault(key, {})[ev[0]] = ev[1]
        return ev

    def emit(self):
        nc = self.nc
        waits = []
        for s, o in self.n_dma.items():
            if self.seen["sp"].get(s, 0) < o:
                waits.append((s, o))
        self.ops.append(("sp", None, waits, None, False))
        sigmap = {}
        per = {e: sorted(o for (s, o) in self.sig if s == e) for e in self.n_eng}
        for e, lst in per.items():
            for i, o in enumerate(lst):
                sigmap[(e, o)] = (i // EPOCH, i % EPOCH + 1)
        with ExitStack() as es:
            esem = {}
            for e in self.n_eng:
                for k in range(max(1, (len(per[e]) + EPOCH - 1) // EPOCH)):
                    esem[(e, k)] = es.enter_context(nc.semaphore(f"s_{e}_{k}"))
            dsem = {}
            for s in self.n_dma:
                dsem[s] = es.enter_context(nc.semaphore(f"d_{s[1]}_{s[2]}"))
            block = es.enter_context(nc.Block())
            streams = {e: [] for e in self.n_eng}
            for (eng, fn, w, ev, dma) in self.ops:
                streams[eng].append((fn, w, ev, dma))
            sig = self.sig

            def lower(s, o):
                if isinstance(s, tuple):
                    return dsem[s], 16 * o
                k, v = sigmap[(s, o)]
                return esem[(s, k)], v

            def run(name, eng):
                if name == "pool":
                    REG["bc"] = eng.to_reg(NSLOT - 1)
                for (fn, w, ev, dma) in streams[name]:
                    for (s, o) in w:
                        sem, v = lower(s, o)
                        eng.wait_ge(sem, v)
                    if fn is None:
                        continue
                    ins = fn(eng)
                    if dma:
                        ins.then_inc(dsem[ev[0]], 16)
                    elif ev in sig:
                        k, v = sigmap[ev]
                        ins.then_inc(esem[(name, k)], 1)

            @block.tensor
            def _(e):
                run("pe", e)

            @block.scalar
            def _(e):
                run("act", e)

            @block.vector
            def _(e):
                run("dve", e)

            @block.gpsimd
            def _(e):
                run("pool", e)

            @block.sync
            def _(e):
                run("sp", e)


def _ap(x):
    return x[0] if isinstance(x, tuple) else x


def _rk(x):
    if isinstance(x, tuple):
        return (x[0].tensor.name, x[1])
    return (x.tensor.name, None)


class KB:
    def __init__(self, nc, es):
        self.nc = nc
        self.es = es
        self.S = Sched(nc)

    def sb(self, name, shape, dt=F32):
        return self.es.enter_context(self.nc.sbuf_tensor(name, shape, dt))

    def _add(self, eng, fn, ins, outs, acc=(), dma=False):
        self.S.add(eng, fn, [_rk(i) for i in ins if i is not None and not isinstance(i, (int, float))],
                   [_rk(o) for o in outs], [_rk(a) for a in acc], dma)

    def dma(self, out, in_, q="sp", acc=False):
        o, i = _ap(out), _ap(in_)
        self._add(q, lambda e: e.dma_start(out=o, in_=i), [in_], [] if acc else [out], [out] if acc else [], dma=True)

    def mm(self, out, lhsT, rhs, start=True, stop=True, sgc=False):
        o, l, r = _ap(out), _ap(lhsT), _ap(rhs)
        self._add("pe", lambda e: e.matmul(o, lhsT=l, rhs=r, start=start, stop=stop, skip_group_check=sgc), [lhsT, rhs], [out])

    def tr(self, out, in_, ident):
        o, i, d = _ap(out), _ap(in_), _ap(ident)
        self._add("pe", lambda e: e.transpose(o, i, d), [in_, ident], [out])

    def act(self, out, in_, func, bias=0.0, scale=1.0, accum_out=None, extra_ins=()):
        o, i = _ap(out), _ap(in_)
        b = _ap(bias) if not isinstance(bias, (int, float)) else float(bias)
        sc = _ap(scale) if not isinstance(scale, (int, float)) else float(scale)
        ac = _ap(accum_out) if accum_out is not None else None
        outs = [out] + ([accum_out] if accum_out is not None else [])

        def fn(e):
            kw = {}
            if ac is not None:
                kw["accum_out"] = ac
            return e.activation(out=o, in_=i, func=func, bias=b, scale=sc, **kw)
        self._add("act", fn, [in_, bias, scale] + list(extra_ins), outs)

    def copy(self, eng, out, in_):
        o, i = _ap(out), _ap(in_)
        if eng == "act":
            self._add("act", lambda e: e.copy(out=o, in_=i), [in_], [out])
        else:
            self._add(eng, lambda e: e.tensor_copy(out=o, in_=i), [in_], [out])

    def tt(self, eng, out, in0, in1, op):
        o, a, b = _ap(out), _ap(in0), _ap(in1)
        self._add(eng, lambda e: e.tensor_tensor(out=o, in0=a, in1=b, op=op), [in0, in1], [out])

    def ts(self, eng, out, in0, s1, s2, op0, op1=None, accum_out=None):
        o, a = _ap(out), _ap(in0)
        x1 = _ap(s1) if not isinstance(s1, (int, float)) else float(s1)
        x2 = None if s2 is None else (_ap(s2) if not isinstance(s2, (int, float)) else float(s2))
        ac = _ap(accum_out) if accum_out is not None else None
        outs = [out] + ([accum_out] if accum_out is not None else [])

        def fn(e):
            kw = {}
            if op1 is not None:
                kw["op1"] = op1
            if ac is not None:
                kw["accum_out"] = ac
            return e.tensor_scalar(out=o, in0=a, scalar1=x1, scalar2=x2, op0=op0, **kw)
        self._add(eng, fn, [in0, s1, s2], outs)

    def stt(self, out, in0, scalar, in1, op0, op1, accum_out=None):
        o, a, b = _ap(out), _ap(in0), _ap(in1)
        sc = _ap(scalar) if not isinstance(scalar, (int, float)) else float(scalar)
        ac = _ap(accum_out) if accum_out is not None else None
        outs = [out] + ([accum_out] if accum_out is not None else [])

        def fn(e):
            kw = {}
            if ac is not None:
                kw["accum_out"] = ac
            return e.scalar_tensor_tensor(out=o, in0=a, scalar=sc, in1=b, op0=op0, op1=op1, **kw)
        self._add("dve", fn, [in0, scalar, in1], outs)

    def scan(self, out, data0, data1, initial):
        o, a, b = _ap(out), _ap(data0), _ap(data1)
        ini = _ap(initial) if not isinstance(initial, (int, float)) else float(initial)
        self._add("dve", lambda e: e.tensor_tensor_scan(out=o, data0=a, data1=b, initial=ini, op0=ALU.mult, op1=ALU.add),
                  [data0, data1, initial], [out])

    def memset(self, eng, out, val):
        o = _ap(out)
        self._add(eng, lambda e: e.memset(o, val), [], [out])

    def recip(self, out, in_):
        o, i = _ap(out), _ap(in_)
        self._add("dve", lambda e: e.reciprocal(out=o, in_=i), [in_], [out])

    def generic(self, eng, fn, ins, outs):
        self._add(eng, fn, ins, outs)


def host_consts():
    c = {}
    c["ident"] = np.eye(128, dtype=np.float32)
    i = np.arange(128)
    c["stri"] = (i[:, None] < i[None, :]).astype(np.float32)
    c["ones"] = np.ones((128, 128), np.float32)
    c["iota32"] = np.tile(np.arange(32, dtype=np.float32)[None, :], (128, 1))
    c["ebase"] = np.tile((np.arange(32, dtype=np.float32) * CAP)[None, :], (128, 1))
    c["jota"] = np.tile(np.arange(1, 129, dtype=np.float32)[None, :], (128, 1))
    c["tri"] = (i[:, None] <= i[None, :]).astype(np.float32)
    lg_ = np.log(1.0 - np.power(2.0, -5.0 - np.arange(4, dtype=np.float32))).astype(np.float32)
    c["lfc"] = np.tile(lg_[None, :], (128, 1)).astype(np.float32)
    inv = (10000.0 ** (-np.arange(0, 96, 2, dtype=np.float32) / 96.0)).astype(np.float32)
    c["invf"] = np.concatenate([inv, inv])[:, None].astype(np.float32)
    c["sgn"] = np.concatenate([-np.ones(48), np.ones(48)])[:, None].astype(np.float32)
    return c


def s5_layouts(p):
    o = {}
    bblk = np.zeros((128, 6, 2, 2, 128), np.float32)
    cblk = np.zeros((128, 2, 24, 64), np.float32)
    are = np.zeros((128, 24), np.float32)
    aim = np.zeros((128, 24), np.float32)
    ldt = np.zeros((128, 24), np.float32)
    for q in range(24):
        for gi in range(2):
            g = 2 * q + gi
            r0 = 32 * (q % 4) + 16 * gi
            bblk[r0:r0 + 16, q // 4, 0, q % 2, 64 * gi:64 * gi + 64] = p["b_re"][g].T
            bblk[r0:r0 + 16, q // 4, 1, q % 2, 64 * gi:64 * gi + 64] = p["b_im"][g].T
            co = 32 * (q % 2) + 16 * gi
            cblk[64 * gi:64 * gi + 64, 0, q, co:co + 16] = p["c_re"][g].T
            cblk[64 * gi:64 * gi + 64, 1, q, co:co + 16] = p["c_im"][g].T
            are[64 * gi:64 * gi + 64, q] = p["a_re"][g]
            aim[64 * gi:64 * gi + 64, q] = p["a_im"][g]
            ldt[64 * gi:64 * gi + 64, q] = p["log_dt"][g]
    o["bblk"] = bblk
    o["cblk"] = cblk
    o["are"] = are
    o["aim"] = aim
    o["ldt"] = ldt
    o["dsk"] = np.ascontiguousarray(p["d"].reshape(6, 128).T)
    o["bglu"] = np.ascontiguousarray(p["b_glu"].reshape(6, 128).T)
    o["wglu"] = np.ascontiguousarray(p["w_glu"])
    return o


KINDS = [0, 1, 2, 0]
N_IN = [1024, 2568, 2560, 1024]
BT = 256
NBLK = L // BT
TPB = BT // 128


def layer_norm(K, z, out, g, b, stats, mv, rstd, keyed=True, eng="pool"):
    zin = [(z[:, 0:512], 0), (z[:, 512:1024], 1)] if keyed else [z[:], z[:]]
    for hh in range(2):
        K.generic("dve", (lambda e, hh=hh: e.bn_stats(out=stats[:, hh, :], in_=z[:, hh * 512:(hh + 1) * 512])),
                  [zin[hh]], [(stats[:, hh, :], hh)])
    K.generic("dve", lambda e: e.bn_aggr(out=mv[:], in_=stats[:, :, :].rearrange("p a b -> p (a b)")),
              [(stats[:, 0, :], 0), (stats[:, 1, :], 1)], [mv[:]])
    K.ts("dve", rstd[:], mv[:, 1:2], EPS, None, ALU.add)
    K.act(rstd[:], rstd[:], AF.Sqrt)
    K.recip(rstd[:], rstd[:])
    K.generic("dve", lambda e: e.tensor_scalar(out=out[:], in0=z[:], scalar1=mv[:, 0:1], scalar2=rstd[:, 0:1],
                                               op0=ALU.subtract, op1=ALU.mult),
              zin + [mv[:], rstd[:]], [out[:]])
    K.tt(eng, out[:], out[:], g[:], ALU.mult)
    K.tt(eng, out[:], out[:], b[:], ALU.add)


class StopBuild(Exception):
    pass


def build(nlayers=DEPTH, dbg=(), stop=None):
    nc = bass.Bass("TRN2", target_bir_lowering=False)
    nc.dge_precook = False
    T = {}

    def din(name, shape, dt=F32):
        T[name] = nc.dram_tensor(name, list(shape), dt, kind="ExternalInput")
        return T[name]

    def dscr(name, shape, dt=F32):
        T[name] = nc.dram_tensor(name, list(shape), dt, kind="Internal")
        return T[name]

    din("x", [L, D]); din("mem", [256, D]); din("pos", [1, L], I32)
    din("mem_w_k", [D, 256], F32R); din("mem_w_v", [D, 256], F32R)
    for k, v in host_consts().items():
        din("c_" + k, v.shape)
    for l in range(nlayers):
        din(f"w_in{l}", [D, N_IN[l]], F32R)
        if KINDS[l] == 0:
            din(f"bblk{l}", [128, 6, 2, 2, 128], F32R); din(f"cblk{l}", [128, 2, 24, 64])
            din(f"are{l}", [128, 24]); din(f"aim{l}", [128, 24]); din(f"ldt{l}", [128, 24])
            din(f"dsk{l}", [128, 6]); din(f"bglu{l}", [128, 6]); din(f"wglu{l}", [768, 768], F32R)
        if KINDS[l] == 1:
            din(f"convq{l}", [96, 4, 4]); din(f"convk{l}", [96, 4, 4]); din(f"bi{l}", [1, 4]); din(f"bf{l}", [1, 4])
        if KINDS[l] in (1, 2):
            din(f"ng{l}", [1, 768])
    din("w_out", [DEPTH, D, D], F32R)
    for n in ("ln1_g", "ln1_b", "ln2_g", "ln2_b"):
        din(n, [DEPTH, D])
    din("router_w", [DEPTH, D, NE]); din("router_b", [DEPTH, NE])
    din("exp_w_gu", [nlayers, NE, D, 2 * D], F32R)
    din("exp_b_gu_l", [nlayers, 128, NE, 16])
    din("exp_w_down", [nlayers, NE, D, D], F32R)
    din("exp_b_down", [nlayers, NE, D], F32R)
    out_t = nc.dram_tensor("out", [L, D], F32, kind="ExternalOutput")
    dscr("H1", [L, D]); dscr("H2", [L, D]); dscr("HT", [D, L], F32R)
    dscr("Xs", [NSLOT, D]); dscr("Ys", [NSLOT, D])
    dbg_t = {n: nc.dram_tensor("dbg_" + n, [L, D], F32, kind="ExternalOutput") for n in dbg}

    with ExitStack() as es:
        K = KB(nc, es)
        sb = K.sb
        ps = [es.enter_context(nc.psum_tensor(f"ps{i}", [128, 512], F32)) for i in range(6)]
        psY = es.enter_context(nc.psum_tensor("psY", [128, 1024], F32))

        def tr8(src, dst_of_hb, engs=("dve", "act")):
            for hb in range(2):
                for c4 in range(4):
                    c = hb * 4 + c4
                    K.tr(ps[hb][:, c4 * 128:(c4 + 1) * 128], src[:, c * 128:(c + 1) * 128], ident[:])
                K.copy(engs[hb], dst_of_hb(hb), ps[hb][:, :].rearrange("p (c t) -> p c t", c=4))

        ident = sb("ident", [128, 128]); stri = sb("stri", [128, 128])
        ones = sb("ones", [128, 128]); iota32 = sb("iota32", [128, 32]); ebase = sb("ebase", [128, 32])
        jota = sb("jota", [128, 128]); tri = sb("tri", [128, 128]); lfc = sb("lfc", [128, 4])
        invf = sb("invf", [96, 1]); sgn = sb("sgn", [96, 1])
        for t_, n_ in ((ident, "ident"), (stri, "stri"), (ones, "ones"), (iota32, "iota32"),
                       (ebase, "ebase"), (jota, "jota"), (tri, "tri"), (lfc, "lfc"), (invf, "invf"), (sgn, "sgn")):
            K.dma(t_[:], T["c_" + n_].ap())
        ones_r = sb("ones_r", [128, 128], F32R)
        K.copy("dve", ones_r[:], ones[:])
        kT = sb("kT", [64, 4, 256], F32R)
        vv = sb("vv", [128, 2, 256], F32R)
        gates_all = sb("gates_all", [128, NT, 4])
        dest_all = sb("dest_all", [128, NT, 4], I32)
        mask_all = sb("mask_all", [128, NT, 32])

        with ExitStack() as es2:
            def sb2(name, shape, dt=F32):
                return es2.enter_context(nc.sbuf_tensor(name, shape, dt))
            zrow = sb2("zrow", [128, 1024])
            K.memset("pool", zrow[:], 0.0)
            for r in range(0, NSLOT, 128):
                K.dma((T["Xs"][r:r + 128, :], r // CAP), zrow[:])
            memT = sb2("memT", [128, 8, 256], F32R)
            wk = sb2("wk", [128, 8, 256], F32R)
            wv = sb2("wv", [128, 8, 256], F32R)
            mt = [sb2(f"mt{i}", [128, 1024]) for i in range(2)]
            K.dma(wk[:], T["mem_w_k"].ap().rearrange("(c p) n -> p c n", p=128))
            K.dma(wv[:], T["mem_w_v"].ap().rearrange("(c p) n -> p c n", p=128))
            for m in range(2):
                K.dma(mt[m][:], T["mem"][m * 128:(m + 1) * 128, :])
                tr8(mt[m], lambda hb, m=m: memT[:, hb * 4:(hb + 1) * 4, m * 128:(m + 1) * 128])
            for h in range(4):
                for c in range(8):
                    K.mm(ps[2][0:64, 0:256], wk[:, c, 64 * h:64 * h + 64], memT[:, c, :], start=(c == 0), stop=(c == 7))
                K.copy("dve", kT[:, h, :], ps[2][0:64, 0:256])
            for m in range(2):
                for c in range(8):
                    K.mm(ps[3][:, 0:256], memT[:, c, m * 128:(m + 1) * 128], wv[:, c, :], start=(c == 0), stop=(c == 7))
                K.copy("act", vv[:, m, :], ps[3][:, 0:256])
            HTv = T["HT"].ap().rearrange("(c p) t -> p c t", p=128)
            xts = [sb2(f"xts{i}", [128, 8, 128], F32R) for i in range(2)]
            for t in range(NT):
                xt = mt[t % 2]
                K.dma(xt[:], T["x"][t * 128:(t + 1) * 128, :])
                tr8(xt, lambda hb, t=t: xts[t % 2][:, hb * 4:(hb + 1) * 4, :])
                K.dma((HTv[:, :, t * 128:(t + 1) * 128], t // TPB), xts[t % 2][:])

        if stop == "setup":
            K.S.stopped = True
        for l in range(nlayers):
            kind = KINDS[l]
            Hres = T["x"] if l == 0 else T["H2"]
            last = (l == nlayers - 1)
            with ExitStack() as esA:
                K.S.barrier()
                def sbA(name, shape, dt=F32):
                    return esA.enter_context(nc.sbuf_tensor(f"{name}_{l}", shape, dt))
                g1 = sbA("g1", [128, 1024]); b1 = sbA("b1", [128, 1024])
                K.dma(g1[:], T["ln1_g"][l:l + 1, :].partition_broadcast(128))
                K.dma(b1[:], T["ln1_b"][l:l + 1, :].partition_broadcast(128))
                rw = sbA("rw", [128, 8, 32]); rb = sbA("rb", [1, 32])
                K.dma(rw[:], T["router_w"][l].rearrange("(c p) n -> p c n", p=128))
                K.dma(rb[:], T["router_b"][l:l + 1, :])
                hTb = sbA("hTb", [128, 8, BT], F32R)
                wp = [sbA(f"wp{i}", [128, 8, 128], F32R) for i in range(3)]
                wpi = [0]
                xqT = sbA("xqT", [64, 4, BT], F32R)
                catT = sbA("catT", [128, 6, BT], F32R)
                Eb = [sbA(f"E{i}", [128, BT], F32R) for i in range(2)]
                rec = sbA("rec", [64, BT])
                hres = [sbA(f"hres{i}", [128, 1024]) for i in range(2)]
                zt = sbA("zt", [128, 1024])
                h1t = [sbA(f"h1t{i}", [128, 1024]) for i in range(2)]
                h1T = sbA("h1T", [128, 8, 128])
                stats = sbA("stats", [128, 2, 6]); mv = sbA("mv", [128, 2]); rstd = sbA("rstd", [128, 1])
                lg = sbA("lg", [128, 32]); m8 = sbA("m8", [128, 8]); i8 = sbA("i8", [128, 8], U32)
                negm = sbA("negm", [128, 1]); e4 = sbA("e4", [128, 4]); ssum = sbA("ssum", [128, 1])
                idxf = sbA("idxf", [128, 4]); ovf = sbA("ovf", [128, 32]); slot = sbA("slot", [128, 32])
                junk = sbA("junk", [128, 32]); destf = sbA("destf", [128, 4])
                w_in = T[f"w_in{l}"].ap().rearrange("(c p) n -> p c n", p=128)
                HTv = T["HT"].ap().rearrange("(c p) t -> p c t", p=128)

                def next_wp():
                    w = wp[wpi[0] % 3]
                    wpi[0] += 1
                    return w

                def proj_fm(col0, ncols, evac):
                    w = next_wp()
                    pb = ps[wpi[0] % 2]
                    K.dma(w[:, :, 0:ncols], w_in[:, :, col0:col0 + ncols])
                    for c in range(8):
                        K.mm(pb[0:ncols, 0:BT], w[:, c, 0:ncols], hTb[:, c, :], start=(c == 0), stop=(c == 7))
                    evac(pb[0:ncols, 0:BT])

                if kind == 0:
                    bblk = sbA("bblk", [128, 6, 2, 2, 128], F32R)
                    K.dma(bblk[:], T[f"bblk{l}"].ap())
                    dsk = sbA("dsk", [128, 6])
                    K.dma(dsk[:], T[f"dsk{l}"].ap())
                    bglu = sbA("bglu", [128, 6])
                    K.dma(bglu[:], T[f"bglu{l}"].ap())
                    CSC = sbA("CSC", [128, 24, 384])
                    COS = CSC[:, :, 0:128]; SIN = CSC[:, :, 128:256]
                    rmag = sbA("rmag", [128, 24]); theta = sbA("theta", [128, 24])
                    Cp = sbA("Cp", [128, 24, 64]); Cin = sbA("Cin", [128, 24, 64])
                    with ExitStack() as esP:
                        def sbP(name, shape):
                            return esP.enter_context(nc.sbuf_tensor(f"{name}_{l}", shape, F32))
                        cblk = sbP("cblk", [128, 2, 24, 64])
                        K.dma(cblk[:], T[f"cblk{l}"].ap())
                        are = sbP("are", [128, 24]); aim = sbP("aim", [128, 24]); ldt = sbP("ldt", [128, 24])
                        K.dma(are[:], T[f"are{l}"].ap()); K.dma(aim[:], T[f"aim{l}"].ap()); K.dma(ldt[:], T[f"ldt{l}"].ap())
                        lam = sbP("lam", [128, 24]); dtt = sbP("dtt", [128, 24]); tmp = sbP("tmp", [128, 24])
                        tmp2 = sbP("tmp2", [128, 24]); sn = sbP("sn", [128, 24]); cs = sbP("cs", [128, 24])
                        abr = sbP("abr", [128, 24]); abi = sbP("abi", [128, 24]); den = sbP("den", [128, 24])
                        cre = sbP("cre", [128, 24]); cim = sbP("cim", [128, 24])
                        ANG = sbP("ANG", [128, 24, 128]); KK = sbP("KK", [128, 24, 128])
                        t32a = sbP("t32a", [128, 24, 64]); t32b = sbP("t32b", [128, 24, 64])

                        def range_sin(out, ang, kk, shift):
                            K.ts("dve", kk, ang, 1.0 / TWO_PI, shift / TWO_PI, ALU.mult, ALU.add)
                            K.ts("dve", kk, kk, MAGIC, MAGIC, ALU.add, ALU.subtract)
                            K.stt(out, kk, -C1, ang, ALU.mult, ALU.add)
                            K.stt(out, kk, -C2, out, ALU.mult, ALU.add)
                            K.ts("dve", out, out, shift, None, ALU.add)
                            K.ts("dve", out, out, -PI_LO, PI_LO, ALU.max, ALU.min)
                            K.act(out, out, AF.Sin)

                        K.ts("dve", lam[:], are[:], -1e-4, None, ALU.min)
                        K.act(dtt[:], ldt[:], AF.Exp)
                        K.tt("dve", tmp[:], dtt[:], lam[:], ALU.mult)
                        K.act(rmag[:], tmp[:], AF.Exp)
                        K.tt("dve", theta[:], dtt[:], aim[:], ALU.mult)
                        range_sin(sn[:], theta[:], tmp[:], 0.0)
                        range_sin(cs[:], theta[:], tmp[:], math.pi / 2)
                        K.tt("dve", abr[:], rmag[:], cs[:], ALU.mult)
                        K.tt("dve", abi[:], rmag[:], sn[:], ALU.mult)
                        K.ts("dve", abr[:], abr[:], -1.0, None, ALU.add)
                        K.tt("dve", den[:], lam[:], lam[:], ALU.mult)
                        K.tt("dve", tmp[:], aim[:], aim[:], ALU.mult)
                        K.tt("dve", den[:], den[:], tmp[:], ALU.add)
                        K.recip(den[:], den[:])
                        K.tt("dve", tmp[:], abr[:], lam[:], ALU.mult)
                        K.tt("dve", tmp2[:], abi[:], aim[:], ALU.mult)
                        K.tt("dve", tmp[:], tmp[:], tmp2[:], ALU.add)
                        K.tt("dve", cre[:], tmp[:], den[:], ALU.mult)
                        K.tt("dve", tmp[:], abi[:], lam[:], ALU.mult)
                        K.tt("dve", tmp2[:], abr[:], aim[:], ALU.mult)
                        K.tt("dve", tmp[:], tmp[:], tmp2[:], ALU.subtract)
                        K.tt("dve", cim[:], tmp[:], den[:], ALU.mult)
                        creb = cre[:].unsqueeze(2).to_broadcast([128, 24, 64])
                        cimb = cim[:].unsqueeze(2).to_broadcast([128, 24, 64])
                        K.tt("dve", t32a[:], cblk[:, 0, :, :], creb, ALU.mult)
                        K.tt("dve", t32b[:], cblk[:, 1, :, :], cimb, ALU.mult)
                        K.tt("dve", Cp[:], t32a[:], t32b[:], ALU.subtract)
                        K.tt("dve", t32a[:], cblk[:, 0, :, :], cimb, ALU.mult)
                        K.tt("dve", t32b[:], cblk[:, 1, :, :], creb, ALU.mult)
                        K.tt("dve", t32a[:], t32a[:], t32b[:], ALU.add)
                        K.ts("dve", Cin[:], t32a[:], -1.0, None, ALU.mult)
                        for q in range(24):
                            K.ts("dve", ANG[:, q, :], jota[:], theta[:, q:q + 1], None, ALU.mult)
                        range_sin(SIN, ANG[:, :, :], KK[:, :, :], 0.0)
                        range_sin(COS, ANG[:, :, :], KK[:, :, :], math.pi / 2)
                        K.copy("dve", CSC[:, :, 256:384], COS)

                    K.S.barrier()
                    if stop == f"P{l}":
                        K.S.stopped = True
                    uT = catT
                    ygT = sbA("ygT", [128, 6, BT], F32R)
                    ygT_f = ygT.bitcast(F32)
                    car_re = sbA("car_re", [128, 24]); car_im = sbA("car_im", [128, 24])
                    K.memset("pool", car_re[:], 0.0); K.memset("pool", car_im[:], 0.0)
                    NBUF = 4
                    mk = lambda n, w_: [sbA(f"{n}_{i}", [128, w_]) for i in range(NBUF)]
                    T12, T43, RR, WW, VAC, VBD, XX = [mk(n, 256) for n in ("T12", "T43", "RR", "WW", "VAC", "VBD", "XX")]
                    ytok = sbA("ytok", [128, 768]); yx2 = sbA("yx2", [128, 768])
                    sgl = [sbA(f"sgl{i}", [128, BT]) for i in range(2)]
                    wgluv = T[f"wglu{l}"].ap().rearrange("(c p) n -> p c n", p=128)


                if kind in (1, 2):
                    LNS = math.log(96.0 ** -0.5)
                    qT = sbA("qT", [96, 4, BT], F32R); kTt = sbA("kTt", [96, 4, BT], F32R)
                    qT_f = qT.bitcast(F32); kTt_f = kTt.bitcast(F32)
                    vt = [sbA(f"vt{i}", [128, 4, 194], F32R) for i in range(2)]
                    for v_ in vt:
                        K.memset("pool", v_.bitcast(F32)[:], 0.0)
                        K.copy("dve", v_[:, :, 192:193], ones[:, 0:4].unsqueeze(2))
                    clns = sbA("clns", [128, 1])
                    K.memset("pool", clns[:], LNS)
                    gt = [sbA(f"gt{i}", [128, 768]) for i in range(2)]
                    gps = sbA("gps", [128, 8]); lf = sbA("lf", [128, 4]); igb = sbA("igb", [128, 4]); bcol = sbA("bcol", [128, 4])
                    bias1 = sbA("bias1", [128, 4]); bias2 = sbA("bias2", [128, 4])
                    rhsB = [sbA(f"rhsB{i}", [128, 128]) for i in range(4)]
                    DT = [sbA(f"DT{i}", [128, 128]) for i in range(4)]
                    SD = [sbA(f"SD{i}", [128, 128], F32R) for i in range(4)]
                    EBt = [sbA(f"EBt{i}", [96, 128]) for i in range(4)]
                    qs = [sbA(f"qs{i}", [96, 128], F32R) for i in range(4)]
                    kw = [sbA(f"kw{i}", [128, 96], F32R) for i in range(4)]
                    wvv = [sbA(f"wvv{i}", [128, 1]) for i in range(4)]
                    ebt = [sbA(f"ebt{i}", [96, 1]) for i in range(4)]
                    Cst = [sbA(f"Cst{i}", [96, 4, 194], F32R) for i in range(2)]
                    Cst_f = [c_.bitcast(F32) for c_ in Cst]
                    K.memset("pool", Cst_f[0][:], 0.0); K.memset("pool", Cst_f[1][:], 0.0)
                    hN = [sbA(f"hN{i}", [128, 192]) for i in range(4)]; dn = [sbA(f"dn{i}", [128, 1]) for i in range(4)]
                    hst = [sbA(f"hst{i}", [128, 6]) for i in range(4)]; hmv = [sbA(f"hmv{i}", [128, 2]) for i in range(4)]
                    hrs = [sbA(f"hrs{i}", [128, 1]) for i in range(4)]
                    ymx = sbA("ymx", [128, 768]); gsg = sbA("gsg", [128, 768])
                    ngb = sbA("ngb", [128, 768])
                    K.dma(ngb[:], T[f"ng{l}"].ap().partition_broadcast(128))
                    if kind == 1:
                        qpre = sbA("qpre", [96, 4, 3 + BT]); kpre = sbA("kpre", [96, 4, 3 + BT])
                        K.memset("pool", qpre[:], 0.0); K.memset("pool", kpre[:], 0.0)
                        cacc = sbA("cacc", [96, BT])
                        convq = sbA("convq", [96, 4, 4]); convk = sbA("convk", [96, 4, 4])
                        K.dma(convq[:], T[f"convq{l}"].ap()); K.dma(convk[:], T[f"convk{l}"].ap())
                        bib = sbA("bib", [128, 4]); bfb = sbA("bfb", [128, 4])
                        K.dma(bib[:], T[f"bi{l}"].ap().partition_broadcast(128))
                        K.dma(bfb[:], T[f"bf{l}"].ap().partition_broadcast(128))
                    else:
                        RC = sbA("RC", [96, L]); RS = sbA("RS", [96, L])
                        qraw = sbA("qraw", [96, BT]); qsw = sbA("qsw", [96, BT])
                        with ExitStack() as esR:
                            posb = esR.enter_context(nc.sbuf_tensor(f"posb_{l}", [96, L], I32))
                            posf = esR.enter_context(nc.sbuf_tensor(f"posf_{l}", [96, L], F32))
                            kkr = esR.enter_context(nc.sbuf_tensor(f"kkr_{l}", [96, L], F32))
                            K.dma(posb[:], T["pos"].ap().partition_broadcast(96))
                            K.copy("dve", posf[:], posb[:])
                            K.ts("dve", posf[:], posf[:], invf[:, 0:1], None, ALU.mult)

                            def range_sin2(out, ang, kk, shift):
                                K.ts("dve", kk, ang, 1.0 / TWO_PI, shift / TWO_PI, ALU.mult, ALU.add)
                                K.ts("dve", kk, kk, MAGIC, MAGIC, ALU.add, ALU.subtract)
                                K.stt(out, kk, -C1, ang, ALU.mult, ALU.add)
                                K.stt(out, kk, -C2, out, ALU.mult, ALU.add)
                                K.ts("dve", out, out, shift, None, ALU.add)
                                K.ts("dve", out, out, -PI_LO, PI_LO, ALU.max, ALU.min)
                                K.act(out, out, AF.Sin)
                            range_sin2(RS[:], posf[:], kkr[:], 0.0)
                            range_sin2(RC[:], posf[:], kkr[:], math.pi / 2)
                            K.ts("dve", RS[:], RS[:], sgn[:, 0:1], None, ALU.mult)
                        K.S.barrier()

                for tb in range(NBLK):
                    tsl = slice(tb * BT, (tb + 1) * BT)
                    K.dma(hTb[:], HTv[:, :, tsl])
                    xq0 = N_IN[l] - 256
                    for h in range(4):
                        proj_fm(xq0 + 64 * h, 64, lambda p, h=h: K.copy("act", xqT[:, h, :], p))
                    if stop == "J0":
                        K.S.stopped = True
                    if kind == 0:
                        for c in range(6):
                            proj_fm(128 * c, 128, lambda p, c=c: K.copy("dve" if c % 2 else "act", uT[:, c, :], p))
                        for s in range(TPB):
                            ssl = slice(s * 128, (s + 1) * 128)

                            banks = [ps[2], ps[5], ps[3], ps[4]]

                            def bu(q):
                                c, r0 = q // 4, 64 * ((q % 4) // 2)
                                pq = banks[q % 4]
                                K.mm(pq[:, 0:128], bblk[r0:r0 + 64, c, 0, q % 2, :], uT[r0:r0 + 64, c, ssl])
                                K.mm(pq[:, 128:256], bblk[r0:r0 + 64, c, 1, q % 2, :], uT[r0:r0 + 64, c, ssl])
                            for q in range(4):
                                bu(q)
                            for g in range(6):
                                qs_ = range(4 * g, 4 * g + 4)
                                c = g
                                for q in qs_:
                                    K.tt("dve", T12[q % 4][:], banks[q % 4][:, 0:256], CSC[:, q, 0:256], ALU.mult)
                                for q in qs_:
                                    K.tt("dve", T43[q % 4][:], banks[q % 4][:, 0:256], CSC[:, q, 128:384], ALU.mult)
                                if g + 1 < 6:
                                    for q in range(4 * g + 4, 4 * g + 8):
                                        bu(q)
                                for q in qs_:
                                    b = q % 4
                                    K.tt("dve", RR[b][:, 0:128], T12[b][:, 0:128], T12[b][:, 128:256], ALU.add)
                                for q in qs_:
                                    b = q % 4
                                    K.tt("dve", RR[b][:, 128:256], T43[b][:, 128:256], T43[b][:, 0:128], ALU.subtract)
                                for q in qs_:
                                    b = q % 4
                                    K.scan(WW[b][:, 0:128], rmag[:, q:q + 1].to_broadcast([128, 128]), RR[b][:, 0:128], (car_re[:, q:q + 1], q))
                                for q in qs_:
                                    b = q % 4
                                    K.scan(WW[b][:, 128:256], rmag[:, q:q + 1].to_broadcast([128, 128]), RR[b][:, 128:256], (car_im[:, q:q + 1], q))
                                for q in qs_:
                                    b = q % 4
                                    K.tt("dve", VAC[b][:].rearrange("p (a b) -> p a b", a=2),
                                         WW[b][:, 0:128].unsqueeze(1).to_broadcast([128, 2, 128]),
                                         CSC[:, q, 0:256].rearrange("p (a b) -> p a b", a=2), ALU.mult)
                                for q in qs_:
                                    b = q % 4
                                    K.tt("dve", VBD[b][:].rearrange("p (a b) -> p a b", a=2),
                                         WW[b][:, 128:256].unsqueeze(1).to_broadcast([128, 2, 128]),
                                         CSC[:, q, 128:384].rearrange("p (a b) -> p a b", a=2), ALU.mult)
                                for q in qs_:
                                    b = q % 4
                                    K.tt("pool", XX[b][:, 0:128], VAC[b][:, 0:128], VBD[b][:, 0:128], ALU.subtract)
                                for q in qs_:
                                    b = q % 4
                                    K.tt("pool", XX[b][:, 128:256], VAC[b][:, 128:256], VBD[b][:, 128:256], ALU.add)
                                for q in qs_:
                                    b = q % 4
                                    K.copy("pool", (car_re[:, q:q + 1], q), XX[b][:, 127:128])
                                    K.copy("pool", (car_im[:, q:q + 1], q), XX[b][:, 255:256])
                                for q in qs_:
                                    b = q % 4
                                    hq = (q % 4) // 2
                                    yo = (psY[64 * hq:64 * hq + 64, 128 * c:128 * c + 128], c // 4)
                                    K.mm(yo, Cp[:, q, :], XX[b][:, 0:128], start=(q % 2 == 0), stop=False, sgc=True)
                                    K.mm(yo, Cin[:, q, :], XX[b][:, 128:256], start=False, stop=(q % 2 == 1), sgc=True)
                            uT_f = uT.bitcast(F32)
                            for c in range(6):
                                K.stt((ytok[:, 128 * c:128 * c + 128], c), uT_f[:, c, ssl], dsk[:, c:c + 1],
                                      (psY[:, 128 * c:128 * c + 128], c // 4), ALU.mult, ALU.add)
                            K.act(yx2[:], ytok[:], AF.Square)
                            K.ts("dve", yx2[:], yx2[:], 0.044715, 1.0, ALU.mult, ALU.add)
                            K.tt("dve", yx2[:], yx2[:], ytok[:], ALU.mult)
                            K.act(yx2[:], yx2[:], AF.Sigmoid, scale=1.5957691216057308)
                            K.tt("dve", ygT[:, :, ssl], ytok[:].rearrange("p (c t) -> p c t", c=6),
                                 yx2[:].rearrange("p (c t) -> p c t", c=6), ALU.mult)
                        if stop == "S0":
                            K.S.stopped = True
                        for j in range(6):
                            w = next_wp()
                            pb = ps[j % 2]
                            K.dma(w[:, 0:6, :], wgluv[:, :, 128 * j:128 * j + 128])
                            for c in range(6):
                                K.mm(pb[:, 0:BT], w[:, c, :], ygT[:, c, :], start=(c == 0), stop=(c == 5))
                            K.act(sgl[j % 2][:], pb[:, 0:BT], AF.Sigmoid, bias=bglu[:, j:j + 1])
                            K.tt("dve", catT[:, j, :], ygT_f[:, j, :], sgl[j % 2][:], ALU.mult)
                    else:
                        for h in range(4):
                            if kind == 1:
                                proj_fm(96 * h, 96, lambda p, h=h: K.copy("act", qpre[:, h, 3:3 + BT], p))
                                proj_fm(384 + 96 * h, 96, lambda p, h=h: K.copy("act", kpre[:, h, 3:3 + BT], p))
                                for (pre, cw, dstT) in ((qpre, convq, qT), (kpre, convk, kTt)):
                                    K.ts("dve", cacc[:], pre[:, h, 3:3 + BT], cw[:, h, 3:4], None, ALU.mult)
                                    for w_ in (2, 1, 0):
                                        K.stt(cacc[:], pre[:, h, w_:w_ + BT], cw[:, h, w_:w_ + 1], cacc[:], ALU.mult, ALU.add)
                                    K.act(dstT[:, h, :], cacc[:], AF.Silu)
                            else:
                                for (c0_, dstT, dst_f) in ((0, qT, qT_f), (384, kTt, kTt_f)):
                                    proj_fm(c0_ + 96 * h, 96, lambda p: K.copy("act", qraw[:], p))
                                    w = next_wp()
                                    pb = ps[wpi[0] % 2]
                                    K.dma(w[:, :, 0:48], w_in[:, :, c0_ + 96 * h + 48:c0_ + 96 * h + 96])
                                    K.dma(w[:, :, 48:96], w_in[:, :, c0_ + 96 * h:c0_ + 96 * h + 48])
                                    for c in range(8):
                                        K.mm(pb[0:96, 0:BT], w[:, c, 0:96], hTb[:, c, :], start=(c == 0), stop=(c == 7))
                                    K.tt("dve", qsw[:], pb[0:96, 0:BT], RS[:, tsl], ALU.mult)
                                    K.tt("pool", qraw[:], qraw[:], RC[:, tsl], ALU.mult)
                                    K.tt("dve", dstT[:, h, :], qraw[:], qsw[:], ALU.add)
                        if kind == 1:
                            K.copy("pool", qpre[:, :, 0:3], qpre[:, :, BT:BT + 3])
                            K.copy("pool", kpre[:, :, 0:3], kpre[:, :, BT:BT + 3])
                        for pc in range(12):
                            w = next_wp()
                            K.dma(w[:], w_in[:, :, 768 + 128 * pc:768 + 128 * pc + 128])
                            for j in range(TPB):
                                pb = ps[(pc * TPB + j) % 2]
                                for c in range(8):
                                    K.mm(pb[:, 0:128], hTb[:, c, j * 128:(j + 1) * 128], w[:, c, :], start=(c == 0), stop=(c == 7))
                                if pc < 6:
                                    n0 = 128 * pc
                                    while n0 < 128 * pc + 128:
                                        hh_ = n0 // 192
                                        n1 = min(128 * pc + 128, 192 * (hh_ + 1))
                                        K.copy("act" if (n0 // 64) % 2 else "dve", vt[j][:, hh_, n0 - 192 * hh_:n1 - 192 * hh_],
                                               pb[:, n0 - 128 * pc:n1 - 128 * pc])
                                        n0 = n1
                                else:
                                    K.copy("act", gt[j][:, 128 * (pc - 6):128 * (pc - 6) + 128], pb[:, 0:128])
                        if kind == 1:
                            wg_ = next_wp()
                            K.dma(wg_[:, :, 0:8], w_in[:, :, 2304:2312])
                        for j in range(TPB):
                            csl = slice(j * 128, (j + 1) * 128)
                            if kind == 1:
                                for c in range(8):
                                    K.mm(ps[2][:, 0:8], hTb[:, c, csl], wg_[:, c, 0:8], start=(c == 0), stop=(c == 7))
                                K.copy("dve", gps[:], ps[2][:, 0:8])
                                K.tt("dve", igb[:], gps[:, 0:4], bib[:], ALU.add)
                                K.tt("dve", lf[:], gps[:, 4:8], bfb[:], ALU.add)
                                K.act(lf[:], lf[:], AF.Exp, scale=-1.0)
                                K.act(lf[:], lf[:], AF.Ln, bias=ones[:, 0:1])
                                K.ts("dve", lf[:], lf[:], -1.0, None, ALU.mult)
                                lfx = lf
                            else:
                                lfx = lfc
                            K.mm(ps[2][:, 0:4], tri[:], lfx[:], start=True, stop=True)
                            if kind == 1:
                                K.tt("dve", bias2[:], igb[:], ps[2][:, 0:4], ALU.subtract)
                                K.ts("dve", bias1[:], bias2[:], LNS, None, ALU.add)
                            else:
                                K.ts("dve", bias1[:], ps[2][:, 0:4], -1.0, LNS, ALU.mult, ALU.add)
                                K.copy("dve", bias2[:], bias1[:])
                            cur = Cst[(tb * TPB + j) % 2]; nxt = Cst[(tb * TPB + j + 1) % 2]
                            cur_f = Cst_f[(tb * TPB + j) % 2]
                            H4 = range(4)
                            for h in H4:
                                K.ts("dve", rhsB[h][:], tri[:], lfx[:, h:h + 1], None, ALU.mult)
                            for h in H4:
                                K.mm(ps[3][:, 128 * h:128 * h + 128], ones[:], rhsB[h][:])
                            for h in H4:
                                K.act(DT[h][:], ps[3][:, 128 * h:128 * h + 128], AF.Exp, bias=bias1[:, h:h + 1])
                            for h in H4:
                                K.act(EBt[h][:], ps[3][0:96, 128 * h:128 * h + 128], AF.Exp, bias=(clns[0:96, 0:1] if kind == 1 else 0.0))
                            for h in H4:
                                K.act(wvv[h][:], ps[3][:, 128 * h + 127:128 * h + 128], AF.Exp, bias=bias2[:, h:h + 1])
                                K.act(ebt[h][:], ps[3][0:96, 128 * h + 127:128 * h + 128], AF.Exp)
                            for h in H4:
                                K.tr(ps[4][:, 96 * h:96 * h + 96], kTt_f[:, h, csl], ident[0:96, 0:96])
                            for h in H4:
                                K.mm(ps[5][:, 128 * h:128 * h + 128], kTt[:, h, csl], qT[:, h, csl])
                            for h in H4:
                                K.tt("dve", DT[h][:], DT[h][:], tri[:], ALU.mult)
                            for h in H4:
                                K.tt("dve", qs[h][:], qT_f[:, h, csl], EBt[h][:], ALU.mult)
                            for h in H4:
                                K.ts("dve", kw[h][:], ps[4][:, 96 * h:96 * h + 96], wvv[h][:, 0:1], None, ALU.mult)
                            for h in H4:
                                K.tt("dve", SD[h][:], ps[5][:, 128 * h:128 * h + 128], DT[h][:], ALU.mult)
                            psN = [(psY[:, 512 * (h // 2) + 256 * (h % 2):512 * (h // 2) + 256 * (h % 2) + 194], h // 2) for h in H4]
                            psS = [ps[h // 2][0:96, 256 * (h % 2):256 * (h % 2) + 194] for h in H4]
                            for h in H4:
                                K.mm(psN[h], SD[h][:], vt[j][:, h, :], start=True, stop=False, sgc=True)
                                K.mm(psN[h], qs[h][:], (cur[:, h, :], h), start=False, stop=True, sgc=True)
                            for h in H4:
                                K.mm(psS[h], kw[h][:], vt[j][:, h, :], start=True, stop=True, sgc=True)
                            for h in H4:
                                K.stt((nxt[:, h, :], h), (cur_f[:, h, :], h), ebt[h][:, 0:1], psS[h], ALU.mult, ALU.add)
                            if kind == 1:
                                for h in H4:
                                    K.act(dn[h][:], (psN[h][0][:, 192:193], h // 2), AF.Abs)
                                for h in H4:
                                    K.ts("dve", dn[h][:], dn[h][:], 1.0, None, ALU.max)
                                for h in H4:
                                    K.recip(dn[h][:], dn[h][:])
                                for h in H4:
                                    K.ts("dve", hN[h][:], (psN[h][0][:, 0:192], h // 2), dn[h][:, 0:1], None, ALU.mult)
                            else:
                                for h in H4:
                                    K.copy("act" if h % 2 else "dve", hN[h][:], (psN[h][0][:, 0:192], h // 2))
                            for h in H4:
                                K.generic("dve", lambda e, a=hst[h], b_=hN[h]: e.bn_stats(out=a[:], in_=b_[:]), [hN[h][:]], [hst[h][:]])
                            for h in H4:
                                K.generic("dve", lambda e, a=hst[h], b_=hmv[h]: e.bn_aggr(out=b_[:], in_=a[:]), [hst[h][:]], [hmv[h][:]])
                            for h in H4:
                                K.ts("dve", hrs[h][:], hmv[h][:, 1:2], EPS, None, ALU.add)
                            for h in H4:
                                K.act(hrs[h][:], hrs[h][:], AF.Sqrt)
                            for h in H4:
                                K.recip(hrs[h][:], hrs[h][:])
                            for h in H4:
                                K.ts("dve", (ymx[:, 192 * h:192 * h + 192], h), hN[h][:], hmv[h][:, 0:1], hrs[h][:, 0:1], ALU.subtract, ALU.mult)
                            K.tt("dve", ymx[:], ymx[:], ngb[:], ALU.mult)
                            K.act(gsg[:], gt[j][:], AF.Sigmoid if kind == 1 else AF.Silu)
                            K.tt("dve", ymx[:], ymx[:], gsg[:], ALU.mult)
                            for c in range(6):
                                pb = ps[c // 4]
                                K.tr(pb[:, (c % 4) * 128:(c % 4) * 128 + 128], ymx[:, 128 * c:128 * c + 128], ident[:])
                            K.copy("act", catT[:, 0:4, csl], ps[0][:, :].rearrange("p (c t) -> p c t", c=4))
                            K.copy("dve", catT[:, 4:6, csl], ps[1][:, 0:256].rearrange("p (c t) -> p c t", c=2))

                    if stop == "G0":
                        K.S.stopped = True
                    for h in range(4):
                        for m in range(2):
                            K.mm(ps[3 + m][:, 0:BT], kT[:, h, m * 128:(m + 1) * 128], xqT[:, h, :])
                            K.act(Eb[m][:], ps[3 + m][:, 0:BT], AF.Exp, scale=0.125)
                        for m in range(2):
                            K.mm(ps[5][0:64, 0:BT], vv[:, m, 64 * h:64 * h + 64], Eb[m][:], start=(m == 0), stop=(m == 1))
                        for m in range(2):
                            K.mm(ps[2][0:64, 0:BT], ones_r[:, 0:64], Eb[m][:], start=(m == 0), stop=(m == 1))
                        K.recip(rec[:], ps[2][0:64, 0:BT])
                        K.tt("dve", xqT[:, h, :], ps[5][0:64, 0:BT], rec[:], ALU.mult)
                    ymT = xqT

                    if stop == "X0":
                        K.S.stopped = True
                    accs = [(psY[:, 0:512], 0), (psY[:, 512:1024], 1), ps[3][:, :], ps[4][:, :]]
                    for c in range(10):
                        w = next_wp()
                        wv_ = w[:, :, :].rearrange("p a b -> p (a b)")
                        if c < 6:
                            K.dma(w[:], T["w_out"][l, 128 * c:128 * c + 128, :].rearrange("p (a b) -> p a b", b=128))
                        else:
                            hq = c - 6
                            K.generic_dma = None
                            K.dma((w[0:64, :, :], None), T["w_out"][l, 768 + 64 * hq:768 + 64 * hq + 64, :].rearrange("p (a b) -> p a b", b=128))
                        for j in range(TPB):
                            jsl = slice(j * 128, (j + 1) * 128)
                            for hh in range(2):
                                a = accs[j * 2 + hh]
                                if c < 6:
                                    K.mm(a, catT[:, c, jsl], wv_[:, hh * 512:(hh + 1) * 512], start=(c == 0), stop=False)
                                else:
                                    K.mm(a, ymT[:, c - 6, jsl], wv_[0:64, hh * 512:(hh + 1) * 512], start=False, stop=(c == 9))
                    if stop == "O0":
                        K.S.stopped = True
                    for j in range(TPB):
                        ti = tb * TPB + j
                        hr = hres[ti % 2]; z = zt; h1 = h1t[ti % 2]
                        K.dma(hr[:], (Hres[ti * 128:(ti + 1) * 128, :], ti))
                        for hh in range(2):
                            K.stt((z[:, hh * 512:(hh + 1) * 512], hh), hr[:, hh * 512:(hh + 1) * 512], ALPHA, accs[j * 2 + hh], ALU.mult, ALU.add)
                        layer_norm(K, z, h1, g1, b1, stats, mv, rstd, eng=("pool" if kind == 0 else "dve"))
                        K.dma((T["H1"][ti * 128:(ti + 1) * 128, :], ti), h1[:], q="act")
                        if stop == "L0":
                            K.S.stopped = True
                        tr8(h1, lambda hb: h1T[:, hb * 4:(hb + 1) * 4, :])
                        for c in range(8):
                            K.mm(ps[5][:, 0:32], h1T[:, c, :], rw[:, c, :], start=(c == 0), stop=False)
                        K.mm(ps[5][:, 0:32], ones[0:1, :], rb[0:1, :], start=False, stop=True)
                        K.copy("dve", lg[:], ps[5][:, 0:32])
                        K.generic("dve", lambda e, m8=m8, lg=lg: e.max(out=m8[:], in_=lg[:]), [lg[:]], [m8[:]])
                        K.generic("dve", lambda e, m8=m8, lg=lg, i8=i8: e.max_index(out=i8[:], in_max=m8[:], in_values=lg[:]), [lg[:], m8[:]], [i8[:]])
                        K.ts("dve", negm[:], m8[:, 0:1], -1.0, None, ALU.mult)
                        K.act(e4[:], m8[:, 0:4], AF.Exp, bias=negm[:], accum_out=ssum[:])
                        K.recip(ssum[:], ssum[:])
                        K.ts("dve", (gates_all[:, ti, :], ti), e4[:], ssum[:], None, ALU.mult)
                        K.copy("dve", idxf[:], i8[:, 0:4])
                        K.ts("dve", (mask_all[:, ti, :], ti), lg[:], m8[:, 3:4], None, ALU.is_ge)
                        for t2_ in range(ti):
                            K.mm(ps[2][:, 0:32], ones[:], (mask_all[:, t2_, :], t2_), start=(t2_ == 0), stop=False)
                        K.mm(ps[2][:, 0:32], stri[:], (mask_all[:, ti, :], ti), start=(ti == 0), stop=True)
                        K.ts("dve", ovf[:], ps[2][:, 0:32], float(CAP), 1.0e6, ALU.is_ge, ALU.mult)
                        K.tt("dve", slot[:], ps[2][:, 0:32], ebase[:], ALU.add)
                        K.tt("dve", slot[:], slot[:], ovf[:], ALU.add)
                        for k in range(4):
                            K.stt(junk[:], iota32[:], idxf[:, k:k + 1], slot[:], ALU.is_equal, ALU.mult, accum_out=(destf[:, k:k + 1], k))
                        K.generic("dve", lambda e, ti=ti, destf=destf: e.tensor_copy(out=dest_all[:, ti, :], in_=destf[:]),
                                  [(destf[:, k:k + 1], k) for k in range(4)], [(dest_all[:, ti, :], ti)])
                        if stop == "R0":
                            K.S.stopped = True
                        for k in range(4):
                            def sc(e, ti=ti, k=k, h1=h1):
                                return e.indirect_dma_start(
                                    out=T["Xs"].ap(), out_offset=bass.IndirectOffsetOnAxis(ap=dest_all[:, ti, k:k + 1], axis=0),
                                    in_=h1[:], in_offset=None, bounds_check=REG["bc"], oob_is_err=False)
                            K.S.add("pool", sc, [_rk(h1[:]), _rk((dest_all[:, ti, :], ti))], [],
                                    [("Xs", e_) for e_ in range(NE)], dma=True)
            if stop == f"A{l}":
                K.S.stopped = True

            with ExitStack() as esB:
                K.S.barrier()
                def sbB(name, shape, dt=F32):
                    return esB.enter_context(nc.sbuf_tensor(f"{name}_{l}", shape, dt))
                XT = [sbB(f"XT{i}", [128, 8, CAP], F32R) for i in range(2)]
                xs = [sbB(f"xs{i}", [128, 1024]) for i in range(2)]
                NWG = 6
                wg = [sbB(f"wg{i}", [128, 8, 256], F32R) for i in range(NWG)]
                NWD = 10
                wd = [sbB(f"wd{i}", [128, 1024], F32R) for i in range(NWD)]
                bd = [sbB(f"bd{i}", [1, 1024], F32R) for i in range(2)]
                actT = sbB("actT", [128, 8, CAP], F32R)
                bgu = sbB("bgu", [128, NE, 16])
                K.dma(bgu[:], T["exp_b_gu_l"][l])
                K.ts("dve", bgu[:, :, 8:16], bgu[:, :, 8:16], 1.0, None, ALU.add)
                gg, sg, ll = [[sbB(f"{n}{i}", [128, CAP]) for i in range(2)] for n in ("gg", "sg", "ll")]
                yt = [sbB(f"yt{i}", [128, 1024]) for i in range(2)]
                xi = 0; gi_ = 0; yi = 0; wdi = 0
                def load_xt(e2):
                    nonlocal_xi = xi_box
                    X2 = XT[e2 % 2]
                    for t in range(3):
                        x_ = xs[nonlocal_xi[0] % 2]; nonlocal_xi[0] += 1
                        r0 = e2 * CAP + t * 128
                        K.dma(x_[:], (T["Xs"][r0:r0 + 128, :], e2))
                        tr8(x_, lambda hb, X2=X2, t=t: X2[:, hb * 4:(hb + 1) * 4, t * 128:(t + 1) * 128])
                xi_box = [0]
                load_xt(0)
                for e_ in range(NE):
                    X = XT[e_ % 2]
                    wgu = T["exp_w_gu"][l, e_].rearrange("(c p) n -> p c n", p=128)
                    for jj in range(4):
                        wG = wg[gi_ % NWG]; gi_ += 1
                        wL = wg[gi_ % NWG]; gi_ += 1
                        K.dma(wG[:], wgu[:, :, 256 * jj:256 * jj + 256])
                        K.dma(wL[:], wgu[:, :, 1024 + 256 * jj:1024 + 256 * jj + 256])
                        for j2 in range(2):
                            j = 2 * jj + j2
                            pG = ps[2 + 2 * j2]; pL = ps[3 + 2 * j2]
                            for c in range(8):
                                K.mm(pG[:, 0:CAP], wG[:, c, 128 * j2:128 * j2 + 128], X[:, c, :], start=(c == 0), stop=(c == 7))
                            for c in range(8):
                                K.mm(pL[:, 0:CAP], wL[:, c, 128 * j2:128 * j2 + 128], X[:, c, :], start=(c == 0), stop=(c == 7))
                            b = j2
                            K.ts("dve", gg[b][:], pG[:, 0:CAP], bgu[:, e_, j:j + 1], 7.0, ALU.add, ALU.min)
                            K.act(sg[b][:], gg[b][:], AF.Silu, scale=1.702)
                            K.act(ll[b][:], pL[:, 0:CAP], AF.Identity, bias=bgu[:, e_, 8 + j:9 + j])
                            K.ts("dve", ll[b][:], ll[b][:], 8.0, -6.0, ALU.min, ALU.max)
                            K.stt((actT[:, j, :], j), sg[b][:], 1.0 / 1.702, ll[b][:], ALU.mult, ALU.mult)
                    wds = []
                    for j in range(8):
                        w = wd[wdi % NWD]; wdi += 1
                        wds.append(w)
                        K.dma(w[:], T["exp_w_down"][l, e_, 128 * j:128 * j + 128, :])
                    K.dma(bd[e_ % 2][:], T["exp_b_down"][l, e_:e_ + 1, :])
                    if e_ + 1 < NE:
                        load_xt(e_ + 1)
                    for t in range(3):
                        y_ = yt[yi % 2]; yi += 1
                        for hh in range(2):
                            pb = (psY[:, hh * 512:(hh + 1) * 512], hh)
                            for j in range(8):
                                K.mm(pb, (actT[:, j, t * 128:(t + 1) * 128], j), wds[j][:, hh * 512:(hh + 1) * 512],
                                     start=(j == 0), stop=False)
                            K.mm(pb, ones_r[0:1, :], bd[e_ % 2][0:1, hh * 512:(hh + 1) * 512], start=False, stop=True)
                            K.copy("act" if hh else "dve", (y_[:, hh * 512:(hh + 1) * 512], hh), pb)
                        r0 = e_ * CAP + t * 128
                        K.S.add("act", (lambda e, r0=r0, y_=y_: e.dma_start(out=T["Ys"][r0:r0 + 128, :], in_=y_[:])),
                                [_rk(y_[:])], [], [("Ys", None)], dma=True)
            if stop == f"B{l}":
                K.S.stopped = True

            with ExitStack() as esC:
                K.S.barrier()
                def sbC(name, shape, dt=F32):
                    return esC.enter_context(nc.sbuf_tensor(f"{name}_{l}", shape, dt))
                g2 = sbC("g2", [128, 1024]); b2 = sbC("b2", [128, 1024])
                K.dma(g2[:], T["ln2_g"][l:l + 1, :].partition_broadcast(128))
                K.dma(b2[:], T["ln2_b"][l:l + 1, :].partition_broadcast(128))
                yg = [sbC(f"yg{i}", [128, 1024]) for i in range(8)]
                for y_ in yg:
                    K.memset("pool", y_[:], 0.0)
                hr2 = [sbC(f"hr2{i}", [128, 1024]) for i in range(2)]
                macc = [sbC(f"macc{i}", [128, 1024]) for i in range(2)]
                zt2 = [sbC(f"zt2{i}", [128, 1024]) for i in range(2)]
                h2t = [sbC(f"h2t{i}", [128, 1024]) for i in range(2)]
                hT2 = [sbC(f"hT2{i}", [128, 8, 128], F32R) for i in range(2)]
                stats = sbC("stats2", [128, 2, 6]); mv = sbC("mv2", [128, 2]); rstd = sbC("rstd2", [128, 1])
                HTv = T["HT"].ap().rearrange("(c p) t -> p c t", p=128)
                for ti in range(NT):
                    hr = hr2[ti % 2]; m_ = macc[ti % 2]; z = zt2[ti % 2]; h2 = h2t[ti % 2]
                    K.dma(hr[:], (T["H1"][ti * 128:(ti + 1) * 128, :], ti))
                    for k in range(4):
                        y_ = yg[(ti % 2) * 4 + k]

                        def ga(e, ti=ti, k=k, y_=y_):
                            return e.indirect_dma_start(
                                out=y_[:], out_offset=None, in_=T["Ys"].ap(),
                                in_offset=bass.IndirectOffsetOnAxis(ap=dest_all[:, ti, k:k + 1], axis=0),
                                bounds_check=REG["bc"], oob_is_err=False)
                        K.S.add("pool", ga, [("Ys", None), _rk((dest_all[:, ti, :], ti))], [_rk(y_[:])], [], dma=True)
                        if k == 0:
                            K.ts("dve", m_[:], y_[:], (gates_all[:, ti, 0:1], ti), None, ALU.mult)
                        else:
                            K.stt(m_[:], y_[:], (gates_all[:, ti, k:k + 1], ti), m_[:], ALU.mult, ALU.add)
                    K.stt(z[:], hr[:], ALPHA, m_[:], ALU.mult, ALU.add)
                    layer_norm(K, z, h2, g2, b2, stats, mv, rstd, keyed=False, eng="dve")
                    dst = out_t if last else T["H2"]
                    K.dma((dst[ti * 128:(ti + 1) * 128, :], ti), h2[:], q="act")
                    if not last:
                        hx = hT2[ti % 2]
                        tr8(h2, lambda hb, hx=hx: hx[:, hb * 4:(hb + 1) * 4, :])
                        K.dma((HTv[:, :, ti * 128:(ti + 1) * 128], ti // TPB), hx[:], q="act")

        K.S.stopped = False
        K.S.barrier()
        for n, t_ in dbg_t.items():
            for ti in range(NT):
                K.dma((t_[ti * 128:(ti + 1) * 128, :], ti), (T[n][ti * 128:(ti + 1) * 128, :], ti))
        K.S.emit()
    return nc


def make_in_maps(inputs, nlayers=DEPTH, cores=range(8)):
    f = lambda a: np.ascontiguousarray(a, dtype=np.float32)
    shared = {}
    shared["mem_w_k"] = f(inputs["mem_w_k"]); shared["mem_w_v"] = f(inputs["mem_w_v"])
    for k, v in host_consts().items():
        shared["c_" + k] = v
    for l in range(nlayers):
        shared[f"w_in{l}"] = f(inputs[f"l{l}_w_in"])
        if KINDS[l] == 0:
            p = {n: np.asarray(inputs[f"l{l}_s5_{n}"]) for n in
                 ("a_re", "a_im", "log_dt", "b_re", "b_im", "c_re", "c_im", "d", "w_glu", "b_glu")}
            for k, v in s5_layouts(p).items():
                shared[f"{k}{l}"] = f(v)
        if KINDS[l] == 1:
            cq = np.asarray(inputs[f"l{l}_ml_conv_q"]).reshape(4, 4, 96)
            ck = np.asarray(inputs[f"l{l}_ml_conv_k"]).reshape(4, 4, 96)
            shared[f"convq{l}"] = f(cq.transpose(2, 1, 0)); shared[f"convk{l}"] = f(ck.transpose(2, 1, 0))
            shared[f"bi{l}"] = f(np.asarray(inputs[f"l{l}_ml_b_i"]).reshape(1, 4))
            shared[f"bf{l}"] = f(np.asarray(inputs[f"l{l}_ml_b_f"]).reshape(1, 4))
            shared[f"ng{l}"] = f(np.asarray(inputs[f"l{l}_ml_norm_g"]).reshape(1, 768))
        if KINDS[l] == 2:
            shared[f"ng{l}"] = f(np.asarray(inputs[f"l{l}_ret_norm_g"]).reshape(1, 768))
    for n in ("w_out", "ln1_g", "ln1_b", "ln2_g", "ln2_b", "router_w", "router_b"):
        shared[n] = f(inputs[n])
    for n in ("exp_w_gu", "exp_w_down", "exp_b_down"):
        shared[n] = f(inputs[n][:nlayers])
    bgu = np.asarray(inputs["exp_b_gu"]).reshape(DEPTH, NE, 16, 128)
    shared["exp_b_gu_l"] = f(bgu.transpose(0, 3, 1, 2)[:nlayers])
    maps = []
    for c in cores:
        m = dict(shared)
        m["x"] = f(inputs["x"][c]); m["mem"] = f(inputs["mem"][c])
        m["pos"] = np.ascontiguousarray(np.asarray(inputs["positions"][c]).reshape(1, L).astype(np.int32))
        maps.append(m)
    return maps


def kernel(**inputs):
    nc = build()
    maps = make_in_maps(inputs)
    res = run_bass_kernel_spmd(nc, maps, core_ids=list(range(8)))
    return np.stack([np.asarray(r["out"]) for r in res.results], axis=0).astype(np.float32)
```
